# Optimizing a Trainium2 kernel written in Bass

```python
import math
import jax, jax.numpy as jnp
from jax import lax
import numpy as np

D_MODEL = 1024
BATCH = 2
SEQ = 8192
DEPTH = 4

N_MIXERS = 4
LN_EPS = 1e-5
NEG_BIG = -1e30
ML_HEADS = 8
ML_DQK = D_MODEL // 16
ML_DV = D_MODEL // 8
ML_CHUNK = 128
ML_F_BIAS_LO = 3.0
ML_F_BIAS_HI = 6.0
RET_HEADS = 8
RET_DQK = D_MODEL // RET_HEADS
RET_DV = 2 * D_MODEL // RET_HEADS
RET_CHUNK = 64
ROPE_BASE = 10000.0
GLA_HEADS = 4
GLA_DQK = D_MODEL // (2 * GLA_HEADS)
GLA_DV = D_MODEL // GLA_HEADS
GLA_RANK = 16
GLA_TAU = 16.0
GLA_CHUNK = 64
S5_GROUP = 16
S5_GROUPS = D_MODEL // S5_GROUP
S5_STATE = 64
N_EXPERTS = 32
TOP_K = 4
D_FF = D_MODEL
SWIGLU_LIMIT = 7.0
SWIGLU_ALPHA = 1.702
MOE_BLOCK = 256

kernel_name = 'hybrid_mlstm_retention_gla_s5_moe_deepnorm'


def _n_layers_of(kind):
    return (DEPTH - kind + N_MIXERS - 1) // N_MIXERS


def layer_norm(x, g, b):
    xf = x.astype(jnp.float32)
    mu = xf.mean(-1, keepdims=True)
    var = jnp.square(xf - mu).mean(-1, keepdims=True)
    return (xf - mu) * lax.rsqrt(var + LN_EPS) * g + b


def split_heads(t, n_heads):
    bn, t_len, _ = t.shape
    return t.reshape(bn, t_len, n_heads, -1).transpose(0, 2, 1, 3)


def head_norm(h, g):
    h = h.astype(jnp.float32)
    mu = h.mean(-1, keepdims=True)
    var = jnp.square(h - mu).mean(-1, keepdims=True)
    hn = (h - mu) * lax.rsqrt(var + LN_EPS)
    bn, nh, t_len, d = h.shape
    return hn.transpose(0, 2, 1, 3).reshape(bn, t_len, nh * d) * g


def rotary(t):
    t_len, d = t.shape[2], t.shape[3]
    inv = ROPE_BASE ** (-jnp.arange(0, d, 2, dtype=jnp.float32) / d)
    ang = jnp.arange(t_len, dtype=jnp.float32)[:, None] * inv[None, :]
    cos, sin = jnp.cos(ang), jnp.sin(ang)
    t1, t2 = t[..., : d // 2], t[..., d // 2:]
    return jnp.concatenate([t1 * cos - t2 * sin, t1 * sin + t2 * cos], axis=-1)


def mlstm_chunkwise(q, k, v, i_pre, f_pre):
    bn, nh, t_len, dk = q.shape
    dv = v.shape[-1]
    L = ML_CHUNK
    nc = t_len // L
    q = q.astype(jnp.float32).reshape(bn, nh, nc, L, dk) * (dk ** -0.5)
    k = k.astype(jnp.float32).reshape(bn, nh, nc, L, dk)
    v = v.astype(jnp.float32).reshape(bn, nh, nc, L, dv)
    ig = i_pre.reshape(bn, nh, nc, L)
    a = jnp.cumsum(jax.nn.log_sigmoid(f_pre).reshape(bn, nh, nc, L), axis=-1)
    a_tot = a[..., -1]
    w_log = a_tot[..., None] - a + ig
    m_loc = w_log.max(-1)
    w = jnp.exp(w_log - m_loc[..., None])
    c_loc = jnp.einsum('bhcl,bhcld,bhcle->bhcde', w, k, v)
    n_loc = jnp.einsum('bhcl,bhcld->bhcd', w, k)

    def step(carry, inp):
        c_st, n_st, m_st = carry
        cl, nl, ml, at = inp
        m_new = jnp.maximum(at + m_st, ml)
        s_old = jnp.exp(at + m_st - m_new)
        s_loc = jnp.exp(ml - m_new)
        c_new = s_old[..., None, None] * c_st + s_loc[..., None, None] * cl
        n_new = s_old[..., None] * n_st + s_loc[..., None] * nl
        return (c_new, n_new, m_new), (c_st, n_st, m_st)

    init = (jnp.zeros((bn, nh, dk, dv), jnp.float32), jnp.zeros((bn, nh, dk), jnp.float32),
            jnp.full((bn, nh), NEG_BIG, jnp.float32))
    xs = (jnp.moveaxis(c_loc, 2, 0), jnp.moveaxis(n_loc, 2, 0), jnp.moveaxis(m_loc, 2, 0), jnp.moveaxis(a_tot, 2, 0))
    _, (c_prev, n_prev, m_prev) = lax.scan(step, init, xs)
    c_prev = jnp.moveaxis(c_prev, 0, 2)
    n_prev = jnp.moveaxis(n_prev, 0, 2)
    m_prev = jnp.moveaxis(m_prev, 0, 2)
    causal = jnp.tril(jnp.ones((L, L), dtype=bool))
    d_log = jnp.where(causal, a[..., :, None] - a[..., None, :] + ig[..., None, :], -jnp.inf)
    inter_log = a + m_prev[..., None]
    m_t = jnp.maximum(inter_log, d_log.max(-1))
    s = jnp.einsum('bhcld,bhcsd->bhcls', q, k) * jnp.exp(d_log - m_t[..., None])
    inter_scale = jnp.exp(inter_log - m_t)
    num = jnp.einsum('bhcls,bhcse->bhcle', s, v) + inter_scale[..., None] * jnp.einsum('bhcld,bhcde->bhcle', q, c_prev)
    den = s.sum(-1) + inter_scale * jnp.einsum('bhcld,bhcd->bhcl', q, n_prev)
    h = num / jnp.maximum(jnp.abs(den), jnp.exp(-m_t))[..., None]
    return h.reshape(bn, nh, t_len, dv)


def chunked_decay_attention(q, k, v, log_a, L):
    bn, nh, t_len, dk = q.shape
    dv = v.shape[-1]
    nc = t_len // L
    q = q.astype(jnp.float32).reshape(bn, nh, nc, L, dk)
    k = k.astype(jnp.float32).reshape(bn, nh, nc, L, dk)
    v = v.astype(jnp.float32).reshape(bn, nh, nc, L, dv)
    b = jnp.cumsum(log_a.astype(jnp.float32).reshape(log_a.shape[:2] + (nc, L, log_a.shape[-1])), axis=3)
    b_end = b[:, :, :, -1:]
    q_in = q * jnp.exp(b)
    k_in = k * jnp.exp(-b)
    k_st = k * jnp.exp(b_end - b)
    causal = jnp.tril(jnp.ones((L, L), dtype=bool))
    scores = jnp.where(causal, jnp.einsum('bhcld,bhcsd->bhcls', q_in, k_in), 0.0)
    intra = jnp.einsum('bhcls,bhcse->bhcle', scores, v)
    u = jnp.einsum('bhcld,bhcle->bhcde', k_st, v)
    dec = jnp.exp(b_end[:, :, :, 0])

    def step(s_st, inp):
        u_c, dec_c = inp
        return dec_c[..., None] * s_st + u_c, s_st

    _, s_prev = lax.scan(step, jnp.zeros((bn, nh, dk, dv), jnp.float32), (jnp.moveaxis(u, 2, 0), jnp.moveaxis(dec, 2, 0)))
    s_prev = jnp.moveaxis(s_prev, 0, 2)
    inter = jnp.einsum('bhcld,bhcde->bhcle', q_in, s_prev)
    return (intra + inter).reshape(bn, nh, t_len, dv)


def mlstm_mixer(x, w_in, b_gates, norm_g, w_out):
    qk, vw = ML_HEADS * ML_DQK, ML_HEADS * ML_DV
    q, k, v, o, gates = jnp.split(x @ w_in, [qk, 2 * qk, 2 * qk + vw, 2 * qk + 2 * vw], axis=-1)
    gates = (gates + b_gates).astype(jnp.float32).transpose(0, 2, 1)
    h = mlstm_chunkwise(split_heads(q, ML_HEADS), split_heads(k, ML_HEADS), split_heads(v, ML_HEADS),
                        gates[:, :ML_HEADS], gates[:, ML_HEADS:])
    return (jax.nn.sigmoid(o) * head_norm(h, norm_g)) @ w_out


def retention_mixer(x, w_in, norm_g, w_out):
    qk, vw = RET_HEADS * RET_DQK, RET_HEADS * RET_DV
    q, k, v, g = jnp.split(x @ w_in, [qk, 2 * qk, 2 * qk + vw], axis=-1)
    q = rotary(split_heads(q, RET_HEADS).astype(jnp.float32))
    k = rotary(split_heads(k, RET_HEADS).astype(jnp.float32)) * (RET_DQK ** -0.5)
    log_gamma = jnp.log1p(-(2.0 ** (-5.0 - jnp.arange(RET_HEADS, dtype=jnp.float32))))
    log_a = jnp.broadcast_to(log_gamma[None, :, None, None], (1, RET_HEADS, x.shape[1], 1))
    o = chunked_decay_attention(q, k, split_heads(v, RET_HEADS), log_a, RET_CHUNK)
    return (jax.nn.silu(g) * head_norm(o, norm_g)) @ w_out


def gla_mixer(x, w_in, w_gate, b_gate, norm_g, w_out):
    qk, vw = GLA_HEADS * GLA_DQK, GLA_HEADS * GLA_DV
    q, k, v, r, z = jnp.split(x @ w_in, [qk, 2 * qk, 2 * qk + vw, 2 * qk + 2 * vw], axis=-1)
    log_a = jax.nn.log_sigmoid((z @ w_gate + b_gate).astype(jnp.float32)) / GLA_TAU
    k = split_heads(k, GLA_HEADS) * (GLA_DQK ** -0.5)
    o = chunked_decay_attention(split_heads(q, GLA_HEADS), k, split_heads(v, GLA_HEADS),
                                split_heads(log_a, GLA_HEADS), GLA_CHUNK)
    return (jax.nn.silu(r) * head_norm(o, norm_g)) @ w_out


def s5_mixer(x, w_in, lam_re, lam_im, b_re, b_im, c_re, c_im, d_skip, log_dt, w_glu, b_glu, w_out):
    f32 = jnp.float32
    bn, t_len, _ = x.shape
    u = (x @ w_in).astype(f32).reshape(bn, t_len, S5_GROUPS, S5_GROUP)
    lam = lax.complex(lam_re.astype(f32), lam_im.astype(f32))
    dt = jnp.exp(log_dt.astype(f32))[:, None]
    lam_bar = jnp.exp(lam * dt)
    b_bar = ((lam_bar - 1.0) / lam)[..., None] * lax.complex(b_re.astype(f32), b_im.astype(f32))
    bu = jnp.einsum('gph,btgh->btgp', b_bar, u.astype(jnp.complex64))
    a_elems = jnp.broadcast_to(lam_bar, bu.shape)

    def combine(e1, e2):
        a1, v1 = e1
        a2, v2 = e2
        return a2 * a1, a2 * v1 + v2

    _, states = lax.associative_scan(combine, (a_elems, bu), axis=1)
    c_mat = lax.complex(c_re.astype(f32), c_im.astype(f32))
    y = jnp.einsum('ghp,btgp->btgh', c_mat, states).real + d_skip.reshape(S5_GROUPS, S5_GROUP) * u
    y = jax.nn.gelu(y.reshape(bn, t_len, S5_GROUPS * S5_GROUP))
    y = y * jax.nn.sigmoid(y @ w_glu + b_glu)
    return y @ w_out


def moe_ffn(x, w_router, b_router, w_gate_up, b_gate_up, w_down, b_down):
    bn, t_len, dm = x.shape
    xf = x.reshape(-1, dm)
    n_tok = xf.shape[0]
    logits = (xf @ w_router + b_router).astype(jnp.float32)
    top_val, top_idx = lax.top_k(logits, TOP_K)
    gates = jax.nn.softmax(top_val, axis=-1)
    n_assign = n_tok * TOP_K
    e_flat = top_idx.reshape(-1)
    tok_flat = jnp.arange(n_assign, dtype=jnp.int32) // TOP_K
    order = jnp.argsort(e_flat)
    e_sorted = e_flat[order]
    tok_sorted = tok_flat[order]
    g_sorted = gates.reshape(-1)[order]
    counts = jnp.bincount(e_flat, length=N_EXPERTS)
    padded = (counts + MOE_BLOCK - 1) // MOE_BLOCK * MOE_BLOCK
    start = jnp.cumsum(counts) - counts
    pad_end = jnp.cumsum(padded)
    pad_start = pad_end - padded
    dest = pad_start[e_sorted] + (jnp.arange(n_assign, dtype=jnp.int32) - start[e_sorted])
    n_rows = -(-n_assign // MOE_BLOCK) * MOE_BLOCK + N_EXPERTS * MOE_BLOCK
    n_blocks = n_rows // MOE_BLOCK
    row_tok = jnp.zeros((n_rows,), jnp.int32).at[dest].set(tok_sorted)
    block_start = jnp.arange(n_blocks, dtype=jnp.int32) * MOE_BLOCK
    block_expert = jnp.minimum(jnp.searchsorted(pad_end, block_start, side='right'), N_EXPERTS - 1)
    x_rows = xf[row_tok].reshape(n_blocks, MOE_BLOCK, dm)

    def expert_block(args):
        xb, e = args
        hgu = xb @ w_gate_up[e] + b_gate_up[e]
        gate = jnp.minimum(hgu[:, :D_FF], SWIGLU_LIMIT)
        up = jnp.clip(hgu[:, D_FF:], -SWIGLU_LIMIT, SWIGLU_LIMIT)
        act = (up + 1.0) * gate * jax.nn.sigmoid(SWIGLU_ALPHA * gate)
        return act @ w_down[e] + b_down[e]

    y_rows = lax.map(expert_block, (x_rows, block_expert)).reshape(n_rows, dm)
    y = jnp.zeros((n_tok, dm), y_rows.dtype).at[tok_sorted].add(y_rows[dest] * g_sorted[:, None])
    return y.reshape(bn, t_len, dm)


def setup_inputs(seed: int = 0) -> dict:
    f32 = jnp.float32
    ks = list(jax.random.split(jax.random.key(seed), 48))
    beta = (8 * DEPTH) ** -0.25

    def nrm(shape, scale):
        return jax.random.normal(ks.pop(), shape, f32) * scale

    d = D_MODEL
    n_a, n_b, n_c, n_d = (_n_layers_of(m) for m in range(N_MIXERS))
    ml_cols = 2 * ML_HEADS * ML_DQK + 2 * ML_HEADS * ML_DV + 2 * ML_HEADS
    ret_cols = 2 * RET_HEADS * RET_DQK + 2 * RET_HEADS * RET_DV
    gla_cols = 2 * GLA_HEADS * GLA_DQK + 2 * GLA_HEADS * GLA_DV + GLA_RANK
    f_bias = jnp.linspace(ML_F_BIAS_LO, ML_F_BIAS_HI, ML_HEADS, dtype=f32)
    inp = {}
    inp['x'] = nrm((BATCH, SEQ, d), 1.0)
    inp['ln_g'] = 1.0 + nrm((DEPTH, 2, d), 0.02)
    inp['ln_b'] = nrm((DEPTH, 2, d), 0.02)
    inp['ml_w_in'] = nrm((n_a, d, ml_cols), d ** -0.5)
    inp['ml_b_gates'] = jnp.concatenate([jnp.zeros((n_a, ML_HEADS), f32), jnp.broadcast_to(f_bias, (n_a, ML_HEADS))], -1) + nrm((n_a, 2 * ML_HEADS), 0.1)
    inp['ml_norm_g'] = 1.0 + nrm((n_a, ML_HEADS * ML_DV), 0.02)
    inp['ml_w_out'] = nrm((n_a, ML_HEADS * ML_DV, d), beta * (ML_HEADS * ML_DV) ** -0.5)
    inp['ret_w_in'] = nrm((n_b, d, ret_cols), d ** -0.5)
    inp['ret_norm_g'] = 1.0 + nrm((n_b, RET_HEADS * RET_DV), 0.02)
    inp['ret_w_out'] = nrm((n_b, RET_HEADS * RET_DV, d), beta * (RET_HEADS * RET_DV) ** -0.5)
    inp['gla_w_in'] = nrm((n_c, d, gla_cols), d ** -0.5)
    inp['gla_w_gate'] = nrm((n_c, GLA_RANK, GLA_HEADS * GLA_DQK), GLA_RANK ** -0.5)
    inp['gla_b_gate'] = 2.0 + nrm((n_c, GLA_HEADS * GLA_DQK), 0.1)
    inp['gla_norm_g'] = 1.0 + nrm((n_c, GLA_HEADS * GLA_DV), 0.02)
    inp['gla_w_out'] = nrm((n_c, GLA_HEADS * GLA_DV, d), beta * (GLA_HEADS * GLA_DV) ** -0.5)
    inp['s5_w_in'] = nrm((n_d, d, d), d ** -0.5)
    inp['s5_lam_re'] = -0.5 + nrm((n_d, S5_GROUPS, S5_STATE), 0.01)
    inp['s5_lam_im'] = jnp.pi * jnp.arange(S5_STATE, dtype=f32) + nrm((n_d, S5_GROUPS, S5_STATE), 0.01)
    inp['s5_b_re'] = nrm((n_d, S5_GROUPS, S5_STATE, S5_GROUP), (2.0 * S5_GROUP) ** -0.5)
    inp['s5_b_im'] = nrm((n_d, S5_GROUPS, S5_STATE, S5_GROUP), (2.0 * S5_GROUP) ** -0.5)
    inp['s5_c_re'] = nrm((n_d, S5_GROUPS, S5_GROUP, S5_STATE), (2.0 * S5_STATE) ** -0.5)
    inp['s5_c_im'] = nrm((n_d, S5_GROUPS, S5_GROUP, S5_STATE), (2.0 * S5_STATE) ** -0.5)
    inp['s5_d'] = nrm((n_d, d), 1.0)
    inp['s5_log_dt'] = jax.random.uniform(ks.pop(), (n_d, S5_GROUPS), f32, math.log(1e-3), math.log(1e-1))
    inp['s5_w_glu'] = nrm((n_d, d, d), d ** -0.5)
    inp['s5_b_glu'] = nrm((n_d, d), 0.01)
    inp['s5_w_out'] = nrm((n_d, d, d), beta * d ** -0.5)
    inp['moe_w_router'] = nrm((DEPTH, d, N_EXPERTS), d ** -0.5)
    inp['moe_b_router'] = nrm((DEPTH, N_EXPERTS), 0.01)
    inp['moe_w_gate_up'] = nrm((DEPTH, N_EXPERTS, d, 2 * D_FF), d ** -0.5)
    inp['moe_b_gate_up'] = nrm((DEPTH, N_EXPERTS, 2 * D_FF), 0.01)
    inp['moe_w_down'] = nrm((DEPTH, N_EXPERTS, D_FF, d), beta * D_FF ** -0.5)
    inp['moe_b_down'] = nrm((DEPTH, N_EXPERTS, d), 0.01)
    return inp


def reference(x, ln_g, ln_b, ml_w_in, ml_b_gates, ml_norm_g, ml_w_out, ret_w_in, ret_norm_g, ret_w_out,
              gla_w_in, gla_w_gate, gla_b_gate, gla_norm_g, gla_w_out, s5_w_in, s5_lam_re, s5_lam_im,
              s5_b_re, s5_b_im, s5_c_re, s5_c_im, s5_d, s5_log_dt, s5_w_glu, s5_b_glu, s5_w_out,
              moe_w_router, moe_b_router, moe_w_gate_up, moe_b_gate_up, moe_w_down, moe_b_down):
    alpha = (2 * DEPTH) ** 0.25
    h = x.astype(jnp.float32)
    for layer in range(DEPTH):
        kind, j = layer % N_MIXERS, layer // N_MIXERS
        if kind == 0:
            mix = mlstm_mixer(h, ml_w_in[j], ml_b_gates[j], ml_norm_g[j], ml_w_out[j])
        elif kind == 1:
            mix = retention_mixer(h, ret_w_in[j], ret_norm_g[j], ret_w_out[j])
        elif kind == 2:
            mix = gla_mixer(h, gla_w_in[j], gla_w_gate[j], gla_b_gate[j], gla_norm_g[j], gla_w_out[j])
        else:
            mix = s5_mixer(h, s5_w_in[j], s5_lam_re[j], s5_lam_im[j], s5_b_re[j], s5_b_im[j], s5_c_re[j],
                           s5_c_im[j], s5_d[j], s5_log_dt[j], s5_w_glu[j], s5_b_glu[j], s5_w_out[j])
        h = layer_norm(alpha * h + mix, ln_g[layer, 0], ln_b[layer, 0])
        ffn = moe_ffn(h, moe_w_router[layer], moe_b_router[layer], moe_w_gate_up[layer],
                      moe_b_gate_up[layer], moe_w_down[layer], moe_b_down[layer])
        h = layer_norm(alpha * h + ffn, ln_g[layer, 1], ln_b[layer, 1])
    return h.astype(x.dtype)
```

```python
import contextlib
import numpy as np
import ml_dtypes
import concourse.bass as bass
import concourse.mybir as mybir
from concourse.bass_utils import run_bass_kernel_spmd

F32 = mybir.dt.float32
BF16 = mybir.dt.bfloat16
AF = mybir.ActivationFunctionType
ALU = mybir.AluOpType
AX = mybir.AxisListType
NPBF = ml_dtypes.bfloat16

NCORES = 8
ALPHA = 8.0 ** 0.25
EPS = 1e-5


class Tr:
    __slots__ = ("w", "r")

    def __init__(self):
        self.w = None
        self.r = {}


class V:
    __slots__ = ("ap", "trs")

    def __init__(self, ap, trs):
        self.ap = ap
        self.trs = trs


class Buf:
    def __init__(self, handle, trs=None):
        self.h = handle
        self.trs = trs if trs is not None else (Tr(),)

    def __getitem__(self, key):
        return V(self.h[key], self.trs)

    def v(self, ap):
        return V(ap, self.trs)


WRITE_KW = ("out", "accum_out", "ap", "out_ap")


class Prog:
    ENG = ("pe", "dve", "act", "pool", "sp")
    DMAQ = ("sp", "pool", "act")

    def __init__(self, nc, ndma=6):
        self.nc = nc
        self.es = contextlib.ExitStack()
        self.stream = {e: [] for e in self.ENG}
        self.cnt = {e: 0 for e in self.ENG}
        self.sem = {}
        for e in self.ENG:
            self.sem["c_" + e] = self.es.enter_context(nc.semaphore("c_" + e))
        self.known = {e: {} for e in self.ENG}
        self.ndma = ndma
        for q in self.DMAQ:
            for i in range(ndma):
                self.sem[f"d_{q}{i}"] = self.es.enter_context(nc.semaphore(f"d_{q}{i}"))
        self.dval = {q: [0] * ndma for q in self.DMAQ}
        self.dnext = {q: 0 for q in self.DMAQ}

    def sb(self, name, shape, dtype):
        return Buf(self.nc.alloc_sbuf_tensor(name, list(shape), dtype))

    def ps(self, name, shape, dtype=F32):
        return Buf(self.nc.alloc_psum_tensor(name, list(shape), dtype))

    def dram(self, name, shape, dtype, kind="Internal"):
        return Buf(self.nc.dram_tensor(name, list(shape), dtype, kind=kind))

    def _deps(self, reads, writes):
        deps = []
        for t in reads:
            if t.w is not None:
                deps.append(t.w)
        for t in writes:
            if t.w is not None:
                deps.append(t.w)
            deps.extend(t.r.items())
        return deps

    def _waits(self, eng, deps, skip_self=False):
        kn = self.known[eng]
        need = {}
        own = "c_" + eng
        for key, val in deps:
            if skip_self and key == own:
                continue
            if kn.get(key, 0) >= val:
                continue
            if need.get(key, 0) < val:
                need[key] = val
        for key, val in need.items():
            kn[key] = val
        return [(self.sem[k], v) for k, v in need.items()]

    def _mark(self, tok, reads, writes):
        for t in reads:
            if t.r.get(tok[0], 0) < tok[1]:
                t.r[tok[0]] = tok[1]
        for t in writes:
            t.w = tok
            t.r = {}

    def op(self, eng, fn, reads=(), writes=(), skip_self=False):
        deps = self._deps(reads, writes)
        waits = self._waits(eng, deps, skip_self)
        self.cnt[eng] += 1
        tok = ("c_" + eng, self.cnt[eng])
        self.stream[eng].append((waits, fn, (self.sem[tok[0]], 1)))
        self._mark(tok, reads, writes)
        return tok

    def I(self, eng, name, **kw):
        reads, writes, args = [], [], {}
        for k, v in kw.items():
            if isinstance(v, V):
                args[k] = v.ap
                (writes if k in WRITE_KW else reads).extend(v.trs)
            else:
                args[k] = v
        return self.op(eng, lambda e: getattr(e, name)(**args), reads, writes)

    def mm(self, out, lhsT, rhs, start=True, stop=True):
        o, l, r = out.ap, lhsT.ap, rhs.ap
        return self.op("pe", lambda e: e.matmul(o, l, r, start=start, stop=stop),
                       list(lhsT.trs) + list(rhs.trs), list(out.trs), skip_self=True)

    def tp(self, out, in_, ident):
        o, i, d = out.ap, in_.ap, ident.ap
        return self.op("pe", lambda e: e.transpose(o, i, d),
                       list(in_.trs) + list(ident.trs), list(out.trs), skip_self=True)

    def dma(self, q, out, in_, **kw):
        reads, writes = list(in_.trs), list(out.trs)
        deps = self._deps(reads, writes)
        i = self.dnext[q]
        self.dnext[q] = (i + 1) % self.ndma
        key = f"d_{q}{i}"
        prev = self.dval[q][i]
        if prev > 0:
            deps.append((key, prev))
        self.dval[q][i] = prev + 16
        tok = (key, prev + 16)
        waits = self._waits(q, deps)
        o, a = out.ap, in_.ap
        self.stream[q].append((waits, lambda e: e.dma_start(out=o, in_=a, **kw), (self.sem[key], 16)))
        self._mark(tok, reads, writes)
        return tok

    def emit(self):
        waits = []
        for q in self.DMAQ:
            for i in range(self.ndma):
                if self.dval[q][i] > 0:
                    waits.append((self.sem[f"d_{q}{i}"], self.dval[q][i]))
        for e in self.ENG:
            if e != "sp" and self.cnt[e] > 0:
                waits.append((self.sem["c_" + e], self.cnt[e]))
        self.stream["sp"].append((waits, None, None))
        with self.nc.Block() as block:
            decos = {"pe": block.tensor, "dve": block.vector, "act": block.scalar,
                     "pool": block.gpsimd, "sp": block.sync}
            for e in self.ENG:
                items = self.stream[e]
                if not items:
                    continue

                def body(engine, items=items):
                    for waits, fn, inc in items:
                        for sem, val in waits:
                            engine.wait_ge(sem, val)
                        if fn is not None:
                            fn(engine).then_inc(inc[0], inc[1])

                decos[e](body)
        self.es.close()
        return self.nc


def layer_norm_tile(P, src, dst, gt, bt, st, mv, rs, eng2="pool"):
    for i in range(2):
        P.I("dve", "bn_stats", out=st[:, i, :], in_=src(slice(i * 512, (i + 1) * 512)))
    P.I("dve", "bn_aggr", out=mv[:, :], in_=st[:, :, :])
    P.I("act", "activation", out=rs[:, :], in_=mv[:, 1:2], func=AF.Sqrt, bias=EPS, scale=1.0)
    P.I("dve", "reciprocal", out=rs[:, :], in_=rs[:, :])
    full = slice(0, 1024)
    P.I("dve", "tensor_scalar", out=dst(full), in0=src(full), scalar1=mv[:, 0:1], scalar2=rs[:, 0:1],
        op0=ALU.subtract, op1=ALU.mult)
    P.I(eng2, "tensor_tensor", out=dst(full), in0=dst(full), in1=gt[:, :], op=ALU.mult)
    P.I(eng2, "tensor_tensor", out=dst(full), in0=dst(full), in1=bt[:, :], op=ALU.add)


NT = 16
NE = 32
NQ = 4
NB = 4


def build_l2(KC, n_exp=NE, dbg=0, glu=False):
    nc = bass.Bass("TRN2", target_bir_lowering=False)
    P = Prog(nc)
    Dm = KC * 128
    mixT = P.dram("mixT", [Dm, 2048], BF16, kind="ExternalInput")
    hin = P.dram("hin", [2048, 1024], F32, kind="ExternalInput")
    w_out = P.dram("w_out", [Dm, 1024], F32, kind="ExternalInput")
    lng = P.dram("lng", [2, 1024], F32, kind="ExternalInput")
    lnb = P.dram("lnb", [2, 1024], F32, kind="ExternalInput")
    w_r = P.dram("w_r", [1024, 32], F32, kind="ExternalInput")
    b_r = P.dram("b_r", [1, 32], F32, kind="ExternalInput")
    w_gu = P.dram("w_gu", [max(n_exp, 1), 1024, 2048], F32, kind="ExternalInput")
    b_gu = P.dram("b_gu", [128, NE, 16], F32, kind="ExternalInput")
    w_d = P.dram("w_d", [max(n_exp, 1), 1024, 1024], F32, kind="ExternalInput")
    b_d = P.dram("b_d", [NE, 1024], F32, kind="ExternalInput")
    idn = P.dram("idn", [128, 128], F32, kind="ExternalInput")
    hout = P.dram("hout", [2048, 1024], F32, kind="ExternalOutput")
    if glu:
        ytok = P.dram("ytok", [2048, 1024], BF16, kind="ExternalInput")
        w_glu = P.dram("w_glu", [1024, 1024], F32, kind="ExternalInput")
        b_glu = P.dram("b_glu", [1, 1024], F32, kind="ExternalInput")

    acc = P.sb("acc", [128, NT, 1024], F32)
    xT = P.sb("xT", [128, 8, 2048], BF16)
    G = P.sb("G", [128, NT, 32], F32)
    PIECE = 8 * 512 + 2 * 1024
    slot_tr = [Tr(), Tr(), Tr()]
    arena_h = nc.alloc_sbuf_tensor("arena", [128, 3 * PIECE], BF16)
    wout_sb = Buf(arena_h, tuple(slot_tr))
    slots = [Buf(arena_h, (slot_tr[i],)) for i in range(3)]
    idt = P.sb("idt", [128, 128], F32)
    gt = P.sb("gt", [128, 1024], F32)
    bt = P.sb("bt", [128, 1024], F32)
    wr_sb = P.sb("wr_sb", [128, 8, 32], F32)
    br_sb = P.sb("br_sb", [128, 32], F32)
    bgu_sb = P.sb("bgu_sb", [128, NE, 16], F32)
    bd_sb = P.sb("bd_sb", [128, 1024], F32)
    Gpad = P.sb("Gpad", [128, 128], F32)
    mix_sb = [P.sb(f"mix_sb{i}", [128, KC, 128], BF16) for i in range(2)]
    hin_sb = [P.sb(f"hin_sb{i}", [128, 1024], F32) for i in range(1)]
    rbuf = [P.sb(f"rbuf{i}", [128, 1024], F32) for i in range(1)]
    hm = [P.sb(f"hm{i}", [128, 1024], F32) for i in range(2)]
    hT32 = P.sb("hT32", [128, 8, 128], F32)
    st = P.sb("st", [128, 2, 6], F32)
    mv = P.sb("mv", [128, 2], F32)
    rs = P.sb("rs", [128, 1], F32)
    lg = P.sb("lg", [128, 32], F32)
    m8 = P.sb("m8", [128, 8], F32)
    msk = P.sb("msk", [128, 32], F32)
    nmx = P.sb("nmx", [128, 1], F32)
    ex = P.sb("ex", [128, 32], F32)
    den = P.sb("den", [128, 1], F32)
    GT = P.sb("GT", [128, 128], F32)
    gbuf = [P.sb(f"gbuf{i}", [128, 512], F32) for i in range(2)]
    sgbuf = [P.sb(f"sgbuf{i}", [128, 512], F32) for i in range(2)]
    tbuf = [P.sb(f"tbuf{i}", [128, 512], F32) for i in range(2)]
    actT = [P.sb(f"actT{i}", [128, 2, 512], BF16) for i in range(2)]
    ev = [P.sb(f"ev{i}", [128, 512], F32) for i in range(2)]
    pb = [P.ps(f"pb{i}", [128, 512], F32) for i in range(8)]

    P.dma("sp", idt[:, :], idn[:, :])
    P.dma("sp", gt[:, :], lng.v(lng.h[0:1, :].partition_broadcast(128)))
    P.dma("sp", bt[:, :], lnb.v(lnb.h[0:1, :].partition_broadcast(128)))
    P.dma("sp", wr_sb[:, :, :], w_r.v(w_r.h.rearrange("(c p) n -> p c n", p=128)))
    P.dma("sp", br_sb[:, :], b_r.v(b_r.h[0:1, :].partition_broadcast(128)))
    P.dma("sp", bgu_sb[:, :, :], b_gu[:, :, :])
    P.I("pool", "memset", ap=bd_sb[:, :], constant=0.0)
    P.I("pool", "memset", ap=Gpad[:, :], constant=0.0)
    P.dma("sp", bd_sb[0:32, :], b_d[:, :])
    wout_v = wout_sb.v(arena_h[:, 0:KC * 1024].rearrange("p (c n) -> p c n", c=KC))
    for c0 in range(0, KC, 4):
        P.dma("pool", wout_sb.v(arena_h[:, c0 * 1024:(c0 + 4) * 1024].rearrange("p (c n) -> p c n", c=4)),
              w_out.v(w_out.h[c0 * 128:(c0 + 4) * 128, :].rearrange("(c p) n -> p c n", p=128)))
    if glu:
        wglu_v = wout_sb.v(arena_h[:, 8192:16384].rearrange("p (c n) -> p c n", c=8))
        for c0 in range(0, 8, 4):
            P.dma("pool", wout_sb.v(arena_h[:, 8192 + c0 * 1024:8192 + (c0 + 4) * 1024].rearrange("p (c n) -> p c n", c=4)),
                  w_glu.v(w_glu.h[c0 * 128:(c0 + 4) * 128, :].rearrange("(c p) n -> p c n", p=128)))
        bglu_sb = P.sb("bglu_sb", [128, 1024], F32)
        P.dma("sp", bglu_sb[:, :], b_glu.v(b_glu.h[0:1, :].partition_broadcast(128)))
        yt_sb = [P.sb(f"yt_sb{i}", [128, 1024], BF16) for i in range(1)]
        gms = [P.sb(f"gms{i}", [128, 8, 128], BF16) for i in range(1)]
    P.I("dve", "tensor_scalar", out=bgu_sb[:, :, 8:16], in0=bgu_sb[:, :, 8:16], scalar1=1.0, scalar2=None,
        op0=ALU.add)

    for i in range(NT):
        ms, hs, rb, hmt = mix_sb[i % 2], hin_sb[0], rbuf[0], hm[i % 2]
        tok = slice(i * 128, (i + 1) * 128)
        P.dma("sp", ms[:, :, :], mixT.v(mixT.h[:, tok].rearrange("(c p) t -> p c t", p=128)))
        P.dma("sp", hs[:, :], hin[tok, :])
        if glu:
            yt = yt_sb[0]
            P.dma("sp", yt[:, :], ytok[tok, :])
            for nh in range(2):
                for c in range(8):
                    P.mm(pb[6 + nh][:, :], ms[:, c, :], wout_sb.v(wglu_v.ap[:, c, nh * 512:(nh + 1) * 512]),
                         start=(c == 0), stop=(c == 7))
                P.I("dve", "tensor_tensor", out=rb[:, nh * 512:(nh + 1) * 512], in0=pb[6 + nh][:, :],
                    in1=bglu_sb[:, nh * 512:(nh + 1) * 512], op=ALU.add)
            P.I("act", "activation", out=rb[:, :], in_=rb[:, :], func=AF.Sigmoid)
            P.I("pool", "tensor_tensor", out=hmt[:, :], in0=rb[:, :], in1=yt[:, :], op=ALU.mult)
            for c in range(8):
                P.tp(pb[2 + c // 4][:, (c % 4) * 128:(c % 4 + 1) * 128], hmt[:, c * 128:(c + 1) * 128], idt[:, :])
            ms = gms[0]
            for half in range(2):
                src = pb[2 + half].v(pb[2 + half].h[:, :].rearrange("p (c n) -> p c n", c=4))
                P.I("act", "activation", out=ms[:, half * 4:(half + 1) * 4, :], in_=src, func=AF.Copy)
        for nh in range(2):
            for c in range(KC):
                P.mm(pb[nh][:, :], ms[:, c, :], wout_sb.v(wout_v.ap[:, c, nh * 512:(nh + 1) * 512]),
                     start=(c == 0), stop=(c == KC - 1))
            P.I("dve", "scalar_tensor_tensor", out=rb[:, nh * 512:(nh + 1) * 512],
                in0=hs[:, nh * 512:(nh + 1) * 512], scalar=ALPHA, in1=pb[nh][:, :],
                op0=ALU.mult, op1=ALU.add)
        layer_norm_tile(P, lambda s: rb[:, s], lambda s: hmt[:, s], gt, bt, st, mv, rs)
        if dbg == 1:
            P.dma("sp", hout[i * 128:(i + 1) * 128, :], hmt[:, :])
            continue
        P.I("act", "activation", out=acc[:, i, :], in_=hmt[:, :], func=AF.Copy, scale=ALPHA)
        for c in range(8):
            P.tp(pb[2 + c // 4][:, (c % 4) * 128:(c % 4 + 1) * 128], hmt[:, c * 128:(c + 1) * 128], idt[:, :])
        for half in range(2):
            src = pb[2 + half].v(pb[2 + half].h[:, :].rearrange("p (c n) -> p c n", c=4))
            P.I("act", "activation", out=hT32[:, half * 4:(half + 1) * 4, :], in_=src, func=AF.Copy)
            P.I("dve", "tensor_copy", out=xT[:, half * 4:(half + 1) * 4, tok], in_=hT32[:, half * 4:(half + 1) * 4, :])
        if dbg == 2:
            continue
        for c in range(8):
            P.mm(pb[4][:, 0:32], hT32[:, c, :], wr_sb[:, c, :], start=(c == 0), stop=(c == 7))
        P.I("dve", "tensor_tensor", out=lg[:, :], in0=pb[4][:, 0:32], in1=br_sb[:, :], op=ALU.add)
        P.I("dve", "max", out=m8[:, :], in_=lg[:, :])
        P.I("dve", "tensor_scalar", out=msk[:, :], in0=lg[:, :], scalar1=m8[:, 3:4], scalar2=None, op0=ALU.is_ge)
        P.I("dve", "tensor_scalar", out=nmx[:, :], in0=m8[:, 0:1], scalar1=-1.0, scalar2=None, op0=ALU.mult)
        P.I("act", "activation", out=ex[:, :], in_=lg[:, :], func=AF.Exp, bias=nmx[:, 0:1], scale=1.0)
        P.I("dve", "tensor_tensor", out=ex[:, :], in0=ex[:, :], in1=msk[:, :], op=ALU.mult)
        P.I("dve", "reduce_sum", out=den[:, :], in_=ex[:, :], axis=AX.X)
        P.I("dve", "reciprocal", out=den[:, :], in_=den[:, :])
        P.I("dve", "tensor_scalar", out=G[:, i, :], in0=ex[:, :], scalar1=den[:, 0:1], scalar2=None, op0=ALU.mult)
        if dbg == 3:
            continue
        P.I("dve", "tensor_copy", out=Gpad[:, 0:32], in_=G[:, i, :])
        P.tp(pb[5][:, 0:128], Gpad[:, :], idt[:, :])
        P.I("dve", "tensor_copy", out=GT[:, :], in_=pb[5][:, 0:128])
        for nh in range(2):
            P.mm(pb[6 + nh][:, :], GT[:, :], bd_sb[:, nh * 512:(nh + 1) * 512])
            P.I("dve", "tensor_tensor", out=acc[:, i, nh * 512:(nh + 1) * 512],
                in0=acc[:, i, nh * 512:(nh + 1) * 512], in1=pb[6 + nh][:, :], op=ALU.add)

    if dbg == 1:
        return P.emit()
    P.dma("sp", gt[:, :], lng.v(lng.h[1:2, :].partition_broadcast(128)))
    P.dma("sp", bt[:, :], lnb.v(lnb.h[1:2, :].partition_broadcast(128)))

    pieces = [(e, q) for e in range(n_exp) for q in range(NQ)]

    def load_piece(k):
        e, q = pieces[k]
        sl = slots[k % 3]
        gu_v = sl.v(arena_h[:, (k % 3) * PIECE:(k % 3) * PIECE + 4096].rearrange("p (c n) -> p c n", c=8))
        d_v = sl.v(arena_h[:, (k % 3) * PIECE + 4096:(k % 3 + 1) * PIECE].rearrange("p (c n) -> p c n", c=2))
        P.dma("pool", sl.v(gu_v.ap[:, :, 0:256]),
              w_gu.v(w_gu.h[e, :, q * 256:(q + 1) * 256].rearrange("(c p) n -> p c n", p=128)))
        P.dma("pool", sl.v(gu_v.ap[:, :, 256:512]),
              w_gu.v(w_gu.h[e, :, 1024 + q * 256:1024 + (q + 1) * 256].rearrange("(c p) n -> p c n", p=128)))
        P.dma("pool", d_v, w_d.v(w_d.h[e, q * 256:(q + 1) * 256, :].rearrange("(c p) n -> p c n", p=128)))
        return gu_v, d_v

    views = {}
    for k0 in range(min(2, len(pieces))):
        views[k0] = load_piece(k0)
    it = 0
    evi = 0
    for k, (e, q) in enumerate(pieces):
        if k + 2 < len(pieces):
            views[k + 2] = load_piece(k + 2)
        gu_v, d_v = views.pop(k)
        sl = slots[k % 3]
        for b in range(NB):
            at = actT[it % 2]
            tokb = slice(b * 512, (b + 1) * 512)
            for jj in range(2):
                gps, ups = pb[(it * 2 + jj) % 2], pb[2 + (it * 2 + jj) % 2]
                gb, sgb, tb = gbuf[jj], sgbuf[jj], tbuf[jj]
                for c in range(8):
                    P.mm(gps[:, :], sl.v(gu_v.ap[:, c, jj * 128:(jj + 1) * 128]), xT[:, c, tokb],
                         start=(c == 0), stop=(c == 7))
                for c in range(8):
                    P.mm(ups[:, :], sl.v(gu_v.ap[:, c, 256 + jj * 128:256 + (jj + 1) * 128]), xT[:, c, tokb],
                         start=(c == 0), stop=(c == 7))
                ch = q * 2 + jj
                P.I("dve", "tensor_scalar", out=gb[:, :], in0=gps[:, :], scalar1=bgu_sb[:, e, ch:ch + 1],
                    scalar2=7.0, op0=ALU.add, op1=ALU.min)
                P.I("act", "activation", out=sgb[:, :], in_=gb[:, :], func=AF.Sigmoid, scale=1.702)
                P.I("pool", "tensor_tensor", out=sgb[:, :], in0=sgb[:, :], in1=gb[:, :], op=ALU.mult)
                P.I("dve", "tensor_scalar", out=tb[:, :], in0=ups[:, :], scalar1=bgu_sb[:, e, 8 + ch:9 + ch],
                    scalar2=8.0, op0=ALU.add, op1=ALU.min)
                P.I("dve", "scalar_tensor_tensor", out=at[:, jj, :], in0=tb[:, :], scalar=-6.0, in1=sgb[:, :],
                    op0=ALU.max, op1=ALU.mult)
            for tt in range(4):
                ti = b * 4 + tt
                for nh in range(2):
                    yps = pb[4 + evi % 4]
                    evb = ev[evi % 2]
                    evi += 1
                    for jj in range(2):
                        P.mm(yps[:, :], at[:, jj, tt * 128:(tt + 1) * 128], sl.v(d_v.ap[:, jj, nh * 512:(nh + 1) * 512]),
                             start=(jj == 0), stop=(jj == 1))
                    P.I("act", "activation", out=evb[:, :], in_=yps[:, :], func=AF.Copy, scale=G[:, ti, e:e + 1])
                    P.I("pool", "tensor_tensor", out=acc[:, ti, nh * 512:(nh + 1) * 512],
                        in0=acc[:, ti, nh * 512:(nh + 1) * 512], in1=evb[:, :], op=ALU.add)
            it += 1

    for i in range(NT):
        ob = hm[i % 2]
        layer_norm_tile(P, lambda s: acc[:, i, s], lambda s: ob[:, s], gt, bt, st, mv, rs)
        P.dma("sp", hout[i * 128:(i + 1) * 128, :], ob[:, :])
    return P.emit()


L1CFG = {"ml": dict(HPC=2, dk=64, dv=128), "ret": dict(HPC=2, dk=128, dv=256), "gla": dict(HPC=1, dk=128, dv=256)}
SEQ = 8192
NCH = SEQ // 128


def build_l1(kind, nch=NCH):
    cfg = L1CFG[kind]
    HPC, dk, dv = cfg["HPC"], cfg["dk"], cfg["dv"]
    dvx = dv + 1 if kind == "ml" else dv
    cscale = float(dk) ** -0.5
    nc = bass.Bass("TRN2", target_bir_lowering=False)
    P = Prog(nc)
    IN = dict(kind="ExternalInput")
    xT = P.dram("xT", [1024, SEQ], F32, **IN)
    nq = 2 * HPC * dk if kind == "ret" else HPC * dk
    wq = P.dram("wq", [1024, nq], F32, **IN)
    wk = P.dram("wk", [1024, nq], F32, **IN)
    wv = P.dram("wv", [1024, HPC * dv], F32, **IN)
    wg = P.dram("wg", [1024, HPC * dv], F32, **IN)
    ng = P.dram("ng", [1, HPC * dv], F32, **IN)
    tri = P.dram("tri", [128, 128], F32, **IN)
    o = P.dram("o", [SEQ, HPC * dv], BF16, kind="ExternalOutput")
    if kind == "ret":
        cosT = P.dram("cosT", [128, SEQ], F32, **IN)
        sinT = P.dram("sinT", [128, SEQ], F32, **IN)
        cosk = P.dram("cosk", [SEQ, 128], F32, **IN)
        sink = P.dram("sink", [SEQ, 128], F32, **IN)
        lgc = P.dram("lgc", [128, HPC * 128], F32, **IN)
    if kind == "gla":
        wz = P.dram("wz", [1024, 16], F32, **IN)
        wga = P.dram("wga", [128, 128], F32, **IN)
    if kind == "ml":
        wgt = P.dram("wgt", [1024, 2 * HPC], F32, **IN)
        bgt = P.dram("bgt", [1, 2 * HPC], F32, **IN)

    wq_sb = P.sb("wq_sb", [128, 8, nq], BF16)
    wk_sb = P.sb("wk_sb", [128, 8, nq], BF16)
    wv_sb = P.sb("wv_sb", [128, 8, HPC * dv], BF16)
    wg_sb = P.sb("wg_sb", [128, 8, HPC * dv], BF16)
    ng_sb = P.sb("ng_sb", [128, HPC * dv], F32)
    tri_sb = P.sb("tri_sb", [128, 128], F32)
    for dst, src in ((wq_sb, wq), (wk_sb, wk), (wv_sb, wv), (wg_sb, wg)):
        P.dma("pool", dst[:, :, :], src.v(src.h.rearrange("(c p) n -> p c n", p=128)))
    P.dma("sp", ng_sb[:, :], ng.v(ng.h[0:1, :].partition_broadcast(128)))
    P.dma("sp", tri_sb[:, :], tri[:, :])
    lg = P.sb("lg", [128, 128], F32)
    if kind == "ret":
        lgc_sb = P.sb("lgc_sb", [128, HPC * 128], F32)
        P.dma("sp", lgc_sb[:, :], lgc[:, :])
        tabs = [[P.sb(f"tab{i}_{j}", [128, 128], F32) for j in range(4)] for i in range(2)]
    if kind == "gla":
        wz_sb = P.sb("wz_sb", [128, 8, 16], BF16)
        P.dma("pool", wz_sb[:, :, :], wz.v(wz.h.rearrange("(c p) n -> p c n", p=128)))
        wga_sb = P.sb("wga_sb", [128, 128], F32)
        P.dma("sp", wga_sb[:, :], wga[:, :])
        zaug = P.sb("zaug", [128, 128], F32)
        P.I("pool", "memset", ap=zaug[:, :], constant=1.0)
        esb = P.sb("esb", [128, 128], F32)
    if kind == "ml":
        wgt_sb = P.sb("wgt_sb", [128, 8, 2 * HPC], BF16)
        P.dma("pool", wgt_sb[:, :, :], wgt.v(wgt.h.rearrange("(c p) n -> p c n", p=128)))
        bgt_sb = P.sb("bgt_sb", [128, 2 * HPC], F32)
        P.dma("sp", bgt_sb[:, :], bgt.v(bgt.h[0:1, :].partition_broadcast(128)))
        gsb = P.sb("gsb", [128, 2 * HPC], F32)
        lf = P.sb("lf", [128, HPC], F32)
        ei = P.sb("ei", [128, HPC], F32)
        dd = P.sb("dd", [128, 1], F32)
    xc = [P.sb(f"xc{i}", [128, 8, 128], BF16) for i in range(2)]
    S = [P.sb(f"S{j}", [128, dvx], F32) for j in range(HPC)]
    Sbf = [P.sb(f"Sbf{j}", [128, dvx], BF16) for j in range(HPC)]
    for j in range(HPC):
        P.I("pool", "memset", ap=S[j][:, :], constant=0.0)
        P.I("pool", "memset", ap=Sbf[j][:, :], constant=0.0)
    eT = P.sb("eT", [128, 128], F32)
    enT = P.sb("enT", [128, 128], F32)
    ent = P.sb("ent", [128, 128], F32)
    tmp = P.sb("tmp", [128, 128], F32)
    qr = P.sb("qr", [128, 128], F32)
    A = P.sb("A", [128, 128], BF16)
    B = P.sb("B", [128, 128], BF16)
    C = P.sb("C", [128, 128], BF16)
    D = P.sb("D", [128, dvx], BF16)
    sT = P.sb("sT", [128, 128], BF16)
    hn = P.sb("hn", [128, dv], F32)
    gate = P.sb("gate", [128, dv], F32)
    st6 = P.sb("st6", [128, 6], F32)
    mv = P.sb("mv", [128, 2], F32)
    rs = P.sb("rs", [128, 1], F32)
    ob = [P.sb(f"ob{i}", [128, HPC * dv], BF16) for i in range(2)]
    pb = [P.ps(f"pb{i}", [128, 512], F32) for i in range(8)]

    def proj_fm(dst, w_sb, col0, ncol, x):
        for c in range(8):
            P.mm(dst, w_sb[:, c, col0:col0 + ncol], x[:, c, :], start=(c == 0), stop=(c == 7))

    def proj_tm(dst, w_sb, col0, ncol, x):
        for c in range(8):
            P.mm(dst, x[:, c, :], w_sb[:, c, col0:col0 + ncol], start=(c == 0), stop=(c == 7))

    for ci in range(nch):
        x = xc[ci % 2]
        tok = slice(ci * 128, (ci + 1) * 128)
        P.dma("pool", x[:, :, :], xT.v(xT.h[:, tok].rearrange("(c p) t -> p c t", p=128)))
        if kind == "ret":
            tb = tabs[ci % 2]
            P.dma("sp", tb[0][:, :], cosT[:, tok])
            P.dma("sp", tb[1][:, :], sinT[:, tok])
            P.dma("sp", tb[2][:, :], cosk[tok, :])
            P.dma("sp", tb[3][:, :], sink[tok, :])
        if kind == "ml":
            proj_tm(pb[7][:, 0:2 * HPC], wgt_sb, 0, 2 * HPC, x)
            P.I("dve", "tensor_tensor", out=gsb[:, :], in0=pb[7][:, 0:2 * HPC], in1=bgt_sb[:, :], op=ALU.add)
            P.I("act", "activation", out=ei[:, :], in_=gsb[:, 0:HPC], func=AF.Exp)
            P.I("act", "activation", out=lf[:, :], in_=gsb[:, HPC:2 * HPC], func=AF.Exp, scale=-1.0)
            P.I("act", "activation", out=lf[:, :], in_=lf[:, :], func=AF.Ln, bias=1.0)
            P.I("dve", "tensor_scalar", out=lf[:, :], in0=lf[:, :], scalar1=-1.0, scalar2=None, op0=ALU.mult)
        obuf = ob[ci % 2]
        for j in range(HPC):
            qT_ps, kT_ps = pb[0][0:dk, 0:128], pb[0][0:dk, 128:256]
            kt_ps = pb[1][:, 0:dk]
            vt_ps, gt_ps = pb[2][:, 0:dv], pb[2][:, 256:256 + dv]
            proj_fm(qT_ps, wq_sb, j * dk, dk, x)
            proj_fm(kT_ps, wk_sb, j * dk, dk, x)
            proj_tm(kt_ps, wk_sb, j * dk, dk, x)
            proj_tm(vt_ps, wv_sb, j * dv, dv, x)
            proj_tm(gt_ps, wg_sb, j * dv, dv, x)
            if kind == "ret":
                qsT_ps, ksT_ps = pb[0][0:dk, 256:384], pb[0][0:dk, 384:512]
                kst_ps = pb[1][:, 128:256]
                proj_fm(qsT_ps, wq_sb, (HPC + j) * dk, dk, x)
                proj_fm(ksT_ps, wk_sb, (HPC + j) * dk, dk, x)
                proj_tm(kst_ps, wk_sb, (HPC + j) * dk, dk, x)
                lgv = lgc_sb[:, j * 128:(j + 1) * 128]
            elif kind == "gla":
                proj_fm(pb[7][0:16, 0:128], wz_sb, 0, 16, x)
                P.I("dve", "tensor_copy", out=zaug[0:16, :], in_=pb[7][0:16, 0:128])
                P.mm(pb[7][:, 128:256], zaug[:, :], wga_sb[:, :])
                P.I("act", "activation", out=esb[:, :], in_=pb[7][:, 128:256], func=AF.Exp, scale=-1.0)
                P.I("act", "activation", out=esb[:, :], in_=esb[:, :], func=AF.Ln, bias=1.0)
                P.I("dve", "tensor_scalar", out=lg[:, :], in0=esb[:, :], scalar1=-1.0 / 16.0, scalar2=None,
                    op0=ALU.mult)
                lgv = lg[:, :]
            else:
                P.I("dve", "tensor_copy", out=lg[:, :], in_=lf.v(lf.h[:, j:j + 1].to_broadcast([128, 128])))
                lgv = lg[:, :]
            bT_ps, bt_ps = pb[3][:, 0:128], pb[3][:, 128:256]
            P.mm(bT_ps, lgv, tri_sb[:, :])
            P.mm(bt_ps, tri_sb[:, :], lgv)
            P.I("act", "activation", out=eT[:, :], in_=bT_ps, func=AF.Exp)
            P.I("act", "activation", out=enT[:, :], in_=bT_ps, func=AF.Exp, scale=-1.0)
            P.I("act", "activation", out=ent[:, :], in_=bt_ps, func=AF.Exp, scale=-1.0)
            if kind == "ret":
                P.I("dve", "tensor_tensor", out=tmp[:, :], in0=qsT_ps, in1=tb[1][:, :], op=ALU.mult)
                P.I("dve", "tensor_tensor", out=qr[:, :], in0=qT_ps, in1=tb[0][:, :], op=ALU.mult)
                P.I("pool", "tensor_tensor", out=qr[:, :], in0=qr[:, :], in1=tmp[:, :], op=ALU.add)
                P.I("pool", "tensor_tensor", out=A[:, :], in0=qr[:, :], in1=eT[:, :], op=ALU.mult)
                P.I("dve", "tensor_tensor", out=tmp[:, :], in0=ksT_ps, in1=tb[1][:, :], op=ALU.mult)
                P.I("dve", "tensor_tensor", out=qr[:, :], in0=kT_ps, in1=tb[0][:, :], op=ALU.mult)
                P.I("pool", "tensor_tensor", out=qr[:, :], in0=qr[:, :], in1=tmp[:, :], op=ALU.add)
                P.I("dve", "scalar_tensor_tensor", out=B[:, :], in0=qr[:, :], scalar=cscale, in1=enT[:, :],
                    op0=ALU.mult, op1=ALU.mult)
                P.I("dve", "tensor_tensor", out=tmp[:, :], in0=kst_ps, in1=tb[3][:, :], op=ALU.mult)
                P.I("dve", "tensor_tensor", out=qr[:, :], in0=kt_ps, in1=tb[2][:, :], op=ALU.mult)
                P.I("pool", "tensor_tensor", out=qr[:, :], in0=qr[:, :], in1=tmp[:, :], op=ALU.add)
                P.I("dve", "scalar_tensor_tensor", out=C[:, :], in0=qr[:, :], scalar=cscale, in1=ent[:, :],
                    op0=ALU.mult, op1=ALU.mult)
            else:
                P.I("dve", "tensor_tensor", out=A[0:dk, :], in0=qT_ps, in1=eT[0:dk, :], op=ALU.mult)
                P.I("dve", "scalar_tensor_tensor", out=B[0:dk, :], in0=kT_ps, scalar=cscale, in1=enT[0:dk, :],
                    op0=ALU.mult, op1=ALU.mult)
                P.I("dve", "scalar_tensor_tensor", out=C[:, 0:dk], in0=kt_ps, scalar=cscale, in1=ent[:, 0:dk],
                    op0=ALU.mult, op1=ALU.mult)
            if kind == "ml":
                P.I("dve", "tensor_scalar", out=D[:, 0:dv], in0=vt_ps, scalar1=ei[:, j:j + 1], scalar2=None,
                    op0=ALU.mult)
                P.I("dve", "tensor_copy", out=D[:, dv:dv + 1], in_=ei[:, j:j + 1])
            else:
                P.I("act", "activation", out=D[:, :], in_=vt_ps, func=AF.Copy)
            sT_ps = pb[4][:, 0:128]
            P.mm(sT_ps, B[0:dk, :], A[0:dk, :])
            P.I("dve", "tensor_tensor", out=sT[:, :], in0=sT_ps, in1=tri_sb[:, :], op=ALU.mult)
            o_ps = pb[5][:, 0:dvx]
            P.mm(o_ps, sT[:, :], D[:, :], start=True, stop=False)
            P.mm(o_ps, A[0:dk, :], Sbf[j][0:dk, :], start=False, stop=True)
            U_ps = pb[6][0:dk, 0:dvx]
            P.mm(U_ps, C[:, 0:dk], D[:, :])
            P.I("pool", "tensor_scalar", out=S[j][0:dk, :], in0=S[j][0:dk, :], scalar1=eT[0:dk, 127:128],
                scalar2=None, op0=ALU.mult)
            P.I("dve", "scalar_tensor_tensor", out=S[j][0:dk, :], in0=U_ps, scalar=eT[0:dk, 127:128],
                in1=S[j][0:dk, :], op0=ALU.mult, op1=ALU.add)
            P.I("pool", "tensor_copy", out=Sbf[j][0:dk, :], in_=S[j][0:dk, :])
            if kind == "ml":
                P.I("act", "activation", out=dd[:, :], in_=pb[5][:, dv:dv + 1], func=AF.Abs)
                P.I("dve", "tensor_scalar", out=dd[:, :], in0=dd[:, :], scalar1=1.0, scalar2=None, op0=ALU.max)
                P.I("dve", "reciprocal", out=dd[:, :], in_=dd[:, :])
                P.I("dve", "tensor_scalar", out=hn[:, :], in0=pb[5][:, 0:dv], scalar1=dd[:, 0:1], scalar2=None,
                    op0=ALU.mult)
                P.I("act", "activation", out=gate[:, :], in_=gt_ps, func=AF.Sigmoid)
            else:
                P.I("dve", "tensor_copy", out=hn[:, :], in_=pb[5][:, 0:dv])
                P.I("act", "activation", out=gate[:, :], in_=gt_ps, func=AF.Silu)
            P.I("dve", "bn_stats", out=st6[:, :], in_=hn[:, :])
            P.I("dve", "bn_aggr", out=mv[:, :], in_=st6[:, :])
            P.I("act", "activation", out=rs[:, :], in_=mv[:, 1:2], func=AF.Sqrt, bias=EPS, scale=1.0)
            P.I("dve", "reciprocal", out=rs[:, :], in_=rs[:, :])
            P.I("dve", "tensor_scalar", out=hn[:, :], in0=hn[:, :], scalar1=mv[:, 0:1], scalar2=rs[:, 0:1],
                op0=ALU.subtract, op1=ALU.mult)
            P.I("pool", "tensor_tensor", out=hn[:, :], in0=hn[:, :], in1=ng_sb[:, j * dv:(j + 1) * dv], op=ALU.mult)
            P.I("pool", "tensor_tensor", out=obuf[:, j * dv:(j + 1) * dv], in0=hn[:, :], in1=gate[:, :], op=ALU.mult)
        P.dma("sp", o[tok, :], obuf[:, :])
    return P.emit()


def _c(a):
    return np.ascontiguousarray(a)


def _tri():
    return np.triu(np.ones((128, 128), np.float32))


def l1_in_maps(kind, h, inp, j):
    cfg = L1CFG[kind]
    HPC, dk, dv = cfg["HPC"], cfg["dk"], cfg["dv"]
    maps = []
    for core in range(NCORES):
        b, hb = core // 4, core % 4
        heads = [hb * HPC + i for i in range(HPC)]
        m = {"xT": _c(h[b].T), "tri": _tri()}
        if kind == "ml":
            w = inp["ml_w_in"][j]
            m["wq"] = _c(np.concatenate([w[:, hd * 64:(hd + 1) * 64] for hd in heads], 1))
            m["wk"] = _c(np.concatenate([w[:, 512 + hd * 64:512 + (hd + 1) * 64] for hd in heads], 1))
            m["wv"] = _c(np.concatenate([w[:, 1024 + hd * 128:1024 + (hd + 1) * 128] for hd in heads], 1))
            m["wg"] = _c(np.concatenate([w[:, 2048 + hd * 128:2048 + (hd + 1) * 128] for hd in heads], 1))
            gi = [3072 + hd for hd in heads] + [3080 + hd for hd in heads]
            m["wgt"] = _c(w[:, gi])
            m["bgt"] = _c(inp["ml_b_gates"][j][[hd for hd in heads] + [8 + hd for hd in heads]][None, :])
            m["ng"] = _c(np.concatenate([inp["ml_norm_g"][j][hd * 128:(hd + 1) * 128] for hd in heads])[None, :])
        elif kind == "ret":
            w = inp["ret_w_in"][j]

            def sw(c0):
                return np.concatenate([w[:, c0 + 64:c0 + 128], w[:, c0:c0 + 64]], 1)
            m["wq"] = _c(np.concatenate([w[:, hd * 128:(hd + 1) * 128] for hd in heads] + [sw(hd * 128) for hd in heads], 1))
            m["wk"] = _c(np.concatenate([w[:, 1024 + hd * 128:1024 + (hd + 1) * 128] for hd in heads]
                                        + [sw(1024 + hd * 128) for hd in heads], 1))
            m["wv"] = _c(np.concatenate([w[:, 2048 + hd * 256:2048 + (hd + 1) * 256] for hd in heads], 1))
            m["wg"] = _c(np.concatenate([w[:, 4096 + hd * 256:4096 + (hd + 1) * 256] for hd in heads], 1))
            m["ng"] = _c(np.concatenate([inp["ret_norm_g"][j][hd * 256:(hd + 1) * 256] for hd in heads])[None, :])
            inv = (10000.0 ** (-np.arange(0, 128, 2, dtype=np.float32) / 128.0)).astype(np.float32)
            ang = np.arange(SEQ, dtype=np.float32)[:, None] * inv[None, :]
            cos, sin = np.cos(ang).astype(np.float32), np.sin(ang).astype(np.float32)
            cosk = np.concatenate([cos, cos], 1)
            sink = np.concatenate([-sin, sin], 1)
            m["cosk"], m["sink"] = _c(cosk), _c(sink)
            m["cosT"], m["sinT"] = _c(cosk.T), _c(sink.T)
            lgam = np.log1p(-(2.0 ** (-5.0 - np.arange(8, dtype=np.float32)))).astype(np.float32)
            m["lgc"] = _c(np.concatenate([np.full((128, 128), lgam[hd], np.float32) for hd in heads], 1))
        else:
            w = inp["gla_w_in"][j]
            hd = heads[0]
            m["wq"] = _c(w[:, hd * 128:(hd + 1) * 128])
            m["wk"] = _c(w[:, 512 + hd * 128:512 + (hd + 1) * 128])
            m["wv"] = _c(w[:, 1024 + hd * 256:1024 + (hd + 1) * 256])
            m["wg"] = _c(w[:, 2048 + hd * 256:2048 + (hd + 1) * 256])
            m["wz"] = _c(w[:, 3072:3088])
            wga = np.zeros((128, 128), np.float32)
            wga[0:16] = inp["gla_w_gate"][j][:, hd * 128:(hd + 1) * 128]
            wga[16] = inp["gla_b_gate"][j][hd * 128:(hd + 1) * 128]
            m["wga"] = wga
            m["ng"] = _c(inp["gla_norm_g"][j][hd * 256:(hd + 1) * 256][None, :])
        maps.append(m)
    return maps


def l1_gather(kind, results):
    outs = []
    for b in range(2):
        outs.append(np.concatenate([np.asarray(results[b * 4 + hb]["o"]) for hb in range(4)], 1))
    return np.stack(outs, 0)


def build_s5(T=SEQ):
    nc = bass.Bass("TRN2", target_bir_lowering=False)
    P = Prog(nc)
    IN = dict(kind="ExternalInput")
    NBK = T // 512
    NK = int(np.log2(T))
    xT = P.dram("xT", [1024, SEQ], F32, **IN)
    w_in = P.dram("w_in", [1024, 256], F32, **IN)
    lre = P.dram("lre", [128, 8], F32, **IN)
    lim = P.dram("lim", [128, 8], F32, **IN)
    ldt = P.dram("ldt", [128, 8], F32, **IN)
    bbr = P.dram("bbr", [32, 8, 128], F32, **IN)
    bbi = P.dram("bbi", [32, 8, 128], F32, **IN)
    ccr = P.dram("ccr", [128, 8, 32], F32, **IN)
    cci = P.dram("cci", [128, 8, 32], F32, **IN)
    ddg = P.dram("ddg", [32, 8, 32], F32, **IN)
    yT = P.dram("yT", [256, SEQ], BF16, kind="ExternalOutput")

    w_sb = P.sb("w_sb", [128, 8, 256], BF16)
    P.dma("pool", w_sb[:, :, :], w_in.v(w_in.h.rearrange("(c p) n -> p c n", p=128)))
    bbr_sb = P.sb("bbr_sb", [32, 8, 128], BF16)
    bbi_sb = P.sb("bbi_sb", [32, 8, 128], BF16)
    ddg_sb = P.sb("ddg_sb", [32, 8, 32], BF16)
    P.dma("pool", bbr_sb[:, :, :], bbr[:, :, :])
    P.dma("pool", bbi_sb[:, :, :], bbi[:, :, :])
    P.dma("pool", ddg_sb[:, :, :], ddg[:, :, :])
    ccr_sb = P.sb("ccr_sb", [128, 8, 32], F32)
    cci_sb = P.sb("cci_sb", [128, 8, 32], F32)
    P.dma("sp", ccr_sb[:, :, :], ccr[:, :, :])
    P.dma("sp", cci_sb[:, :, :], cci[:, :, :])
    P.I("dve", "tensor_scalar", out=cci_sb[:, :, :], in0=cci_sb[:, :, :], scalar1=-1.0, scalar2=None, op0=ALU.mult)

    def small(name, n=8):
        return P.sb(name, [128, n], F32)

    lr, li, dt = small("lr"), small("li"), small("dt")
    P.dma("sp", lr[:, :], lre[:, :])
    P.dma("sp", li[:, :], lim[:, :])
    P.dma("sp", dt[:, :], ldt[:, :])
    P.I("act", "activation", out=dt[:, :], in_=dt[:, :], func=AF.Exp)
    rr, th, cs, sn, t1, t2 = small("rr"), small("th"), small("cs"), small("sn"), small("t1"), small("t2")

    def tt(out, a, b, op, eng="dve"):
        P.I(eng, "tensor_tensor", out=out, in0=a, in1=b, op=op)

    tt(rr[:, :], lr[:, :], dt[:, :], ALU.mult)
    P.I("act", "activation", out=rr[:, :], in_=rr[:, :], func=AF.Exp)
    tt(th[:, :], li[:, :], dt[:, :], ALU.mult)
    P.I("act", "activation", out=sn[:, :], in_=th[:, :], func=AF.Sin, scale=1.0 / 16.0)
    hp = small("hp", 1)
    P.I("pool", "memset", ap=hp[:, :], constant=float(np.pi / 2))
    P.I("act", "activation", out=cs[:, :], in_=th[:, :], func=AF.Sin, scale=1.0 / 16.0, bias=hp[:, 0:1])

    def csq(c, s):
        tt(t1[:, :], c, c, ALU.mult)
        tt(t2[:, :], s, s, ALU.mult)
        tt(s, c, s, ALU.mult)
        P.I("dve", "tensor_scalar", out=s, in0=s, scalar1=2.0, scalar2=None, op0=ALU.mult)
        tt(c, t1[:, :], t2[:, :], ALU.subtract)

    for _ in range(4):
        csq(cs[:, :], sn[:, :])
    ar = P.sb("ar", [128, NK, 8], F32)
    ai = P.sb("ai", [128, NK, 8], F32)
    nai = P.sb("nai", [128, NK, 8], F32)
    tt(ar[:, 0, :], rr[:, :], cs[:, :], ALU.mult)
    tt(ai[:, 0, :], rr[:, :], sn[:, :], ALU.mult)
    for k in range(1, NK):
        tt(t1[:, :], ar[:, k - 1, :], ar[:, k - 1, :], ALU.mult)
        tt(t2[:, :], ai[:, k - 1, :], ai[:, k - 1, :], ALU.mult)
        tt(ar[:, k, :], t1[:, :], t2[:, :], ALU.subtract)
        tt(t1[:, :], ar[:, k - 1, :], ai[:, k - 1, :], ALU.mult)
        P.I("dve", "tensor_scalar", out=ai[:, k, :], in0=t1[:, :], scalar1=2.0, scalar2=None, op0=ALU.mult)
    P.I("dve", "tensor_scalar", out=nai[:, :, :], in0=ai[:, :, :], scalar1=-1.0, scalar2=None, op0=ALU.mult)
    cr, ci, nci, m2 = small("cr"), small("ci"), small("nci"), small("m2")
    am1 = small("am1")
    P.I("dve", "tensor_scalar", out=am1[:, :], in0=ar[:, 0, :], scalar1=-1.0, scalar2=None, op0=ALU.add)
    tt(t1[:, :], lr[:, :], lr[:, :], ALU.mult)
    tt(t2[:, :], li[:, :], li[:, :], ALU.mult)
    tt(m2[:, :], t1[:, :], t2[:, :], ALU.add)
    P.I("dve", "reciprocal", out=m2[:, :], in_=m2[:, :])
    tt(t1[:, :], am1[:, :], lr[:, :], ALU.mult)
    tt(t2[:, :], ai[:, 0, :], li[:, :], ALU.mult)
    tt(cr[:, :], t1[:, :], t2[:, :], ALU.add)
    tt(cr[:, :], cr[:, :], m2[:, :], ALU.mult)
    tt(t1[:, :], ai[:, 0, :], lr[:, :], ALU.mult)
    tt(t2[:, :], am1[:, :], li[:, :], ALU.mult)
    tt(ci[:, :], t1[:, :], t2[:, :], ALU.subtract)
    tt(ci[:, :], ci[:, :], m2[:, :], ALU.mult)
    P.I("dve", "tensor_scalar", out=nci[:, :], in0=ci[:, :], scalar1=-1.0, scalar2=None, op0=ALU.mult)

    X = [[P.sb(f"X{a}{b}", [128, T], F32) for b in range(2)] for a in range(2)]
    uT = P.sb("uT", [32, T], BF16)
    xc = [P.sb(f"xc{i}", [128, 8, 512], BF16) for i in range(2)]
    g1 = [P.sb(f"g1_{i}", [32, 512], F32) for i in range(2)]
    g2 = [P.sb(f"g2_{i}", [32, 512], F32) for i in range(2)]
    yo = [P.sb(f"yo{i}", [32, 512], BF16) for i in range(2)]
    pb = [P.ps(f"pb{i}", [128, 512], F32) for i in range(8)]

    it = 0
    for pp in range(8):
        cur = X[0]
        for tb in range(NBK):
            x = xc[it % 2]
            tok = slice(tb * 512, (tb + 1) * 512)
            P.dma("pool", x[:, :, :], xT.v(xT.h[:, tok].rearrange("(c p) t -> p c t", p=128)))
            ups = pb[it % 2][0:32, :]
            for c in range(8):
                P.mm(ups, w_sb[:, c, pp * 32:(pp + 1) * 32], x[:, c, :], start=(c == 0), stop=(c == 7))
            P.I("act", "activation", out=uT[:, tok], in_=ups, func=AF.Copy)
            br_ps, bi_ps = pb[2 + it % 2], pb[4 + it % 2]
            P.mm(br_ps[:, :], bbr_sb[:, pp, :], uT[:, tok])
            P.mm(bi_ps[:, :], bbi_sb[:, pp, :], uT[:, tok])
            P.I("dve", "tensor_scalar", out=cur[0][:, tok], in0=br_ps[:, :], scalar1=cr[:, pp:pp + 1], scalar2=None,
                op0=ALU.mult)
            P.I("dve", "scalar_tensor_tensor", out=cur[0][:, tok], in0=bi_ps[:, :], scalar=nci[:, pp:pp + 1],
                in1=cur[0][:, tok], op0=ALU.mult, op1=ALU.add)
            P.I("dve", "tensor_scalar", out=cur[1][:, tok], in0=bi_ps[:, :], scalar1=cr[:, pp:pp + 1], scalar2=None,
                op0=ALU.mult)
            P.I("dve", "scalar_tensor_tensor", out=cur[1][:, tok], in0=br_ps[:, :], scalar=ci[:, pp:pp + 1],
                in1=cur[1][:, tok], op0=ALU.mult, op1=ALU.add)
            it += 1
        src_i = 0
        for k in range(NK):
            d = 1 << k
            s, o2 = X[src_i], X[1 - src_i]
            a_r, a_i, na_i = ar[:, k, pp:pp + 1], ai[:, k, pp:pp + 1], nai[:, k, pp:pp + 1]
            P.I("act", "activation", out=o2[0][:, 0:d], in_=s[0][:, 0:d], func=AF.Copy)
            P.I("act", "activation", out=o2[1][:, 0:d], in_=s[1][:, 0:d], func=AF.Copy)
            P.I("dve", "scalar_tensor_tensor", out=o2[0][:, d:T], in0=s[0][:, 0:T - d], scalar=a_r, in1=s[0][:, d:T],
                op0=ALU.mult, op1=ALU.add)
            P.I("dve", "scalar_tensor_tensor", out=o2[0][:, d:T], in0=s[1][:, 0:T - d], scalar=na_i, in1=o2[0][:, d:T],
                op0=ALU.mult, op1=ALU.add)
            P.I("dve", "scalar_tensor_tensor", out=o2[1][:, d:T], in0=s[1][:, 0:T - d], scalar=a_r, in1=s[1][:, d:T],
                op0=ALU.mult, op1=ALU.add)
            P.I("dve", "scalar_tensor_tensor", out=o2[1][:, d:T], in0=s[0][:, 0:T - d], scalar=a_i, in1=o2[1][:, d:T],
                op0=ALU.mult, op1=ALU.add)
            src_i = 1 - src_i
        fin = X[src_i]
        for tb in range(NBK):
            tok = slice(tb * 512, (tb + 1) * 512)
            yps = pb[6 + tb % 2][0:32, :]
            P.mm(yps, ccr_sb[:, pp, :], fin[0][:, tok], start=True, stop=False)
            P.mm(yps, cci_sb[:, pp, :], fin[1][:, tok], start=False, stop=True)
            dps = pb[tb % 2][0:32, :]
            P.mm(dps, ddg_sb[:, pp, :], uT[:, tok])
            a1, a2, yb = g1[tb % 2], g2[tb % 2], yo[tb % 2]
            P.I("act", "activation", out=a1[:, :], in_=yps, func=AF.Copy)
            P.I("dve", "tensor_tensor", out=a1[:, :], in0=a1[:, :], in1=dps, op=ALU.add)
            P.I("pool", "tensor_tensor", out=a2[:, :], in0=a1[:, :], in1=a1[:, :], op=ALU.mult)
            P.I("pool", "tensor_scalar", out=a2[:, :], in0=a2[:, :], scalar1=0.044715, scalar2=1.0,
                op0=ALU.mult, op1=ALU.add)
            P.I("pool", "tensor_tensor", out=a2[:, :], in0=a2[:, :], in1=a1[:, :], op=ALU.mult)
            P.I("act", "activation", out=a2[:, :], in_=a2[:, :], func=AF.Tanh, scale=0.7978845608028654)
            P.I("pool", "tensor_scalar", out=a2[:, :], in0=a2[:, :], scalar1=1.0, scalar2=0.5,
                op0=ALU.add, op1=ALU.mult)
            P.I("pool", "tensor_tensor", out=yb[:, :], in0=a2[:, :], in1=a1[:, :], op=ALU.mult)
            P.dma("sp", yT[pp * 32:(pp + 1) * 32, tok], yb[:, :])
    return P.emit()


def s5_in_maps(h, inp, j):
    maps = []
    for core in range(NCORES):
        b, hb = core // 4, core % 4
        g0 = hb * 16
        m = {"xT": _c(h[b].T), "w_in": _c(inp["s5_w_in"][j][:, g0 * 16:(g0 + 16) * 16])}
        lre = inp["s5_lam_re"][j][g0:g0 + 16]
        lim = inp["s5_lam_im"][j][g0:g0 + 16]
        ldt = np.repeat(inp["s5_log_dt"][j][g0:g0 + 16][:, None], 64, 1)

        def lay(a):
            return _c(a.reshape(8, 2, 64).transpose(1, 2, 0).reshape(128, 8))
        m["lre"], m["lim"], m["ldt"] = lay(lre), lay(lim), lay(ldt)
        bre = inp["s5_b_re"][j][g0:g0 + 16]
        bim = inp["s5_b_im"][j][g0:g0 + 16]
        cre = inp["s5_c_re"][j][g0:g0 + 16]
        cim = inp["s5_c_im"][j][g0:g0 + 16]
        dsk = inp["s5_d"][j][g0 * 16:(g0 + 16) * 16]
        bbr = np.zeros((32, 8, 128), np.float32)
        bbi = np.zeros((32, 8, 128), np.float32)
        ccr = np.zeros((128, 8, 32), np.float32)
        cci = np.zeros((128, 8, 32), np.float32)
        ddg = np.zeros((32, 8, 32), np.float32)
        for pp in range(8):
            for g2 in range(2):
                g = pp * 2 + g2
                bbr[g2 * 16:(g2 + 1) * 16, pp, g2 * 64:(g2 + 1) * 64] = bre[g].T
                bbi[g2 * 16:(g2 + 1) * 16, pp, g2 * 64:(g2 + 1) * 64] = bim[g].T
                ccr[g2 * 64:(g2 + 1) * 64, pp, g2 * 16:(g2 + 1) * 16] = cre[g].T
                cci[g2 * 64:(g2 + 1) * 64, pp, g2 * 16:(g2 + 1) * 16] = cim[g].T
            idx = np.arange(32)
            ddg[idx, pp, idx] = dsk[pp * 32:(pp + 1) * 32]
        m.update(bbr=bbr, bbi=bbi, ccr=ccr, cci=cci, ddg=ddg)
        maps.append(m)
    return maps


def s5_gather(results):
    outs = []
    for b in range(2):
        yT = np.concatenate([np.asarray(results[b * 4 + hb]["yT"]) for hb in range(4)], 0)
        outs.append(yT.T)
    return np.stack(outs, 0)


_PROGS = {}


def _prog(key, fn):
    if key not in _PROGS:
        _PROGS[key] = fn()
    return _PROGS[key]


def _run(nc, maps):
    return run_bass_kernel_spmd(nc, maps, core_ids=list(range(NCORES))).results


def _l2_maps(mo, h, inp, layer, w_out, glu_w=None):
    Dm = mo.shape[-1]
    mo_f = mo.reshape(-1, Dm)
    h_f = h.reshape(-1, 1024)
    bgu = inp["moe_b_gate_up"][layer]
    bgu_t = _c(bgu.reshape(32, 16, 128).transpose(2, 0, 1))
    base = {
        "w_out": _c(w_out), "lng": _c(inp["ln_g"][layer]), "lnb": _c(inp["ln_b"][layer]),
        "w_r": _c(inp["moe_w_router"][layer]), "b_r": _c(inp["moe_b_router"][layer][None, :]),
        "w_gu": _c(inp["moe_w_gate_up"][layer]), "b_gu": bgu_t,
        "w_d": _c(inp["moe_w_down"][layer]), "b_d": _c(inp["moe_b_down"][layer]),
        "idn": np.eye(128, dtype=np.float32),
    }
    if glu_w is not None:
        base["w_glu"], base["b_glu"] = _c(glu_w[0]), _c(glu_w[1][None, :])
    maps = []
    for c in range(NCORES):
        sl = slice(c * 2048, (c + 1) * 2048)
        m = dict(base)
        m["mixT"] = _c(mo_f[sl].T)
        m["hin"] = _c(h_f[sl])
        if glu_w is not None:
            m["ytok"] = _c(mo_f[sl])
        maps.append(m)
    return maps


def kernel(**inp):
    inp = {k: np.asarray(v) for k, v in inp.items()}
    h = np.asarray(inp["x"], np.float32)
    for layer in range(4):
        kind = ("ml", "ret", "gla", "s5")[layer % 4]
        j = layer // 4
        glu_w = None
        if kind == "s5":
            nc1 = _prog("s5", build_s5)
            mo = s5_gather(_run(nc1, s5_in_maps(h, inp, j)))
            w_out = inp["s5_w_out"][j]
            glu_w = (inp["s5_w_glu"][j], inp["s5_b_glu"][j])
        else:
            nc1 = _prog(kind, lambda: build_l1(kind))
            mo = l1_gather(kind, _run(nc1, l1_in_maps(kind, h, inp, j)))
            w_out = inp[{"ml": "ml_w_out", "ret": "ret_w_out", "gla": "gla_w_out"}[kind]][j]
        KC = mo.shape[-1] // 128
        nc2 = _prog(("l2", KC, glu_w is not None), lambda: build_l2(KC, glu=glu_w is not None))
        res = _run(nc2, _l2_maps(mo, h, inp, layer, w_out, glu_w))
        h = np.concatenate([np.asarray(r["hout"]) for r in res], 0).reshape(2, SEQ, 1024).astype(np.float32)
    return h
```

```python
import contextlib
import numpy as np
import ml_dtypes
import concourse.bass as bass
import concourse.mybir as mybir
from concourse.bass_utils import run_bass_kernel_spmd

F32 = mybir.dt.float32
BF16 = mybir.dt.bfloat16
AF = mybir.ActivationFunctionType
ALU = mybir.AluOpType
AX = mybir.AxisListType
NPBF = ml_dtypes.bfloat16

NCORES = 8
SB_BASE = 16512
SB_TOP = 229344
GROUPS = [[0, 1, 2, 3], [4, 5, 6, 7]]
ALPHA = 8.0 ** 0.25
EPS = 1e-5


class Tr:
    __slots__ = ("w", "r")

    def __init__(self):
        self.w = None
        self.r = {}


class V:
    __slots__ = ("ap", "trs")

    def __init__(self, ap, trs):
        self.ap = ap
        self.trs = trs


class Buf:
    def __init__(self, handle, trs=None):
        self.h = handle
        self.trs = trs if trs is not None else (Tr(),)

    def __getitem__(self, key):
        return V(self.h[key], self.trs)

    def v(self, ap):
        return V(ap, self.trs)


WRITE_KW = ("out", "accum_out", "ap", "out_ap")


class Prog:
    ENG = ("pe", "dve", "act", "pool", "sp")
    DMAQ = ("sp", "pool", "act")

    def __init__(self, nc, ndma=6):
        self.nc = nc
        self.es = contextlib.ExitStack()
        self.stream = {e: [] for e in self.ENG}
        self.cnt = {e: 0 for e in self.ENG}
        self.sem = {}
        self.epoch = 0
        self.ck = {}
        for e in self.ENG:
            self.ck[e] = "c_" + e
            self.sem["c_" + e] = self.es.enter_context(nc.semaphore("c_" + e))
        self.known = {e: {} for e in self.ENG}
        self.ndma = ndma
        for q in self.DMAQ:
            for i in range(ndma):
                self.sem[f"d_{q}{i}"] = self.es.enter_context(nc.semaphore(f"d_{q}{i}"))
        self.dval = {q: [0] * ndma for q in self.DMAQ}
        self.dnext = {q: 0 for q in self.DMAQ}
        self.sb_off = SB_BASE
        self.dyn = {}
        self.nname = 0
        self.pb = None

    def banks(self):
        if self.pb is None:
            self.pb = [self.ps(f"pb{i}", [128, 512], F32) for i in range(8)]
        return self.pb

    def sb(self, name, shape, dtype):
        nbytes = int(np.prod(shape[1:])) * (4 if dtype == F32 else 2)
        nbytes = (nbytes + 63) // 64 * 64
        off = self.sb_off
        assert off + nbytes <= SB_TOP, f"out of SBUF for {name}: {off}+{nbytes}"
        self.sb_off = off + nbytes
        self.nname += 1
        return Buf(self.nc.alloc_sbuf_tensor_at(f"{name}_{self.nname}", list(shape), dtype, offset=off))

    def sb_reset(self, off=None):
        self.sb_off = SB_BASE if off is None else off

    def barrier(self):
        allv = [(self.ck[e], self.cnt[e]) for e in self.ENG if self.cnt[e] > 0]
        for q in self.DMAQ:
            for i in range(self.ndma):
                if self.dval[q][i] > 0:
                    allv.append((f"d_{q}{i}", self.dval[q][i]))
        if "cc" in self.sem and self.ccval > 0:
            allv.append(("cc", self.ccval))
        for e in self.ENG:
            waits = self._waits(e, [d for d in allv if d[0] != self.ck[e]])
            if waits:
                self.stream[e].append((waits, None, None))

    def new_epoch(self):
        self.epoch += 1
        for e in self.ENG:
            k = f"c_{e}_{self.epoch}"
            self.ck[e] = k
            self.sem[k] = self.es.enter_context(self.nc.semaphore(k))
            self.cnt[e] = 0

    def ps(self, name, shape, dtype=F32):
        return Buf(self.nc.alloc_psum_tensor(name, list(shape), dtype))

    def dram(self, name, shape, dtype, kind="Internal"):
        return Buf(self.nc.dram_tensor(name, list(shape), dtype, kind=kind))

    def _deps(self, reads, writes):
        deps = []
        for t in reads:
            if t.w is not None:
                deps.append(t.w)
        for t in writes:
            if t.w is not None:
                deps.append(t.w)
            deps.extend(t.r.items())
        return deps

    def _waits(self, eng, deps, skip_self=False):
        kn = self.known[eng]
        need = {}
        own = self.ck[eng]
        for key, val in deps:
            if skip_self and key == own:
                continue
            if kn.get(key, 0) >= val:
                continue
            if need.get(key, 0) < val:
                need[key] = val
        for key, val in need.items():
            kn[key] = val
        return [(self.sem[k], v) for k, v in need.items()]

    def _mark(self, tok, reads, writes):
        for t in reads:
            if t.r.get(tok[0], 0) < tok[1]:
                t.r[tok[0]] = tok[1]
        for t in writes:
            t.w = tok
            t.r = {}

    def op(self, eng, fn, reads=(), writes=(), skip_self=False):
        deps = self._deps(reads, writes)
        waits = self._waits(eng, deps, skip_self)
        self.cnt[eng] += 1
        tok = (self.ck[eng], self.cnt[eng])
        self.stream[eng].append((waits, fn, (self.sem[tok[0]], 1)))
        self._mark(tok, reads, writes)
        return tok

    def I(self, eng, name, **kw):
        reads, writes, args = [], [], {}
        for k, v in kw.items():
            if isinstance(v, V):
                args[k] = v.ap
                (writes if k in WRITE_KW else reads).extend(v.trs)
            else:
                args[k] = v
        return self.op(eng, lambda e: getattr(e, name)(**args), reads, writes)

    def mm(self, out, lhsT, rhs, start=True, stop=True):
        o, l, r = out.ap, lhsT.ap, rhs.ap
        return self.op("pe", lambda e: e.matmul(o, l, r, start=start, stop=stop),
                       list(lhsT.trs) + list(rhs.trs), list(out.trs), skip_self=True)

    def tp(self, out, in_, ident):
        o, i, d = out.ap, in_.ap, ident.ap
        return self.op("pe", lambda e: e.transpose(o, i, d),
                       list(in_.trs) + list(ident.trs), list(out.trs), skip_self=True)

    def dma(self, q, out, in_, **kw):
        reads, writes = list(in_.trs), list(out.trs)
        deps = self._deps(reads, writes)
        i = self.dnext[q]
        self.dnext[q] = (i + 1) % self.ndma
        key = f"d_{q}{i}"
        prev = self.dval[q][i]
        if prev > 0:
            deps.append((key, prev))
        self.dval[q][i] = prev + 16
        tok = (key, prev + 16)
        waits = self._waits(q, deps)
        o, a = out.ap, in_.ap

        def fn(e):
            return e.dma_start(out=(o(e) if callable(o) else o), in_=(a(e) if callable(a) else a), **kw)

        self.stream[q].append((waits, fn, (self.sem[key], 16)))
        self._mark(tok, reads, writes)
        return tok

    def coll(self, kind, out, in_, groups):
        q = "pool"
        if "cc" not in self.sem:
            self.sem["cc"] = self.es.enter_context(self.nc.semaphore("cc"))
            self.ccval = 0
        reads, writes = list(in_.trs), list(out.trs)
        deps = self._deps(reads, writes)
        if self.ccval > 0:
            deps.append(("cc", self.ccval))
        self.ccval += 1
        tok = ("cc", self.ccval)
        waits = self._waits(q, deps)
        o, a = out.ap, in_.ap
        self.stream[q].append((waits, lambda e: e.collective_compute(kind, ALU.bypass, groups, [a], [o]),
                               (self.sem["cc"], 1)))
        self._mark(tok, reads, writes)
        return tok

    def emit(self):
        waits = []
        for q in self.DMAQ:
            for i in range(self.ndma):
                if self.dval[q][i] > 0:
                    waits.append((self.sem[f"d_{q}{i}"], self.dval[q][i]))
        for e in self.ENG:
            if e != "sp" and self.cnt[e] > 0:
                waits.append((self.sem[self.ck[e]], self.cnt[e]))
        if "cc" in self.sem and self.ccval > 0:
            waits.append((self.sem["cc"], self.ccval))
        self.stream["sp"].append((waits, None, None))
        with self.nc.Block() as block:
            decos = {"pe": block.tensor, "dve": block.vector, "act": block.scalar,
                     "pool": block.gpsimd, "sp": block.sync}
            for e in self.ENG:
                items = self.stream[e]
                if not items:
                    continue

                def body(engine, items=items):
                    for waits, fn, inc in items:
                        for sem, val in waits:
                            engine.wait_ge(sem, val)
                        if fn is not None:
                            fn(engine).then_inc(inc[0], inc[1])

                decos[e](body)
        self.es.close()
        return self.nc


def layer_norm_tile(P, src, dst, gt, bt, st, mv, rs, eng2="pool"):
    for i in range(2):
        P.I("dve", "bn_stats", out=st[:, i, :], in_=src(slice(i * 512, (i + 1) * 512)))
    P.I("dve", "bn_aggr", out=mv[:, :], in_=st[:, :, :])
    P.I("act", "activation", out=rs[:, :], in_=mv[:, 1:2], func=AF.Sqrt, bias=EPS, scale=1.0)
    P.I("dve", "reciprocal", out=rs[:, :], in_=rs[:, :])
    full = slice(0, 1024)
    P.I("dve", "tensor_scalar", out=dst(full), in0=src(full), scalar1=mv[:, 0:1], scalar2=rs[:, 0:1],
        op0=ALU.subtract, op1=ALU.mult)
    P.I(eng2, "tensor_tensor", out=dst(full), in0=dst(full), in1=gt[:, :], op=ALU.mult)
    P.I(eng2, "tensor_tensor", out=dst(full), in0=dst(full), in1=bt[:, :], op=ALU.add)


NT = 16
NE = 32
NQ = 4
NB = 4


def stage_l2(P, D, layer, KC, oT_all, hin, hout, hT_loc, n_exp=NE, glu=False):
    nc = P.nc
    dbg = 0
    Dm = KC * 128
    IN = dict(kind="ExternalInput")
    L = f"_{layer}"
    w_out = D("w_out" + L, [Dm, 1024], F32)
    lng = D("lng" + L, [2, 1024], F32)
    lnb = D("lnb" + L, [2, 1024], F32)
    w_r = D("w_r" + L, [1024, 32], F32)
    b_r = D("b_r" + L, [1, 32], F32)
    w_gu = D("w_gu" + L, [max(n_exp, 1), 1024, 2048], F32)
    b_gu = D("b_gu" + L, [128, NE, 16], F32)
    w_d = D("w_d" + L, [max(n_exp, 1), 1024, 1024], F32)
    b_d = D("b_d" + L, [NE, 1024], F32)
    idn = D("idn", [128, 128], F32)
    if glu:
        w_glu = D("w_glu" + L, [1024, 1024], F32)
        b_glu = D("b_glu" + L, [128, 8], F32)

    acc = P.sb("acc", [128, NT, 1024], F32)
    xT = P.sb("xT", [128, 8, 2048], BF16)
    G = P.sb("G", [128, NT, 32], F32)
    PIECE = 8 * 512 + 2 * 1024
    slot_tr = [Tr(), Tr(), Tr()]
    arena_h = P.sb("arena", [128, 3 * PIECE], BF16).h
    wout_sb = Buf(arena_h, tuple(slot_tr))
    slots = [Buf(arena_h, (slot_tr[i],)) for i in range(3)]
    idt = P.sb("idt", [128, 128], F32)
    gt = P.sb("gt", [128, 1024], F32)
    bt = P.sb("bt", [128, 1024], F32)
    wr_sb = P.sb("wr_sb", [128, 8, 32], F32)
    br_sb = P.sb("br_sb", [128, 32], F32)
    bgu_sb = P.sb("bgu_sb", [128, NE, 16], F32)
    bd_sb = P.sb("bd_sb", [128, 1024], F32)
    Gpad = P.sb("Gpad", [128, 128], F32)
    mix_sb = [P.sb(f"mix_sb{i}", [128, KC, 128], BF16) for i in range(2)]
    hin_sb = [P.sb(f"hin_sb{i}", [128, 1024], F32) for i in range(1)]
    rbuf = [P.sb(f"rbuf{i}", [128, 1024], F32) for i in range(1)]
    hm = [P.sb(f"hm{i}", [128, 1024], F32) for i in range(2)]
    hT32 = P.sb("hT32", [128, 8, 128], F32)
    st = P.sb("st", [128, 2, 6], F32)
    mv = P.sb("mv", [128, 2], F32)
    rs = P.sb("rs", [128, 1], F32)
    lg = P.sb("lg", [128, 32], F32)
    m8 = P.sb("m8", [128, 8], F32)
    msk = P.sb("msk", [128, 32], F32)
    nmx = P.sb("nmx", [128, 1], F32)
    ex = P.sb("ex", [128, 32], F32)
    den = P.sb("den", [128, 1], F32)
    GT = P.sb("GT", [128, 128], F32)
    gbuf = [P.sb(f"gbuf{i}", [128, 512], F32) for i in range(2)]
    sgbuf = [P.sb(f"sgbuf{i}", [128, 512], F32) for i in range(2)]
    tbuf = [P.sb(f"tbuf{i}", [128, 512], F32) for i in range(2)]
    actT = [P.sb(f"actT{i}", [128, 2, 512], BF16) for i in range(2)]
    ev = [P.sb(f"ev{i}", [128, 512], F32) for i in range(2)]
    pb = P.banks()

    P.dma("sp", idt[:, :], idn[:, :])
    P.dma("sp", gt[:, :], lng.v(lng.h[0:1, :].partition_broadcast(128)))
    P.dma("sp", bt[:, :], lnb.v(lnb.h[0:1, :].partition_broadcast(128)))
    P.dma("sp", wr_sb[:, :, :], w_r.v(w_r.h.rearrange("(c p) n -> p c n", p=128)))
    P.dma("sp", br_sb[:, :], b_r.v(b_r.h[0:1, :].partition_broadcast(128)))
    P.dma("sp", bgu_sb[:, :, :], b_gu[:, :, :])
    P.I("pool", "memset", ap=bd_sb[:, :], constant=0.0)
    P.I("pool", "memset", ap=Gpad[:, :], constant=0.0)
    P.dma("sp", bd_sb[0:32, :], b_d[:, :])
    wout_v = wout_sb.v(arena_h[:, 0:KC * 1024].rearrange("p (c n) -> p c n", c=KC))
    for c0 in range(0, KC, 4):
        P.dma("pool", wout_sb.v(arena_h[:, c0 * 1024:(c0 + 4) * 1024].rearrange("p (c n) -> p c n", c=4)),
              w_out.v(w_out.h[c0 * 128:(c0 + 4) * 128, :].rearrange("(c p) n -> p c n", p=128)))
    if glu:
        wglu_v = wout_sb.v(arena_h[:, 8192:16384].rearrange("p (c n) -> p c n", c=8))
        for c0 in range(0, 8, 4):
            P.dma("pool", wout_sb.v(arena_h[:, 8192 + c0 * 1024:8192 + (c0 + 4) * 1024].rearrange("p (c n) -> p c n", c=4)),
                  w_glu.v(w_glu.h[c0 * 128:(c0 + 4) * 128, :].rearrange("(c p) n -> p c n", p=128)))
        bglu_sb = P.sb("bglu_sb", [128, 8], F32)
        P.dma("sp", bglu_sb[:, :], b_glu[:, :])
        sgl = P.sb("sgl", [128, 128], F32)
        gms = [P.sb(f"gms{i}", [128, 8, 128], BF16) for i in range(1)]
    P.I("dve", "tensor_scalar", out=bgu_sb[:, :, 8:16], in0=bgu_sb[:, :, 8:16], scalar1=1.0, scalar2=None,
        op0=ALU.add)

    for i in range(NT):
        ms, hs, rb, hmt = mix_sb[i % 2], hin_sb[0], rbuf[0], hm[i % 2]
        tok = slice(i * 128, (i + 1) * 128)
        def mix_src(e, i=i):
            if "hb" not in P.dyn:
                P.dyn["hb"] = e.snap(e.partition_id() % 4, min_val=0, max_val=3)
            key = ("off", i // 4)
            if key not in P.dyn:
                P.dyn[key] = e.snap((P.dyn["hb"] * 4 + i // 4) * 2048, min_val=0, max_val=15 * 2048)
            return oT_all.h[bass.ds(P.dyn[key], Dm), (i % 4) * 128:(i % 4 + 1) * 128] \
                .rearrange("(c p) t -> p c t", p=128)

        P.dma("sp", ms[:, :, :], oT_all.v(mix_src))
        P.dma("sp", hs[:, :], hin[tok, :])
        if glu:
            gm = gms[0]
            for n in range(8):
                zps = pb[6 + n % 2][:, 0:128]
                for c in range(8):
                    P.mm(zps, wout_sb.v(wglu_v.ap[:, c, n * 128:(n + 1) * 128]), ms[:, c, :],
                         start=(c == 0), stop=(c == 7))
                P.I("act", "activation", out=sgl[:, :], in_=zps, func=AF.Sigmoid, bias=bglu_sb[:, n:n + 1], scale=1.0)
                P.I("dve", "tensor_tensor", out=gm[:, n, :], in0=sgl[:, :], in1=ms[:, n, :], op=ALU.mult)
            ms = gm
        for nh in range(2):
            for c in range(KC):
                P.mm(pb[nh][:, :], ms[:, c, :], wout_sb.v(wout_v.ap[:, c, nh * 512:(nh + 1) * 512]),
                     start=(c == 0), stop=(c == KC - 1))
            P.I("dve", "scalar_tensor_tensor", out=rb[:, nh * 512:(nh + 1) * 512],
                in0=hs[:, nh * 512:(nh + 1) * 512], scalar=ALPHA, in1=pb[nh][:, :],
                op0=ALU.mult, op1=ALU.add)
        layer_norm_tile(P, lambda s: rb[:, s], lambda s: hmt[:, s], gt, bt, st, mv, rs)
        if dbg == 1:
            P.dma("sp", hout[i * 128:(i + 1) * 128, :], hmt[:, :])
            continue
        P.I("act", "activation", out=acc[:, i, :], in_=hmt[:, :], func=AF.Copy, scale=ALPHA)
        for c in range(8):
            P.tp(pb[2 + c // 4][:, (c % 4) * 128:(c % 4 + 1) * 128], hmt[:, c * 128:(c + 1) * 128], idt[:, :])
        for half in range(2):
            src = pb[2 + half].v(pb[2 + half].h[:, :].rearrange("p (c n) -> p c n", c=4))
            P.I("act", "activation", out=hT32[:, half * 4:(half + 1) * 4, :], in_=src, func=AF.Copy)
            P.I("dve", "tensor_copy", out=xT[:, half * 4:(half + 1) * 4, tok], in_=hT32[:, half * 4:(half + 1) * 4, :])
        if dbg == 2:
            continue
        for c in range(8):
            P.mm(pb[4][:, 0:32], hT32[:, c, :], wr_sb[:, c, :], start=(c == 0), stop=(c == 7))
        P.I("dve", "tensor_tensor", out=lg[:, :], in0=pb[4][:, 0:32], in1=br_sb[:, :], op=ALU.add)
        P.I("dve", "max", out=m8[:, :], in_=lg[:, :])
        P.I("dve", "tensor_scalar", out=msk[:, :], in0=lg[:, :], scalar1=m8[:, 3:4], scalar2=None, op0=ALU.is_ge)
        P.I("dve", "tensor_scalar", out=nmx[:, :], in0=m8[:, 0:1], scalar1=-1.0, scalar2=None, op0=ALU.mult)
        P.I("act", "activation", out=ex[:, :], in_=lg[:, :], func=AF.Exp, bias=nmx[:, 0:1], scale=1.0)
        P.I("dve", "tensor_tensor", out=ex[:, :], in0=ex[:, :], in1=msk[:, :], op=ALU.mult)
        P.I("dve", "reduce_sum", out=den[:, :], in_=ex[:, :], axis=AX.X)
        P.I("dve", "reciprocal", out=den[:, :], in_=den[:, :])
        P.I("dve", "tensor_scalar", out=G[:, i, :], in0=ex[:, :], scalar1=den[:, 0:1], scalar2=None, op0=ALU.mult)
        if dbg == 3:
            continue
        P.I("dve", "tensor_copy", out=Gpad[:, 0:32], in_=G[:, i, :])
        P.tp(pb[5][:, 0:128], Gpad[:, :], idt[:, :])
        P.I("dve", "tensor_copy", out=GT[:, :], in_=pb[5][:, 0:128])
        for nh in range(2):
            P.mm(pb[6 + nh][:, :], GT[:, :], bd_sb[:, nh * 512:(nh + 1) * 512])
            P.I("dve", "tensor_tensor", out=acc[:, i, nh * 512:(nh + 1) * 512],
                in0=acc[:, i, nh * 512:(nh + 1) * 512], in1=pb[6 + nh][:, :], op=ALU.add)

    P.dma("sp", gt[:, :], lng.v(lng.h[1:2, :].partition_broadcast(128)))
    P.dma("sp", bt[:, :], lnb.v(lnb.h[1:2, :].partition_broadcast(128)))

    pieces = [(e, q) for e in range(n_exp) for q in range(NQ)]

    def load_piece(k):
        e, q = pieces[k]
        sl = slots[k % 3]
        gu_v = sl.v(arena_h[:, (k % 3) * PIECE:(k % 3) * PIECE + 4096].rearrange("p (c n) -> p c n", c=8))
        d_v = sl.v(arena_h[:, (k % 3) * PIECE + 4096:(k % 3 + 1) * PIECE].rearrange("p (c n) -> p c n", c=2))
        P.dma("pool", sl.v(gu_v.ap[:, :, 0:256]),
              w_gu.v(w_gu.h[e, :, q * 256:(q + 1) * 256].rearrange("(c p) n -> p c n", p=128)))
        P.dma("pool", sl.v(gu_v.ap[:, :, 256:512]),
              w_gu.v(w_gu.h[e, :, 1024 + q * 256:1024 + (q + 1) * 256].rearrange("(c p) n -> p c n", p=128)))
        P.dma("pool", d_v, w_d.v(w_d.h[e, q * 256:(q + 1) * 256, :].rearrange("(c p) n -> p c n", p=128)))
        return gu_v, d_v

    views = {}
    for k0 in range(min(2, len(pieces))):
        views[k0] = load_piece(k0)
    it = 0
    evi = 0
    for k, (e, q) in enumerate(pieces):
        if k + 2 < len(pieces):
            views[k + 2] = load_piece(k + 2)
        gu_v, d_v = views.pop(k)
        sl = slots[k % 3]
        for b in range(NB):
            at = actT[it % 2]
            tokb = slice(b * 512, (b + 1) * 512)
            for jj in range(2):
                gps, ups = pb[(it * 2 + jj) % 2], pb[2 + (it * 2 + jj) % 2]
                gb, sgb, tb = gbuf[jj], sgbuf[jj], tbuf[jj]
                for c in range(8):
                    P.mm(gps[:, :], sl.v(gu_v.ap[:, c, jj * 128:(jj + 1) * 128]), xT[:, c, tokb],
                         start=(c == 0), stop=(c == 7))
                for c in range(8):
                    P.mm(ups[:, :], sl.v(gu_v.ap[:, c, 256 + jj * 128:256 + (jj + 1) * 128]), xT[:, c, tokb],
                         start=(c == 0), stop=(c == 7))
                ch = q * 2 + jj
                P.I("dve", "tensor_scalar", out=gb[:, :], in0=gps[:, :], scalar1=bgu_sb[:, e, ch:ch + 1],
                    scalar2=7.0, op0=ALU.add, op1=ALU.min)
                P.I("act", "activation", out=sgb[:, :], in_=gb[:, :], func=AF.Sigmoid, scale=1.702)
                P.I("pool", "tensor_tensor", out=sgb[:, :], in0=sgb[:, :], in1=gb[:, :], op=ALU.mult)
                P.I("dve", "tensor_scalar", out=tb[:, :], in0=ups[:, :], scalar1=bgu_sb[:, e, 8 + ch:9 + ch],
                    scalar2=8.0, op0=ALU.add, op1=ALU.min)
                P.I("dve", "scalar_tensor_tensor", out=at[:, jj, :], in0=tb[:, :], scalar=-6.0, in1=sgb[:, :],
                    op0=ALU.max, op1=ALU.mult)
            for tt in range(4):
                ti = b * 4 + tt
                for nh in range(2):
                    yps = pb[4 + evi % 4]
                    evb = ev[evi % 2]
                    evi += 1
                    for jj in range(2):
                        P.mm(yps[:, :], at[:, jj, tt * 128:(tt + 1) * 128], sl.v(d_v.ap[:, jj, nh * 512:(nh + 1) * 512]),
                             start=(jj == 0), stop=(jj == 1))
                    P.I("act", "activation", out=evb[:, :], in_=yps[:, :], func=AF.Copy, scale=G[:, ti, e:e + 1])
                    P.I("pool", "tensor_tensor", out=acc[:, ti, nh * 512:(nh + 1) * 512],
                        in0=acc[:, ti, nh * 512:(nh + 1) * 512], in1=evb[:, :], op=ALU.add)
            it += 1

    for i in range(NT):
        ob = hm[i % 2]
        layer_norm_tile(P, lambda s: acc[:, i, s], lambda s: ob[:, s], gt, bt, st, mv, rs)
        P.dma("sp", hout[i * 128:(i + 1) * 128, :], ob[:, :])
        if hT_loc is not None:
            for c in range(8):
                P.tp(pb[2 + c // 4][:, (c % 4) * 128:(c % 4 + 1) * 128], ob[:, c * 128:(c + 1) * 128], idt[:, :])
            hb16 = mix_sb[i % 2]
            for half in range(2):
                src = pb[2 + half].v(pb[2 + half].h[:, :].rearrange("p (c n) -> p c n", c=4))
                P.I("act", "activation", out=hb16[:, half * 4:(half + 1) * 4, :], in_=src, func=AF.Copy)
            P.dma("sp", hT_loc.v(hT_loc.h[(i // 2) * 1024:(i // 2 + 1) * 1024, (i % 2) * 128:(i % 2 + 1) * 128]
                                 .rearrange("(c p) t -> p c t", p=128)), hb16[:, 0:8, :])


L1CFG = {"ml": dict(HPC=2, dk=64, dv=128), "ret": dict(HPC=2, dk=128, dv=256), "gla": dict(HPC=1, dk=128, dv=256)}
SEQ = 8192
NCH = SEQ // 128


def stage_l1(P, D, kind, xsrc, oT_loc, nch=NCH):
    cfg = L1CFG[kind]
    HPC, dk, dv = cfg["HPC"], cfg["dk"], cfg["dv"]
    dvx = dv + 1 if kind == "ml" else dv
    cscale = float(dk) ** -0.5
    nc = P.nc
    K_ = kind + "_"
    nq = 2 * HPC * dk if kind == "ret" else HPC * dk
    wq = D(K_ + "wq", [1024, nq], F32)
    wk = D(K_ + "wk", [1024, nq], F32)
    wv = D(K_ + "wv", [1024, HPC * dv], F32)
    wg = D(K_ + "wg", [1024, HPC * dv], F32)
    ng = D(K_ + "ng", [1, HPC * dv], F32)
    tri = D("tri", [128, 128], F32)
    idn = D("idn", [128, 128], F32)
    if kind == "ret":
        cosT = D("cosT", [128, SEQ], F32)
        sinT = D("sinT", [128, SEQ], F32)
        cosk = D("cosk", [SEQ, 128], F32)
        sink = D("sink", [SEQ, 128], F32)
        lgc = D("lgc", [128, HPC * 128], F32)
    if kind == "gla":
        wz = D(K_ + "wz", [1024, 16], F32)
        wga = D(K_ + "wga", [128, 128], F32)
    if kind == "ml":
        wgt = D(K_ + "wgt", [1024, 2 * HPC], F32)
        bgt = D(K_ + "bgt", [1, 2 * HPC], F32)

    idt = P.sb("idt", [128, 128], F32)
    P.dma("sp", idt[:, :], idn[:, :])
    wq_sb = P.sb("wq_sb", [128, 8, nq], BF16)
    wk_sb = P.sb("wk_sb", [128, 8, nq], BF16)
    wv_sb = P.sb("wv_sb", [128, 8, HPC * dv], BF16)
    wg_sb = P.sb("wg_sb", [128, 8, HPC * dv], BF16)
    ng_sb = P.sb("ng_sb", [128, HPC * dv], F32)
    tri_sb = P.sb("tri_sb", [128, 128], F32)
    for dst, src in ((wq_sb, wq), (wk_sb, wk), (wv_sb, wv), (wg_sb, wg)):
        P.dma("pool", dst[:, :, :], src.v(src.h.rearrange("(c p) n -> p c n", p=128)))
    P.dma("sp", ng_sb[:, :], ng.v(ng.h[0:1, :].partition_broadcast(128)))
    P.dma("sp", tri_sb[:, :], tri[:, :])
    lg = P.sb("lg", [128, 128], F32)
    if kind == "ret":
        lgc_sb = P.sb("lgc_sb", [128, HPC * 128], F32)
        P.dma("sp", lgc_sb[:, :], lgc[:, :])
        tabs = [[P.sb(f"tab{i}_{j}", [128, 128], F32) for j in range(4)] for i in range(2)]
    if kind == "gla":
        wz_sb = P.sb("wz_sb", [128, 8, 16], BF16)
        P.dma("pool", wz_sb[:, :, :], wz.v(wz.h.rearrange("(c p) n -> p c n", p=128)))
        wga_sb = P.sb("wga_sb", [128, 128], F32)
        P.dma("sp", wga_sb[:, :], wga[:, :])
        zaug = P.sb("zaug", [128, 128], F32)
        P.I("pool", "memset", ap=zaug[:, :], constant=1.0)
        esb = P.sb("esb", [128, 128], F32)
    if kind == "ml":
        wgt_sb = P.sb("wgt_sb", [128, 8, 2 * HPC], BF16)
        P.dma("pool", wgt_sb[:, :, :], wgt.v(wgt.h.rearrange("(c p) n -> p c n", p=128)))
        bgt_sb = P.sb("bgt_sb", [128, 2 * HPC], F32)
        P.dma("sp", bgt_sb[:, :], bgt.v(bgt.h[0:1, :].partition_broadcast(128)))
        gsb = P.sb("gsb", [128, 2 * HPC], F32)
        lf = P.sb("lf", [128, HPC], F32)
        ei = P.sb("ei", [128, HPC], F32)
        dd = P.sb("dd", [128, 1], F32)
    xc = [P.sb(f"xc{i}", [128, 8, 128], BF16) for i in range(2)]
    S = [P.sb(f"S{j}", [128, dvx], F32) for j in range(HPC)]
    Sbf = [P.sb(f"Sbf{j}", [128, dvx], BF16) for j in range(HPC)]
    for j in range(HPC):
        P.I("pool", "memset", ap=S[j][:, :], constant=0.0)
        P.I("pool", "memset", ap=Sbf[j][:, :], constant=0.0)
    eT = P.sb("eT", [128, 128], F32)
    enT = P.sb("enT", [128, 128], F32)
    ent = P.sb("ent", [128, 128], F32)
    tmp = P.sb("tmp", [128, 128], F32)
    qr = P.sb("qr", [128, 128], F32)
    A = P.sb("A", [128, 128], BF16)
    B = P.sb("B", [128, 128], BF16)
    C = P.sb("C", [128, 128], BF16)
    D = P.sb("D", [128, dvx], BF16)
    sT = P.sb("sT", [128, 128], BF16)
    hn = P.sb("hn", [128, dv], F32)
    gate = P.sb("gate", [128, dv], F32)
    st6 = P.sb("st6", [128, 6], F32)
    mv = P.sb("mv", [128, 2], F32)
    rs = P.sb("rs", [128, 1], F32)
    ob = [P.sb(f"ob{i}", [128, HPC * dv], F32) for i in range(2)]
    NBLK = HPC * dv // 128
    oT_sb = [P.sb(f"oT_sb{i}", [128, NBLK, 128], BF16) for i in range(2)]
    pb = P.banks()

    def proj_fm(dst, w_sb, col0, ncol, x):
        for c in range(8):
            P.mm(dst, w_sb[:, c, col0:col0 + ncol], x[:, c, :], start=(c == 0), stop=(c == 7))

    def proj_tm(dst, w_sb, col0, ncol, x):
        for c in range(8):
            P.mm(dst, x[:, c, :], w_sb[:, c, col0:col0 + ncol], start=(c == 0), stop=(c == 7))

    for ci in range(nch):
        x = xc[ci % 2]
        tok = slice(ci * 128, (ci + 1) * 128)
        xq, xv = xsrc(ci)
        P.dma(xq, x[:, :, :], xv)
        if kind == "ret":
            tb = tabs[ci % 2]
            P.dma("sp", tb[0][:, :], cosT[:, tok])
            P.dma("sp", tb[1][:, :], sinT[:, tok])
            P.dma("sp", tb[2][:, :], cosk[tok, :])
            P.dma("sp", tb[3][:, :], sink[tok, :])
        if kind == "ml":
            proj_tm(pb[7][:, 0:2 * HPC], wgt_sb, 0, 2 * HPC, x)
            P.I("dve", "tensor_tensor", out=gsb[:, :], in0=pb[7][:, 0:2 * HPC], in1=bgt_sb[:, :], op=ALU.add)
            P.I("act", "activation", out=ei[:, :], in_=gsb[:, 0:HPC], func=AF.Exp)
            P.I("act", "activation", out=lf[:, :], in_=gsb[:, HPC:2 * HPC], func=AF.Exp, scale=-1.0)
            P.I("act", "activation", out=lf[:, :], in_=lf[:, :], func=AF.Ln, bias=1.0)
            P.I("dve", "tensor_scalar", out=lf[:, :], in0=lf[:, :], scalar1=-1.0, scalar2=None, op0=ALU.mult)
        obuf = ob[ci % 2]
        for j in range(HPC):
            qT_ps, kT_ps = pb[0][0:dk, 0:128], pb[0][0:dk, 128:256]
            kt_ps = pb[1][:, 0:dk]
            vt_ps, gt_ps = pb[2][:, 0:dv], pb[2][:, 256:256 + dv]
            proj_fm(qT_ps, wq_sb, j * dk, dk, x)
            proj_fm(kT_ps, wk_sb, j * dk, dk, x)
            proj_tm(kt_ps, wk_sb, j * dk, dk, x)
            proj_tm(vt_ps, wv_sb, j * dv, dv, x)
            proj_tm(gt_ps, wg_sb, j * dv, dv, x)
            if kind == "ret":
                qsT_ps, ksT_ps = pb[0][0:dk, 256:384], pb[0][0:dk, 384:512]
                kst_ps = pb[1][:, 128:256]
                proj_fm(qsT_ps, wq_sb, (HPC + j) * dk, dk, x)
                proj_fm(ksT_ps, wk_sb, (HPC + j) * dk, dk, x)
                proj_tm(kst_ps, wk_sb, (HPC + j) * dk, dk, x)
                lgv = lgc_sb[:, j * 128:(j + 1) * 128]
            elif kind == "gla":
                proj_fm(pb[7][0:16, 0:128], wz_sb, 0, 16, x)
                P.I("dve", "tensor_copy", out=zaug[0:16, :], in_=pb[7][0:16, 0:128])
                P.mm(pb[7][:, 128:256], zaug[:, :], wga_sb[:, :])
                P.I("act", "activation", out=esb[:, :], in_=pb[7][:, 128:256], func=AF.Exp, scale=-1.0)
                P.I("act", "activation", out=esb[:, :], in_=esb[:, :], func=AF.Ln, bias=1.0)
                P.I("dve", "tensor_scalar", out=lg[:, :], in0=esb[:, :], scalar1=-1.0 / 16.0, scalar2=None,
                    op0=ALU.mult)
                lgv = lg[:, :]
            else:
                P.I("dve", "tensor_copy", out=lg[:, :], in_=lf.v(lf.h[:, j:j + 1].to_broadcast([128, 128])))
                lgv = lg[:, :]
            bT_ps, bt_ps = pb[3][:, 0:128], pb[3][:, 128:256]
            P.mm(bT_ps, lgv, tri_sb[:, :])
            P.mm(bt_ps, tri_sb[:, :], lgv)
            P.I("act", "activation", out=eT[:, :], in_=bT_ps, func=AF.Exp)
            P.I("act", "activation", out=enT[:, :], in_=bT_ps, func=AF.Exp, scale=-1.0)
            P.I("act", "activation", out=ent[:, :], in_=bt_ps, func=AF.Exp, scale=-1.0)
            if kind == "ret":
                P.I("dve", "tensor_tensor", out=tmp[:, :], in0=qsT_ps, in1=tb[1][:, :], op=ALU.mult)
                P.I("dve", "tensor_tensor", out=qr[:, :], in0=qT_ps, in1=tb[0][:, :], op=ALU.mult)
                P.I("pool", "tensor_tensor", out=qr[:, :], in0=qr[:, :], in1=tmp[:, :], op=ALU.add)
                P.I("pool", "tensor_tensor", out=A[:, :], in0=qr[:, :], in1=eT[:, :], op=ALU.mult)
                P.I("dve", "tensor_tensor", out=tmp[:, :], in0=ksT_ps, in1=tb[1][:, :], op=ALU.mult)
                P.I("dve", "tensor_tensor", out=qr[:, :], in0=kT_ps, in1=tb[0][:, :], op=ALU.mult)
                P.I("pool", "tensor_tensor", out=qr[:, :], in0=qr[:, :], in1=tmp[:, :], op=ALU.add)
                P.I("dve", "scalar_tensor_tensor", out=B[:, :], in0=qr[:, :], scalar=cscale, in1=enT[:, :],
                    op0=ALU.mult, op1=ALU.mult)
                P.I("dve", "tensor_tensor", out=tmp[:, :], in0=kst_ps, in1=tb[3][:, :], op=ALU.mult)
                P.I("dve", "tensor_tensor", out=qr[:, :], in0=kt_ps, in1=tb[2][:, :], op=ALU.mult)
                P.I("pool", "tensor_tensor", out=qr[:, :], in0=qr[:, :], in1=tmp[:, :], op=ALU.add)
                P.I("dve", "scalar_tensor_tensor", out=C[:, :], in0=qr[:, :], scalar=cscale, in1=ent[:, :],
                    op0=ALU.mult, op1=ALU.mult)
            else:
                P.I("dve", "tensor_tensor", out=A[0:dk, :], in0=qT_ps, in1=eT[0:dk, :], op=ALU.mult)
                P.I("dve", "scalar_tensor_tensor", out=B[0:dk, :], in0=kT_ps, scalar=cscale, in1=enT[0:dk, :],
                    op0=ALU.mult, op1=ALU.mult)
                P.I("dve", "scalar_tensor_tensor", out=C[:, 0:dk], in0=kt_ps, scalar=cscale, in1=ent[:, 0:dk],
                    op0=ALU.mult, op1=ALU.mult)
            if kind == "ml":
                P.I("dve", "tensor_scalar", out=D[:, 0:dv], in0=vt_ps, scalar1=ei[:, j:j + 1], scalar2=None,
                    op0=ALU.mult)
                P.I("dve", "tensor_copy", out=D[:, dv:dv + 1], in_=ei[:, j:j + 1])
            else:
                P.I("act", "activation", out=D[:, :], in_=vt_ps, func=AF.Copy)
            sT_ps = pb[4][:, 0:128]
            P.mm(sT_ps, B[0:dk, :], A[0:dk, :])
            P.I("dve", "tensor_tensor", out=sT[:, :], in0=sT_ps, in1=tri_sb[:, :], op=ALU.mult)
            o_ps = pb[5][:, 0:dvx]
            P.mm(o_ps, sT[:, :], D[:, :], start=True, stop=False)
            P.mm(o_ps, A[0:dk, :], Sbf[j][0:dk, :], start=False, stop=True)
            U_ps = pb[6][0:dk, 0:dvx]
            P.mm(U_ps, C[:, 0:dk], D[:, :])
            P.I("pool", "tensor_scalar", out=S[j][0:dk, :], in0=S[j][0:dk, :], scalar1=eT[0:dk, 127:128],
                scalar2=None, op0=ALU.mult)
            P.I("dve", "scalar_tensor_tensor", out=S[j][0:dk, :], in0=U_ps, scalar=eT[0:dk, 127:128],
                in1=S[j][0:dk, :], op0=ALU.mult, op1=ALU.add)
            P.I("pool", "tensor_copy", out=Sbf[j][0:dk, :], in_=S[j][0:dk, :])
            if kind == "ml":
                P.I("act", "activation", out=dd[:, :], in_=pb[5][:, dv:dv + 1], func=AF.Abs)
                P.I("dve", "tensor_scalar", out=dd[:, :], in0=dd[:, :], scalar1=1.0, scalar2=None, op0=ALU.max)
                P.I("dve", "reciprocal", out=dd[:, :], in_=dd[:, :])
                P.I("dve", "tensor_scalar", out=hn[:, :], in0=pb[5][:, 0:dv], scalar1=dd[:, 0:1], scalar2=None,
                    op0=ALU.mult)
                P.I("act", "activation", out=gate[:, :], in_=gt_ps, func=AF.Sigmoid)
            else:
                P.I("dve", "tensor_copy", out=hn[:, :], in_=pb[5][:, 0:dv])
                P.I("act", "activation", out=gate[:, :], in_=gt_ps, func=AF.Silu)
            P.I("dve", "bn_stats", out=st6[:, :], in_=hn[:, :])
            P.I("dve", "bn_aggr", out=mv[:, :], in_=st6[:, :])
            P.I("act", "activation", out=rs[:, :], in_=mv[:, 1:2], func=AF.Sqrt, bias=EPS, scale=1.0)
            P.I("dve", "reciprocal", out=rs[:, :], in_=rs[:, :])
            P.I("dve", "tensor_scalar", out=hn[:, :], in0=hn[:, :], scalar1=mv[:, 0:1], scalar2=rs[:, 0:1],
                op0=ALU.subtract, op1=ALU.mult)
            P.I("pool", "tensor_tensor", out=hn[:, :], in0=hn[:, :], in1=ng_sb[:, j * dv:(j + 1) * dv], op=ALU.mult)
            P.I("pool", "tensor_tensor", out=obuf[:, j * dv:(j + 1) * dv], in0=hn[:, :], in1=gate[:, :], op=ALU.mult)
        otb = oT_sb[ci % 2]
        for blk in range(NBLK):
            P.tp(pb[7][:, blk * 128:(blk + 1) * 128], obuf[:, blk * 128:(blk + 1) * 128], idt[:, :])
        P.I("act", "activation", out=otb[:, :, :],
            in_=pb[7].v(pb[7].h[:, 0:NBLK * 128].rearrange("p (c n) -> p c n", c=NBLK)), func=AF.Copy)
        R_ = HPC * dv
        P.dma("sp", oT_loc.v(oT_loc.h[(ci // 4) * R_:(ci // 4 + 1) * R_, (ci % 4) * 128:(ci % 4 + 1) * 128]
                             .rearrange("(c p) t -> p c t", p=128)), otb[:, :, :])


def _c(a):
    return np.ascontiguousarray(a)


def _tri():
    return np.triu(np.ones((128, 128), np.float32))


def l1_in_maps(kind, h, inp, j):
    cfg = L1CFG[kind]
    HPC, dk, dv = cfg["HPC"], cfg["dk"], cfg["dv"]
    maps = []
    for core in range(NCORES):
        b, hb = core // 4, core % 4
        heads = [hb * HPC + i for i in range(HPC)]
        m = {"xT": _c(h[b].T), "tri": _tri()}
        if kind == "ml":
            w = inp["ml_w_in"][j]
            m["wq"] = _c(np.concatenate([w[:, hd * 64:(hd + 1) * 64] for hd in heads], 1))
            m["wk"] = _c(np.concatenate([w[:, 512 + hd * 64:512 + (hd + 1) * 64] for hd in heads], 1))
            m["wv"] = _c(np.concatenate([w[:, 1024 + hd * 128:1024 + (hd + 1) * 128] for hd in heads], 1))
            m["wg"] = _c(np.concatenate([w[:, 2048 + hd * 128:2048 + (hd + 1) * 128] for hd in heads], 1))
            gi = [3072 + hd for hd in heads] + [3080 + hd for hd in heads]
            m["wgt"] = _c(w[:, gi])
            m["bgt"] = _c(inp["ml_b_gates"][j][[hd for hd in heads] + [8 + hd for hd in heads]][None, :])
            m["ng"] = _c(np.concatenate([inp["ml_norm_g"][j][hd * 128:(hd + 1) * 128] for hd in heads])[None, :])
        elif kind == "ret":
            w = inp["ret_w_in"][j]

            def sw(c0):
                return np.concatenate([w[:, c0 + 64:c0 + 128], w[:, c0:c0 + 64]], 1)
            m["wq"] = _c(np.concatenate([w[:, hd * 128:(hd + 1) * 128] for hd in heads] + [sw(hd * 128) for hd in heads], 1))
            m["wk"] = _c(np.concatenate([w[:, 1024 + hd * 128:1024 + (hd + 1) * 128] for hd in heads]
                                        + [sw(1024 + hd * 128) for hd in heads], 1))
            m["wv"] = _c(np.concatenate([w[:, 2048 + hd * 256:2048 + (hd + 1) * 256] for hd in heads], 1))
            m["wg"] = _c(np.concatenate([w[:, 4096 + hd * 256:4096 + (hd + 1) * 256] for hd in heads], 1))
            m["ng"] = _c(np.concatenate([inp["ret_norm_g"][j][hd * 256:(hd + 1) * 256] for hd in heads])[None, :])
            inv = (10000.0 ** (-np.arange(0, 128, 2, dtype=np.float32) / 128.0)).astype(np.float32)
            ang = np.arange(SEQ, dtype=np.float32)[:, None] * inv[None, :]
            cos, sin = np.cos(ang).astype(np.float32), np.sin(ang).astype(np.float32)
            cosk = np.concatenate([cos, cos], 1)
            sink = np.concatenate([-sin, sin], 1)
            m["cosk"], m["sink"] = _c(cosk), _c(sink)
            m["cosT"], m["sinT"] = _c(cosk.T), _c(sink.T)
            lgam = np.log1p(-(2.0 ** (-5.0 - np.arange(8, dtype=np.float32)))).astype(np.float32)
            m["lgc"] = _c(np.concatenate([np.full((128, 128), lgam[hd], np.float32) for hd in heads], 1))
        else:
            w = inp["gla_w_in"][j]
            hd = heads[0]
            m["wq"] = _c(w[:, hd * 128:(hd + 1) * 128])
            m["wk"] = _c(w[:, 512 + hd * 128:512 + (hd + 1) * 128])
            m["wv"] = _c(w[:, 1024 + hd * 256:1024 + (hd + 1) * 256])
            m["wg"] = _c(w[:, 2048 + hd * 256:2048 + (hd + 1) * 256])
            m["wz"] = _c(w[:, 3072:3088])
            wga = np.zeros((128, 128), np.float32)
            wga[0:16] = inp["gla_w_gate"][j][:, hd * 128:(hd + 1) * 128]
            wga[16] = inp["gla_b_gate"][j][hd * 128:(hd + 1) * 128]
            m["wga"] = wga
            m["ng"] = _c(inp["gla_norm_g"][j][hd * 256:(hd + 1) * 256][None, :])
        maps.append(m)
    return maps


def l1_gather(kind, results):
    outs = []
    for b in range(2):
        outs.append(np.concatenate([np.asarray(results[b * 4 + hb]["o"]) for hb in range(4)], 1))
    return np.stack(outs, 0)


def stage_s5(P, D, xsrc, yT, T=SEQ):
    nc = P.nc
    NBK = T // 512
    NK = int(np.log2(T))
    w_in = D("s5_w_in", [1024, 256], F32)
    lre = D("s5_lre", [128, 8], F32)
    lim = D("s5_lim", [128, 8], F32)
    ldt = D("s5_ldt", [128, 8], F32)
    bbr = D("s5_bbr", [32, 8, 128], F32)
    bbi = D("s5_bbi", [32, 8, 128], F32)
    ccr = D("s5_ccr", [128, 8, 32], F32)
    cci = D("s5_cci", [128, 8, 32], F32)
    ddg = D("s5_ddg", [32, 8, 32], F32)

    w_sb = P.sb("w_sb", [128, 8, 256], BF16)
    P.dma("pool", w_sb[:, :, :], w_in.v(w_in.h.rearrange("(c p) n -> p c n", p=128)))
    bbr_sb = P.sb("bbr_sb", [32, 8, 128], BF16)
    bbi_sb = P.sb("bbi_sb", [32, 8, 128], BF16)
    ddg_sb = P.sb("ddg_sb", [32, 8, 32], BF16)
    P.dma("pool", bbr_sb[:, :, :], bbr[:, :, :])
    P.dma("pool", bbi_sb[:, :, :], bbi[:, :, :])
    P.dma("pool", ddg_sb[:, :, :], ddg[:, :, :])
    ccr_sb = P.sb("ccr_sb", [128, 8, 32], F32)
    cci_sb = P.sb("cci_sb", [128, 8, 32], F32)
    P.dma("sp", ccr_sb[:, :, :], ccr[:, :, :])
    P.dma("sp", cci_sb[:, :, :], cci[:, :, :])
    P.I("dve", "tensor_scalar", out=cci_sb[:, :, :], in0=cci_sb[:, :, :], scalar1=-1.0, scalar2=None, op0=ALU.mult)

    def small(name, n=8):
        return P.sb(name, [128, n], F32)

    lr, li, dt = small("lr"), small("li"), small("dt")
    P.dma("sp", lr[:, :], lre[:, :])
    P.dma("sp", li[:, :], lim[:, :])
    P.dma("sp", dt[:, :], ldt[:, :])
    P.I("act", "activation", out=dt[:, :], in_=dt[:, :], func=AF.Exp)
    rr, th, cs, sn, t1, t2 = small("rr"), small("th"), small("cs"), small("sn"), small("t1"), small("t2")

    def tt(out, a, b, op, eng="dve"):
        P.I(eng, "tensor_tensor", out=out, in0=a, in1=b, op=op)

    tt(rr[:, :], lr[:, :], dt[:, :], ALU.mult)
    P.I("act", "activation", out=rr[:, :], in_=rr[:, :], func=AF.Exp)
    tt(th[:, :], li[:, :], dt[:, :], ALU.mult)
    P.I("act", "activation", out=sn[:, :], in_=th[:, :], func=AF.Sin, scale=1.0 / 16.0)
    hp = small("hp", 1)
    P.I("pool", "memset", ap=hp[:, :], constant=float(np.pi / 2))
    P.I("act", "activation", out=cs[:, :], in_=th[:, :], func=AF.Sin, scale=1.0 / 16.0, bias=hp[:, 0:1])

    def csq(c, s):
        tt(t1[:, :], c, c, ALU.mult)
        tt(t2[:, :], s, s, ALU.mult)
        tt(s, c, s, ALU.mult)
        P.I("dve", "tensor_scalar", out=s, in0=s, scalar1=2.0, scalar2=None, op0=ALU.mult)
        tt(c, t1[:, :], t2[:, :], ALU.subtract)

    for _ in range(4):
        csq(cs[:, :], sn[:, :])
    ar = P.sb("ar", [128, NK, 8], F32)
    ai = P.sb("ai", [128, NK, 8], F32)
    nai = P.sb("nai", [128, NK, 8], F32)
    tt(ar[:, 0, :], rr[:, :], cs[:, :], ALU.mult)
    tt(ai[:, 0, :], rr[:, :], sn[:, :], ALU.mult)
    for k in range(1, NK):
        tt(t1[:, :], ar[:, k - 1, :], ar[:, k - 1, :], ALU.mult)
        tt(t2[:, :], ai[:, k - 1, :], ai[:, k - 1, :], ALU.mult)
        tt(ar[:, k, :], t1[:, :], t2[:, :], ALU.subtract)
        tt(t1[:, :], ar[:, k - 1, :], ai[:, k - 1, :], ALU.mult)
        P.I("dve", "tensor_scalar", out=ai[:, k, :], in0=t1[:, :], scalar1=2.0, scalar2=None, op0=ALU.mult)
    P.I("dve", "tensor_scalar", out=nai[:, :, :], in0=ai[:, :, :], scalar1=-1.0, scalar2=None, op0=ALU.mult)
    cr, ci, nci, m2 = small("cr"), small("ci"), small("nci"), small("m2")
    am1 = small("am1")
    P.I("dve", "tensor_scalar", out=am1[:, :], in0=ar[:, 0, :], scalar1=-1.0, scalar2=None, op0=ALU.add)
    tt(t1[:, :], lr[:, :], lr[:, :], ALU.mult)
    tt(t2[:, :], li[:, :], li[:, :], ALU.mult)
    tt(m2[:, :], t1[:, :], t2[:, :], ALU.add)
    P.I("dve", "reciprocal", out=m2[:, :], in_=m2[:, :])
    tt(t1[:, :], am1[:, :], lr[:, :], ALU.mult)
    tt(t2[:, :], ai[:, 0, :], li[:, :], ALU.mult)
    tt(cr[:, :], t1[:, :], t2[:, :], ALU.add)
    tt(cr[:, :], cr[:, :], m2[:, :], ALU.mult)
    tt(t1[:, :], ai[:, 0, :], lr[:, :], ALU.mult)
    tt(t2[:, :], am1[:, :], li[:, :], ALU.mult)
    tt(ci[:, :], t1[:, :], t2[:, :], ALU.subtract)
    tt(ci[:, :], ci[:, :], m2[:, :], ALU.mult)
    P.I("dve", "tensor_scalar", out=nci[:, :], in0=ci[:, :], scalar1=-1.0, scalar2=None, op0=ALU.mult)

    X = [[P.sb(f"X{a}{b}", [128, T], F32) for b in range(2)] for a in range(2)]
    uT = P.sb("uT", [32, T], BF16)
    xc = [P.sb(f"xc{i}", [128, 8, 512], BF16) for i in range(2)]
    g1 = [P.sb(f"g1_{i}", [32, 512], F32) for i in range(2)]
    g2 = [P.sb(f"g2_{i}", [32, 512], F32) for i in range(2)]
    yo = [P.sb(f"yo{i}", [32, 512], BF16) for i in range(2)]
    pb = P.banks()

    it = 0
    for pp in range(8):
        cur = X[0]
        for tb in range(NBK):
            x = xc[it % 2]
            tok = slice(tb * 512, (tb + 1) * 512)
            xsrc(tb, x)
            ups = pb[it % 2][0:32, :]
            for c in range(8):
                P.mm(ups, w_sb[:, c, pp * 32:(pp + 1) * 32], x[:, c, :], start=(c == 0), stop=(c == 7))
            P.I("act", "activation", out=uT[:, tok], in_=ups, func=AF.Copy)
            br_ps, bi_ps = pb[2 + it % 2], pb[4 + it % 2]
            P.mm(br_ps[:, :], bbr_sb[:, pp, :], uT[:, tok])
            P.mm(bi_ps[:, :], bbi_sb[:, pp, :], uT[:, tok])
            P.I("dve", "tensor_scalar", out=cur[0][:, tok], in0=br_ps[:, :], scalar1=cr[:, pp:pp + 1], scalar2=None,
                op0=ALU.mult)
            P.I("dve", "scalar_tensor_tensor", out=cur[0][:, tok], in0=bi_ps[:, :], scalar=nci[:, pp:pp + 1],
                in1=cur[0][:, tok], op0=ALU.mult, op1=ALU.add)
            P.I("dve", "tensor_scalar", out=cur[1][:, tok], in0=bi_ps[:, :], scalar1=cr[:, pp:pp + 1], scalar2=None,
                op0=ALU.mult)
            P.I("dve", "scalar_tensor_tensor", out=cur[1][:, tok], in0=br_ps[:, :], scalar=ci[:, pp:pp + 1],
                in1=cur[1][:, tok], op0=ALU.mult, op1=ALU.add)
            it += 1
        src_i = 0
        for k in range(NK):
            d = 1 << k
            s, o2 = X[src_i], X[1 - src_i]
            a_r, a_i, na_i = ar[:, k, pp:pp + 1], ai[:, k, pp:pp + 1], nai[:, k, pp:pp + 1]
            P.I("act", "activation", out=o2[0][:, 0:d], in_=s[0][:, 0:d], func=AF.Copy)
            P.I("act", "activation", out=o2[1][:, 0:d], in_=s[1][:, 0:d], func=AF.Copy)
            P.I("dve", "scalar_tensor_tensor", out=o2[0][:, d:T], in0=s[0][:, 0:T - d], scalar=a_r, in1=s[0][:, d:T],
                op0=ALU.mult, op1=ALU.add)
            P.I("dve", "scalar_tensor_tensor", out=o2[0][:, d:T], in0=s[1][:, 0:T - d], scalar=na_i, in1=o2[0][:, d:T],
                op0=ALU.mult, op1=ALU.add)
            P.I("dve", "scalar_tensor_tensor", out=o2[1][:, d:T], in0=s[1][:, 0:T - d], scalar=a_r, in1=s[1][:, d:T],
                op0=ALU.mult, op1=ALU.add)
            P.I("dve", "scalar_tensor_tensor", out=o2[1][:, d:T], in0=s[0][:, 0:T - d], scalar=a_i, in1=o2[1][:, d:T],
                op0=ALU.mult, op1=ALU.add)
            src_i = 1 - src_i
        fin = X[src_i]
        for tb in range(NBK):
            tok = slice(tb * 512, (tb + 1) * 512)
            yps = pb[6 + tb % 2][0:32, :]
            P.mm(yps, ccr_sb[:, pp, :], fin[0][:, tok], start=True, stop=False)
            P.mm(yps, cci_sb[:, pp, :], fin[1][:, tok], start=False, stop=True)
            dps = pb[tb % 2][0:32, :]
            P.mm(dps, ddg_sb[:, pp, :], uT[:, tok])
            a1, a2, yb = g1[tb % 2], g2[tb % 2], yo[tb % 2]
            P.I("act", "activation", out=a1[:, :], in_=yps, func=AF.Copy)
            P.I("dve", "tensor_tensor", out=a1[:, :], in0=a1[:, :], in1=dps, op=ALU.add)
            P.I("pool", "tensor_tensor", out=a2[:, :], in0=a1[:, :], in1=a1[:, :], op=ALU.mult)
            P.I("pool", "tensor_scalar", out=a2[:, :], in0=a2[:, :], scalar1=0.044715, scalar2=1.0,
                op0=ALU.mult, op1=ALU.add)
            P.I("pool", "tensor_tensor", out=a2[:, :], in0=a2[:, :], in1=a1[:, :], op=ALU.mult)
            P.I("act", "activation", out=a2[:, :], in_=a2[:, :], func=AF.Tanh, scale=0.7978845608028654)
            P.I("pool", "tensor_scalar", out=a2[:, :], in0=a2[:, :], scalar1=1.0, scalar2=0.5,
                op0=ALU.add, op1=ALU.mult)
            P.I("pool", "tensor_tensor", out=yb[:, :], in0=a2[:, :], in1=a1[:, :], op=ALU.mult)
            P.dma("sp", yT[tb * 256 + pp * 32:tb * 256 + (pp + 1) * 32, :], yb[:, :])


def s5_in_maps(h, inp, j):
    maps = []
    for core in range(NCORES):
        b, hb = core // 4, core % 4
        g0 = hb * 16
        m = {"xT": _c(h[b].T), "w_in": _c(inp["s5_w_in"][j][:, g0 * 16:(g0 + 16) * 16])}
        lre = inp["s5_lam_re"][j][g0:g0 + 16]
        lim = inp["s5_lam_im"][j][g0:g0 + 16]
        ldt = np.repeat(inp["s5_log_dt"][j][g0:g0 + 16][:, None], 64, 1)

        def lay(a):
            return _c(a.reshape(8, 2, 64).transpose(1, 2, 0).reshape(128, 8))
        m["lre"], m["lim"], m["ldt"] = lay(lre), lay(lim), lay(ldt)
        bre = inp["s5_b_re"][j][g0:g0 + 16]
        bim = inp["s5_b_im"][j][g0:g0 + 16]
        cre = inp["s5_c_re"][j][g0:g0 + 16]
        cim = inp["s5_c_im"][j][g0:g0 + 16]
        dsk = inp["s5_d"][j][g0 * 16:(g0 + 16) * 16]
        bbr = np.zeros((32, 8, 128), np.float32)
        bbi = np.zeros((32, 8, 128), np.float32)
        ccr = np.zeros((128, 8, 32), np.float32)
        cci = np.zeros((128, 8, 32), np.float32)
        ddg = np.zeros((32, 8, 32), np.float32)
        for pp in range(8):
            for g2 in range(2):
                g = pp * 2 + g2
                bbr[g2 * 16:(g2 + 1) * 16, pp, g2 * 64:(g2 + 1) * 64] = bre[g].T
                bbi[g2 * 16:(g2 + 1) * 16, pp, g2 * 64:(g2 + 1) * 64] = bim[g].T
                ccr[g2 * 64:(g2 + 1) * 64, pp, g2 * 16:(g2 + 1) * 16] = cre[g].T
                cci[g2 * 64:(g2 + 1) * 64, pp, g2 * 16:(g2 + 1) * 16] = cim[g].T
            idx = np.arange(32)
            ddg[idx, pp, idx] = dsk[pp * 32:(pp + 1) * 32]
        m.update(bbr=bbr, bbi=bbi, ccr=ccr, cci=cci, ddg=ddg)
        maps.append(m)
    return maps


def s5_gather(results):
    outs = []
    for b in range(2):
        yT = np.concatenate([np.asarray(results[b * 4 + hb]["yT"]) for hb in range(4)], 0)
        outs.append(yT.T)
    return np.stack(outs, 0)


KINDS = ("ml", "ret", "gla", "s5")


def build_fused(n_exp=NE, nlayers=4, stop=0):
    nc = bass.Bass("TRN2", target_bir_lowering=False)
    P = Prog(nc)
    ext = {}

    def D(name, shape, dtype):
        if name not in ext:
            ext[name] = P.dram(name, shape, dtype, kind="ExternalInput")
        return ext[name]

    xT0 = D("xT0", [1024, SEQ], F32)
    hin0 = D("hin0", [2048, 1024], F32)
    hout = P.dram("hout", [2048, 1024], F32, kind="ExternalOutput")
    h_loc = [P.dram(f"h_loc{i}", [2048, 1024], F32) for i in range(2)]
    hT_loc = P.dram("hT_loc", [8 * 1024, 256], BF16)
    hT_all = P.dram("hT_all", [8 * 4096, 256], BF16)

    def hT_src(t0, n):
        r, tl = t0 // 2048, t0 % 2048
        k, col = tl // 256, tl % 256
        return hT_all.v(hT_all.h[k * 4096 + r * 1024:k * 4096 + (r + 1) * 1024, col:col + n]
                        .rearrange("(c p) t -> p c t", p=128))

    for layer in range(nlayers):
        kind = KINDS[layer % 4]
        rows = 256 if kind == "s5" else L1CFG[kind]["HPC"] * L1CFG[kind]["dv"]
        oT_loc = P.dram(f"oT_loc{layer}", [16 * rows, 512], BF16)
        oT_all = P.dram(f"oT_all{layer}", [16 * 2048, 512], BF16)
        P.sb_reset()
        if kind == "s5":
            def xsrc(tb, x):
                for hh in range(2):
                    P.dma("sp", x[:, :, hh * 256:(hh + 1) * 256], hT_src(tb * 512 + hh * 256, 256))
            stage_s5(P, D, xsrc, oT_loc)
        else:
            if layer == 0:
                def xsrc(ci):
                    return "pool", xT0.v(xT0.h[:, ci * 128:(ci + 1) * 128].rearrange("(c p) t -> p c t", p=128))
            else:
                def xsrc(ci):
                    return "sp", hT_src(ci * 128, 128)
            stage_l1(P, D, kind, xsrc, oT_loc)
        if stop == 10 * layer + 1:
            break
        for k in range(16):
            P.coll("AllGather", oT_all.v(oT_all.h[k * 2048:k * 2048 + 4 * rows, :]),
                   oT_loc.v(oT_loc.h[k * rows:(k + 1) * rows, :]), GROUPS)
        P.barrier()
        P.new_epoch()
        if stop == 10 * layer + 2:
            break
        P.sb_reset()
        last = layer == nlayers - 1
        hin = hin0 if layer == 0 else h_loc[(layer - 1) % 2]
        ho = hout if last else h_loc[layer % 2]
        stage_l2(P, D, layer, 4 * rows // 128, oT_all, hin, ho, None if last else hT_loc, n_exp=n_exp,
                 glu=(kind == "s5"))
        if stop == 10 * layer + 3:
            break
        if not last:
            for k in range(8):
                P.coll("AllGather", hT_all.v(hT_all.h[k * 4096:(k + 1) * 4096, :]),
                       hT_loc.v(hT_loc.h[k * 1024:(k + 1) * 1024, :]), GROUPS)
        P.barrier()
        P.new_epoch()
        if stop == 10 * layer + 4:
            break
    P.emit()
    return nc, list(ext.keys())


def fused_in_maps(inp, names, n_exp=NE):
    x = np.asarray(inp["x"], np.float32)
    xf = x.reshape(-1, 1024)
    shared = {"tri": _tri(), "idn": np.eye(128, dtype=np.float32)}
    for layer in range(4):
        L = f"_{layer}"
        kind = KINDS[layer % 4]
        j = layer // 4
        shared["w_out" + L] = _c(inp[{"ml": "ml_w_out", "ret": "ret_w_out", "gla": "gla_w_out", "s5": "s5_w_out"}[kind]][j])
        shared["lng" + L] = _c(inp["ln_g"][layer])
        shared["lnb" + L] = _c(inp["ln_b"][layer])
        shared["w_r" + L] = _c(inp["moe_w_router"][layer])
        shared["b_r" + L] = _c(inp["moe_b_router"][layer][None, :])
        shared["w_gu" + L] = _c(inp["moe_w_gate_up"][layer][:max(n_exp, 1)])
        shared["b_gu" + L] = _c(inp["moe_b_gate_up"][layer].reshape(32, 16, 128).transpose(2, 0, 1))
        shared["w_d" + L] = _c(inp["moe_w_down"][layer][:max(n_exp, 1)])
        shared["b_d" + L] = _c(inp["moe_b_down"][layer])
        if kind == "s5":
            shared["w_glu" + L] = _c(inp["s5_w_glu"][j])
            shared["b_glu" + L] = _c(inp["s5_b_glu"][j].reshape(8, 128).T)
    per_kind = {k: l1_in_maps(k, x, inp, 0) for k in ("ml", "ret", "gla")}
    s5m = s5_in_maps(x, inp, 0)
    maps = []
    for c in range(NCORES):
        b = c // 4
        m = dict(shared)
        m["xT0"] = _c(x[b].T)
        m["hin0"] = _c(xf[c * 2048:(c + 1) * 2048])
        for k in ("ml", "ret", "gla"):
            for key, val in per_kind[k][c].items():
                if key in ("xT", "tri"):
                    continue
                m[key if key in ("cosT", "sinT", "cosk", "sink", "lgc") else k + "_" + key] = val
        for key, val in s5m[c].items():
            if key != "xT":
                m["s5_" + key] = val
        maps.append({k: m[k] for k in names})
    return maps


_FUSED = {}


def kernel(**inp):
    inp = {k: np.asarray(v) for k, v in inp.items()}
    if "p" not in _FUSED:
        _FUSED["p"] = build_fused()
    nc, names = _FUSED["p"]
    res = run_bass_kernel_spmd(nc, fused_in_maps(inp, names), core_ids=list(range(NCORES))).results
    out = np.concatenate([np.asarray(r["hout"]) for r in res], 0).reshape(2, SEQ, 1024)
    return out.astype(np.float32)
```

```python
import contextlib
import numpy as np
import ml_dtypes
import concourse.bass as bass
import concourse.mybir as mybir
from concourse.bass_utils import run_bass_kernel_spmd

F32 = mybir.dt.float32
BF16 = mybir.dt.bfloat16
AF = mybir.ActivationFunctionType
ALU = mybir.AluOpType
AX = mybir.AxisListType
NPBF = ml_dtypes.bfloat16

NCORES = 8
SB_BASE = 16512
SB_TOP = 229344
GROUPS = [[0, 1, 2, 3], [4, 5, 6, 7]]
ALPHA = 8.0 ** 0.25
EPS = 1e-5


class Tr:
    __slots__ = ("w", "r")

    def __init__(self):
        self.w = None
        self.r = {}


class V:
    __slots__ = ("ap", "trs")

    def __init__(self, ap, trs):
        self.ap = ap
        self.trs = trs


class Buf:
    def __init__(self, handle, trs=None):
        self.h = handle
        self.trs = trs if trs is not None else (Tr(),)

    def __getitem__(self, key):
        return V(self.h[key], self.trs)

    def v(self, ap):
        return V(ap, self.trs)


WRITE_KW = ("out", "accum_out", "ap", "out_ap")


class Prog:
    ENG = ("pe", "dve", "act", "pool", "sp")
    DMAQ = ("sp", "pool", "act")

    def __init__(self, nc, ndma=6):
        self.nc = nc
        self.es = contextlib.ExitStack()
        self.stream = {e: [] for e in self.ENG}
        self.cnt = {e: 0 for e in self.ENG}
        self.sem = {}
        self.epoch = 0
        self.ck = {}
        for e in self.ENG:
            self.ck[e] = "c_" + e
            self.sem["c_" + e] = self.es.enter_context(nc.semaphore("c_" + e))
        self.known = {e: {} for e in self.ENG}
        self.ndma = ndma
        for q in self.DMAQ:
            for i in range(ndma):
                self.sem[f"d_{q}{i}"] = self.es.enter_context(nc.semaphore(f"d_{q}{i}"))
        self.dval = {q: [0] * ndma for q in self.DMAQ}
        self.dnext = {q: 0 for q in self.DMAQ}
        self.sb_off = SB_BASE
        self.dyn = {}
        self.nname = 0
        self.pb = None

    def banks(self):
        if self.pb is None:
            self.pb = [self.ps(f"pb{i}", [128, 512], F32) for i in range(8)]
        return self.pb

    def sb(self, name, shape, dtype):
        nbytes = int(np.prod(shape[1:])) * (4 if dtype == F32 else 2)
        nbytes = (nbytes + 63) // 64 * 64
        off = self.sb_off
        assert off + nbytes <= SB_TOP, f"out of SBUF for {name}: {off}+{nbytes}"
        self.sb_off = off + nbytes
        self.nname += 1
        return Buf(self.nc.alloc_sbuf_tensor_at(f"{name}_{self.nname}", list(shape), dtype, offset=off))

    def sb_reset(self, off=None):
        self.sb_off = SB_BASE if off is None else off

    def barrier(self):
        allv = [(self.ck[e], self.cnt[e]) for e in self.ENG if self.cnt[e] > 0]
        for q in self.DMAQ:
            for i in range(self.ndma):
                if self.dval[q][i] > 0:
                    allv.append((f"d_{q}{i}", self.dval[q][i]))
        if "cc" in self.sem and self.ccval > 0:
            allv.append(("cc", self.ccval))
        for e in self.ENG:
            waits = self._waits(e, [d for d in allv if d[0] != self.ck[e]])
            if waits:
                self.stream[e].append((waits, None, None))

    def new_epoch(self):
        self.epoch += 1
        for e in self.ENG:
            k = f"c_{e}_{self.epoch}"
            self.ck[e] = k
            self.sem[k] = self.es.enter_context(self.nc.semaphore(k))
            self.cnt[e] = 0

    def ps(self, name, shape, dtype=F32):
        return Buf(self.nc.alloc_psum_tensor(name, list(shape), dtype))

    def dram(self, name, shape, dtype, kind="Internal"):
        return Buf(self.nc.dram_tensor(name, list(shape), dtype, kind=kind))

    def _deps(self, reads, writes):
        deps = []
        for t in reads:
            if t.w is not None:
                deps.append(t.w)
        for t in writes:
            if t.w is not None:
                deps.append(t.w)
            deps.extend(t.r.items())
        return deps

    def _waits(self, eng, deps, skip_self=False):
        kn = self.known[eng]
        need = {}
        own = self.ck[eng]
        for key, val in deps:
            if skip_self and key == own:
                continue
            if kn.get(key, 0) >= val:
                continue
            if need.get(key, 0) < val:
                need[key] = val
        for key, val in need.items():
            kn[key] = val
        return [(self.sem[k], v) for k, v in need.items()]

    def _mark(self, tok, reads, writes):
        for t in reads:
            if t.r.get(tok[0], 0) < tok[1]:
                t.r[tok[0]] = tok[1]
        for t in writes:
            t.w = tok
            t.r = {}

    def op(self, eng, fn, reads=(), writes=(), skip_self=False, noinc=False):
        deps = self._deps(reads, writes)
        waits = self._waits(eng, deps, skip_self)
        if noinc:
            tok = (self.ck[eng], self.cnt[eng] + 1)
            self.stream[eng].append((waits, fn, None))
        else:
            self.cnt[eng] += 1
            tok = (self.ck[eng], self.cnt[eng])
            self.stream[eng].append((waits, fn, (self.sem[tok[0]], 1)))
        self._mark(tok, reads, writes)
        return tok

    def I(self, eng, name, **kw):
        reads, writes, args = [], [], {}
        for k, v in kw.items():
            if isinstance(v, V):
                args[k] = v.ap
                (writes if k in WRITE_KW else reads).extend(v.trs)
            else:
                args[k] = v
        return self.op(eng, lambda e: getattr(e, name)(**args), reads, writes)

    def mm(self, out, lhsT, rhs, start=True, stop=True):
        o, l, r = out.ap, lhsT.ap, rhs.ap
        return self.op("pe", lambda e: e.matmul(o, l, r, start=start, stop=stop),
                       list(lhsT.trs) + list(rhs.trs), list(out.trs), skip_self=True, noinc=not stop)

    def tp(self, out, in_, ident):
        o, i, d = out.ap, in_.ap, ident.ap
        return self.op("pe", lambda e: e.transpose(o, i, d),
                       list(in_.trs) + list(ident.trs), list(out.trs), skip_self=True)

    def dma(self, q, out, in_, **kw):
        reads, writes = list(in_.trs), list(out.trs)
        deps = self._deps(reads, writes)
        i = self.dnext[q]
        self.dnext[q] = (i + 1) % self.ndma
        key = f"d_{q}{i}"
        prev = self.dval[q][i]
        if prev > 0:
            deps.append((key, prev))
        self.dval[q][i] = prev + 16
        tok = (key, prev + 16)
        waits = self._waits(q, deps)
        o, a = out.ap, in_.ap

        def fn(e):
            return e.dma_start(out=(o(e) if callable(o) else o), in_=(a(e) if callable(a) else a), **kw)

        self.stream[q].append((waits, fn, (self.sem[key], 16)))
        self._mark(tok, reads, writes)
        return tok

    def coll(self, kind, out, in_, groups):
        q = "pool"
        if "cc" not in self.sem:
            self.sem["cc"] = self.es.enter_context(self.nc.semaphore("cc"))
            self.ccval = 0
        reads, writes = list(in_.trs), list(out.trs)
        deps = self._deps(reads, writes)
        if self.ccval > 0:
            deps.append(("cc", self.ccval))
        self.ccval += 1
        tok = ("cc", self.ccval)
        waits = self._waits(q, deps)
        o, a = out.ap, in_.ap
        self.stream[q].append((waits, lambda e: e.collective_compute(kind, ALU.bypass, groups, [a], [o]),
                               (self.sem["cc"], 1)))
        self._mark(tok, reads, writes)
        return tok

    def emit(self):
        waits = []
        for q in self.DMAQ:
            for i in range(self.ndma):
                if self.dval[q][i] > 0:
                    waits.append((self.sem[f"d_{q}{i}"], self.dval[q][i]))
        for e in self.ENG:
            if e != "sp" and self.cnt[e] > 0:
                waits.append((self.sem[self.ck[e]], self.cnt[e]))
        if "cc" in self.sem and self.ccval > 0:
            waits.append((self.sem["cc"], self.ccval))
        self.stream["sp"].append((waits, None, None))
        with self.nc.Block() as block:
            decos = {"pe": block.tensor, "dve": block.vector, "act": block.scalar,
                     "pool": block.gpsimd, "sp": block.sync}
            for e in self.ENG:
                items = self.stream[e]
                if not items:
                    continue

                def body(engine, items=items):
                    for waits, fn, inc in items:
                        for sem, val in waits:
                            engine.wait_ge(sem, val)
                        if fn is not None:
                            ins = fn(engine)
                            if inc is not None:
                                ins.then_inc(inc[0], inc[1])

                decos[e](body)
        self.es.close()
        return self.nc


def layer_norm_tile(P, src, dst, gt, bt, st, mv, rs, eng2="pool"):
    for i in range(2):
        P.I("dve", "bn_stats", out=st[:, i, :], in_=src(slice(i * 512, (i + 1) * 512)))
    P.I("dve", "bn_aggr", out=mv[:, :], in_=st[:, :, :])
    P.I("act", "activation", out=rs[:, :], in_=mv[:, 1:2], func=AF.Sqrt, bias=EPS, scale=1.0)
    P.I("dve", "reciprocal", out=rs[:, :], in_=rs[:, :])
    full = slice(0, 1024)
    P.I("dve", "tensor_scalar", out=dst(full), in0=src(full), scalar1=mv[:, 0:1], scalar2=rs[:, 0:1],
        op0=ALU.subtract, op1=ALU.mult)
    P.I(eng2, "tensor_tensor", out=dst(full), in0=dst(full), in1=gt[:, :], op=ALU.mult)
    P.I(eng2, "tensor_tensor", out=dst(full), in0=dst(full), in1=bt[:, :], op=ALU.add)


NT = 16
NE = 32
NQ = 4
NB = 4
POOL_EVAC = (1, 4, 7)
NOLOAD_DBG = False


def stage_l2(P, D, layer, KC, oT_all, hin, hout, hT_loc, n_exp=NE, glu=False):
    nc = P.nc
    dbg = 0
    Dm = KC * 128
    IN = dict(kind="ExternalInput")
    L = f"_{layer}"
    w_out = D("w_out" + L, [Dm, 1024], F32)
    lng = D("lng" + L, [2, 1024], F32)
    lnb = D("lnb" + L, [2, 1024], F32)
    w_r = D("w_r" + L, [1024, 32], F32)
    b_r = D("b_r" + L, [1, 32], F32)
    w_gu = D("w_gu" + L, [max(n_exp, 1), 1024, 2048], F32)
    b_gu = D("b_gu" + L, [128, NE, 16], F32)
    w_d = D("w_d" + L, [max(n_exp, 1), 1024, 1024], F32)
    b_d = D("b_d" + L, [NE, 1024], F32)
    idn = D("idn", [128, 128], F32)
    if glu:
        w_glu = D("w_glu" + L, [1024, 1024], F32)
        b_glu = D("b_glu" + L, [128, 8], F32)

    acc_all = P.sb("acc", [128, NT, 1024], F32)
    acc_t = [Buf(acc_all.h) for _ in range(NT)]

    class _Acc:
        def __getitem__(self, key):
            return acc_t[key[1]][key]

    acc = _Acc()
    xT = P.sb("xT", [128, 8, 2048], BF16)
    G = P.sb("G", [128, NT, 32], F32)
    PIECE = 8 * 512 + 2 * 1024
    slot_tr = [Tr(), Tr(), Tr()]
    arena_h = P.sb("arena", [128, 3 * PIECE], BF16).h
    wout_sb = Buf(arena_h, tuple(slot_tr))
    slots = [Buf(arena_h, (slot_tr[i],)) for i in range(3)]
    idt = P.sb("idt", [128, 128], F32)
    gt = P.sb("gt", [128, 1024], F32)
    bt = P.sb("bt", [128, 1024], F32)
    wr_sb = P.sb("wr_sb", [128, 8, 32], F32)
    br_sb = P.sb("br_sb", [128, 32], F32)
    bgu_sb = P.sb("bgu_sb", [128, NE, 16], F32)
    bd_sb = P.sb("bd_sb", [128, 1024], F32)
    Gpad = P.sb("Gpad", [128, 128], F32)
    mix_sb = [P.sb(f"mix_sb{i}", [128, KC, 128], BF16) for i in range(2)]
    hin_sb = [P.sb(f"hin_sb{i}", [128, 1024], F32) for i in range(1)]
    rbuf = [P.sb(f"rbuf{i}", [128, 1024], F32) for i in range(1)]
    hm = [P.sb(f"hm{i}", [128, 1024], F32) for i in range(2)]
    hT32 = P.sb("hT32", [128, 8, 128], F32)
    st = P.sb("st", [128, 2, 6], F32)
    mv = P.sb("mv", [128, 2], F32)
    rs = P.sb("rs", [128, 1], F32)
    lg = P.sb("lg", [128, 32], F32)
    m8 = P.sb("m8", [128, 8], F32)
    msk = P.sb("msk", [128, 32], F32)
    nmx = P.sb("nmx", [128, 1], F32)
    ex = P.sb("ex", [128, 32], F32)
    den = P.sb("den", [128, 1], F32)
    GT = P.sb("GT", [128, 128], F32)
    gbuf = [P.sb(f"gbuf{i}", [128, 512], F32) for i in range(2)]
    sgbuf = [P.sb(f"sgbuf{i}", [128, 512], F32) for i in range(2)]
    tbuf = [P.sb(f"tbuf{i}", [128, 512], F32) for i in range(2)]
    actT = [P.sb(f"actT{i}", [128, 2, 512], BF16) for i in range(2)]
    ev = [P.sb(f"ev{i}", [128, 512], F32) for i in range(2)]
    pb = P.banks()

    P.dma("sp", idt[:, :], idn[:, :])
    P.dma("sp", gt[:, :], lng.v(lng.h[0:1, :].partition_broadcast(128)))
    P.dma("sp", bt[:, :], lnb.v(lnb.h[0:1, :].partition_broadcast(128)))
    P.dma("sp", wr_sb[:, :, :], w_r.v(w_r.h.rearrange("(c p) n -> p c n", p=128)))
    P.dma("sp", br_sb[:, :], b_r.v(b_r.h[0:1, :].partition_broadcast(128)))
    P.dma("sp", bgu_sb[:, :, :], b_gu[:, :, :])
    P.I("pool", "memset", ap=bd_sb[:, :], constant=0.0)
    P.I("pool", "memset", ap=Gpad[:, :], constant=0.0)
    P.dma("sp", bd_sb[0:32, :], b_d[:, :])
    wout_v = wout_sb.v(arena_h[:, 0:KC * 1024].rearrange("p (c n) -> p c n", c=KC))
    for c0 in range(0, KC, 4):
        P.dma("pool", wout_sb.v(arena_h[:, c0 * 1024:(c0 + 4) * 1024].rearrange("p (c n) -> p c n", c=4)),
              w_out.v(w_out.h[c0 * 128:(c0 + 4) * 128, :].rearrange("(c p) n -> p c n", p=128)))
    if glu:
        wglu_v = wout_sb.v(arena_h[:, 8192:16384].rearrange("p (c n) -> p c n", c=8))
        for c0 in range(0, 8, 4):
            P.dma("pool", wout_sb.v(arena_h[:, 8192 + c0 * 1024:8192 + (c0 + 4) * 1024].rearrange("p (c n) -> p c n", c=4)),
                  w_glu.v(w_glu.h[c0 * 128:(c0 + 4) * 128, :].rearrange("(c p) n -> p c n", p=128)))
        bglu_sb = P.sb("bglu_sb", [128, 8], F32)
        P.dma("sp", bglu_sb[:, :], b_glu[:, :])
        sgl = P.sb("sgl", [128, 128], F32)
        gms = [P.sb(f"gms{i}", [128, 8, 128], BF16) for i in range(1)]
    P.I("dve", "tensor_scalar", out=bgu_sb[:, :, 8:16], in0=bgu_sb[:, :, 8:16], scalar1=1.0, scalar2=None,
        op0=ALU.add)

    for i in range(NT):
        ms, hs, rb, hmt = mix_sb[i % 2], hin_sb[0], rbuf[0], hm[i % 2]
        tok = slice(i * 128, (i + 1) * 128)
        def mix_src(e, i=i):
            if "hb" not in P.dyn:
                P.dyn["hb"] = e.snap(e.partition_id() % 4, min_val=0, max_val=3)
            key = ("off", i // 4)
            if key not in P.dyn:
                P.dyn[key] = e.snap((P.dyn["hb"] * 4 + i // 4) * 2048, min_val=0, max_val=15 * 2048)
            return oT_all.h[bass.ds(P.dyn[key], Dm), (i % 4) * 128:(i % 4 + 1) * 128] \
                .rearrange("(c p) t -> p c t", p=128)

        P.dma("sp", ms[:, :, :], oT_all.v(mix_src))
        P.dma("sp", hs[:, :], hin[tok, :])
        if glu:
            gm = gms[0]
            for n in range(8):
                zps = pb[6 + n % 2][:, 0:128]
                for c in range(8):
                    P.mm(zps, wout_sb.v(wglu_v.ap[:, c, n * 128:(n + 1) * 128]), ms[:, c, :],
                         start=(c == 0), stop=(c == 7))
                P.I("act", "activation", out=sgl[:, :], in_=zps, func=AF.Sigmoid, bias=bglu_sb[:, n:n + 1], scale=1.0)
                P.I("dve", "tensor_tensor", out=gm[:, n, :], in0=sgl[:, :], in1=ms[:, n, :], op=ALU.mult)
            ms = gm
        for nh in range(2):
            for c in range(KC):
                P.mm(pb[nh][:, :], ms[:, c, :], wout_sb.v(wout_v.ap[:, c, nh * 512:(nh + 1) * 512]),
                     start=(c == 0), stop=(c == KC - 1))
            P.I("dve", "scalar_tensor_tensor", out=rb[:, nh * 512:(nh + 1) * 512],
                in0=hs[:, nh * 512:(nh + 1) * 512], scalar=ALPHA, in1=pb[nh][:, :],
                op0=ALU.mult, op1=ALU.add)
        layer_norm_tile(P, lambda s: rb[:, s], lambda s: hmt[:, s], gt, bt, st, mv, rs)
        if dbg == 1:
            P.dma("sp", hout[i * 128:(i + 1) * 128, :], hmt[:, :])
            continue
        P.I("act", "activation", out=acc[:, i, :], in_=hmt[:, :], func=AF.Copy, scale=ALPHA)
        for c in range(8):
            P.tp(pb[2 + c // 4][:, (c % 4) * 128:(c % 4 + 1) * 128], hmt[:, c * 128:(c + 1) * 128], idt[:, :])
        for half in range(2):
            src = pb[2 + half].v(pb[2 + half].h[:, :].rearrange("p (c n) -> p c n", c=4))
            P.I("act", "activation", out=hT32[:, half * 4:(half + 1) * 4, :], in_=src, func=AF.Copy)
            P.I("dve", "tensor_copy", out=xT[:, half * 4:(half + 1) * 4, tok], in_=hT32[:, half * 4:(half + 1) * 4, :])
        if dbg == 2:
            continue
        for c in range(8):
            P.mm(pb[4][:, 0:32], hT32[:, c, :], wr_sb[:, c, :], start=(c == 0), stop=(c == 7))
        P.I("dve", "tensor_tensor", out=lg[:, :], in0=pb[4][:, 0:32], in1=br_sb[:, :], op=ALU.add)
        P.I("dve", "max", out=m8[:, :], in_=lg[:, :])
        P.I("dve", "tensor_scalar", out=msk[:, :], in0=lg[:, :], scalar1=m8[:, 3:4], scalar2=None, op0=ALU.is_ge)
        P.I("dve", "tensor_scalar", out=nmx[:, :], in0=m8[:, 0:1], scalar1=-1.0, scalar2=None, op0=ALU.mult)
        P.I("act", "activation", out=ex[:, :], in_=lg[:, :], func=AF.Exp, bias=nmx[:, 0:1], scale=1.0)
        P.I("dve", "tensor_tensor", out=ex[:, :], in0=ex[:, :], in1=msk[:, :], op=ALU.mult)
        P.I("dve", "reduce_sum", out=den[:, :], in_=ex[:, :], axis=AX.X)
        P.I("dve", "reciprocal", out=den[:, :], in_=den[:, :])
        P.I("dve", "tensor_scalar", out=G[:, i, :], in0=ex[:, :], scalar1=den[:, 0:1], scalar2=None, op0=ALU.mult)
        if dbg == 3:
            continue
        P.I("dve", "tensor_copy", out=Gpad[:, 0:32], in_=G[:, i, :])
        P.tp(pb[5][:, 0:128], Gpad[:, :], idt[:, :])
        P.I("dve", "tensor_copy", out=GT[:, :], in_=pb[5][:, 0:128])
        for nh in range(2):
            P.mm(pb[6 + nh][:, :], GT[:, :], bd_sb[:, nh * 512:(nh + 1) * 512])
            P.I("dve", "tensor_tensor", out=acc[:, i, nh * 512:(nh + 1) * 512],
                in0=acc[:, i, nh * 512:(nh + 1) * 512], in1=pb[6 + nh][:, :], op=ALU.add)

    P.dma("sp", gt[:, :], lng.v(lng.h[1:2, :].partition_broadcast(128)))
    P.dma("sp", bt[:, :], lnb.v(lnb.h[1:2, :].partition_broadcast(128)))

    pieces = [(e, q) for e in range(n_exp) for q in range(NQ)]

    def load_piece(k):
        e, q = pieces[k]
        sl = slots[k % 3]
        gu_v = sl.v(arena_h[:, (k % 3) * PIECE:(k % 3) * PIECE + 4096].rearrange("p (c n) -> p c n", c=8))
        d_v = sl.v(arena_h[:, (k % 3) * PIECE + 4096:(k % 3 + 1) * PIECE].rearrange("p (c n) -> p c n", c=2))
        if NOLOAD_DBG and k >= 3:
            return gu_v, d_v
        P.dma("pool", sl.v(gu_v.ap[:, :, 0:256]),
              w_gu.v(w_gu.h[e, :, q * 256:(q + 1) * 256].rearrange("(c p) n -> p c n", p=128)))
        P.dma("pool", sl.v(gu_v.ap[:, :, 256:512]),
              w_gu.v(w_gu.h[e, :, 1024 + q * 256:1024 + (q + 1) * 256].rearrange("(c p) n -> p c n", p=128)))
        P.dma("pool", d_v, w_d.v(w_d.h[e, q * 256:(q + 1) * 256, :].rearrange("(c p) n -> p c n", p=128)))
        return gu_v, d_v

    views = {}
    for k0 in range(min(2, len(pieces))):
        views[k0] = load_piece(k0)
    it = 0
    evi = [0]

    def down_proj(at, sl, d_v, e, b):
        for tt in range(4):
            ti = b * 4 + tt
            for nh in range(2):
                yps = pb[4 + evi[0] % 4]
                evb = ev[evi[0] % 2]
                evi[0] += 1
                for jj in range(2):
                    P.mm(yps[:, :], at[:, jj, tt * 128:(tt + 1) * 128], sl.v(d_v.ap[:, jj, nh * 512:(nh + 1) * 512]),
                         start=(jj == 0), stop=(jj == 1))
                if (tt * 2 + nh) in POOL_EVAC:
                    P.I("act", "activation", out=evb[:, :], in_=yps[:, :], func=AF.Copy, scale=G[:, ti, e:e + 1])
                    P.I("pool", "tensor_tensor", out=acc[:, ti, nh * 512:(nh + 1) * 512],
                        in0=acc[:, ti, nh * 512:(nh + 1) * 512], in1=evb[:, :], op=ALU.add)
                else:
                    P.I("dve", "scalar_tensor_tensor", out=acc[:, ti, nh * 512:(nh + 1) * 512], in0=yps[:, :],
                        scalar=G[:, ti, e:e + 1], in1=acc[:, ti, nh * 512:(nh + 1) * 512],
                        op0=ALU.mult, op1=ALU.add)

    pending = None
    for k, (e, q) in enumerate(pieces):
        gu_v, d_v = views.pop(k)
        sl = slots[k % 3]
        for b in range(NB):
            at = actT[it % 2]
            tokb = slice(b * 512, (b + 1) * 512)
            for jj in range(2):
                gps, ups = pb[jj], pb[2 + jj]
                gb, sgb, tb = gbuf[jj], sgbuf[jj], tbuf[jj]
                for c in range(8):
                    P.mm(gps[:, :], sl.v(gu_v.ap[:, c, jj * 128:(jj + 1) * 128]), xT[:, c, tokb],
                         start=(c == 0), stop=(c == 7))
                for c in range(8):
                    P.mm(ups[:, :], sl.v(gu_v.ap[:, c, 256 + jj * 128:256 + (jj + 1) * 128]), xT[:, c, tokb],
                         start=(c == 0), stop=(c == 7))
                ch = q * 2 + jj
                P.I("dve", "tensor_scalar", out=gb[:, :], in0=gps[:, :], scalar1=bgu_sb[:, e, ch:ch + 1],
                    scalar2=7.0, op0=ALU.add, op1=ALU.min)
                P.I("act", "activation", out=sgb[:, :], in_=gb[:, :], func=AF.Sigmoid, scale=1.702)
                P.I("pool", "tensor_tensor", out=sgb[:, :], in0=sgb[:, :], in1=gb[:, :], op=ALU.mult)
                P.I("dve", "tensor_scalar", out=tb[:, :], in0=ups[:, :], scalar1=bgu_sb[:, e, 8 + ch:9 + ch],
                    scalar2=8.0, op0=ALU.add, op1=ALU.min)
                P.I("dve", "scalar_tensor_tensor", out=at[:, jj, :], in0=tb[:, :], scalar=-6.0, in1=sgb[:, :],
                    op0=ALU.max, op1=ALU.mult)
            if pending is not None:
                down_proj(*pending)
            pending = (at, sl, d_v, e, b)
            if b == 0 and k + 2 < len(pieces):
                views[k + 2] = load_piece(k + 2)
            it += 1
    if pending is not None:
        down_proj(*pending)

    for i in range(NT):
        ob = hm[i % 2]
        layer_norm_tile(P, lambda s: acc[:, i, s], lambda s: ob[:, s], gt, bt, st, mv, rs)
        P.dma("sp", hout[i * 128:(i + 1) * 128, :], ob[:, :])
        if hT_loc is not None:
            for c in range(8):
                P.tp(pb[2 + c // 4][:, (c % 4) * 128:(c % 4 + 1) * 128], ob[:, c * 128:(c + 1) * 128], idt[:, :])
            hb16 = mix_sb[i % 2]
            for half in range(2):
                src = pb[2 + half].v(pb[2 + half].h[:, :].rearrange("p (c n) -> p c n", c=4))
                P.I("act", "activation", out=hb16[:, half * 4:(half + 1) * 4, :], in_=src, func=AF.Copy)
            P.dma("sp", hT_loc.v(hT_loc.h[(i // 2) * 1024:(i // 2 + 1) * 1024, (i % 2) * 128:(i % 2 + 1) * 128]
                                 .rearrange("(c p) t -> p c t", p=128)), hb16[:, 0:8, :])


L1CFG = {"ml": dict(HPC=2, dk=64, dv=128), "ret": dict(HPC=2, dk=128, dv=256), "gla": dict(HPC=1, dk=128, dv=256)}
SEQ = 8192
NCH = SEQ // 128


def stage_l1(P, D, kind, xsrc, oT_loc, nch=NCH):
    cfg = L1CFG[kind]
    HPC, dk, dv = cfg["HPC"], cfg["dk"], cfg["dv"]
    dvx = dv + 1 if kind == "ml" else dv
    cscale = float(dk) ** -0.5
    nc = P.nc
    K_ = kind + "_"
    nq = 2 * HPC * dk if kind == "ret" else HPC * dk
    wq = D(K_ + "wq", [1024, nq], F32)
    wk = D(K_ + "wk", [1024, nq], F32)
    wv = D(K_ + "wv", [1024, HPC * dv], F32)
    wg = D(K_ + "wg", [1024, HPC * dv], F32)
    ng = D(K_ + "ng", [1, HPC * dv], F32)
    tri = D("tri", [128, 128], F32)
    idn = D("idn", [128, 128], F32)
    if kind == "ret":
        cosT = D("cosT", [128, SEQ], F32)
        sinT = D("sinT", [128, SEQ], F32)
        cosk = D("cosk", [SEQ, 128], F32)
        sink = D("sink", [SEQ, 128], F32)
        lgc = D("lgc", [128, HPC * 128], F32)
    if kind == "gla":
        wz = D(K_ + "wz", [1024, 16], F32)
        wga = D(K_ + "wga", [128, 128], F32)
    if kind == "ml":
        wgt = D(K_ + "wgt", [1024, 2 * HPC], F32)
        bgt = D(K_ + "bgt", [1, 2 * HPC], F32)

    idt = P.sb("idt", [128, 128], F32)
    P.dma("sp", idt[:, :], idn[:, :])
    wq_sb = P.sb("wq_sb", [128, 8, nq], BF16)
    wk_sb = P.sb("wk_sb", [128, 8, nq], BF16)
    wv_sb = P.sb("wv_sb", [128, 8, HPC * dv], BF16)
    wg_sb = P.sb("wg_sb", [128, 8, HPC * dv], BF16)
    ng_sb = P.sb("ng_sb", [128, HPC * dv], F32)
    tri_sb = P.sb("tri_sb", [128, 128], F32)
    for dst, src in ((wq_sb, wq), (wk_sb, wk), (wv_sb, wv), (wg_sb, wg)):
        P.dma("pool", dst[:, :, :], src.v(src.h.rearrange("(c p) n -> p c n", p=128)))
    P.dma("sp", ng_sb[:, :], ng.v(ng.h[0:1, :].partition_broadcast(128)))
    P.dma("sp", tri_sb[:, :], tri[:, :])
    lg = P.sb("lg", [128, 128], F32)
    if kind == "ret":
        lgc_sb = P.sb("lgc_sb", [128, HPC * 128], F32)
        P.dma("sp", lgc_sb[:, :], lgc[:, :])
        tabs = [[P.sb(f"tab{i}_{j}", [128, 128], F32) for j in range(4)] for i in range(2)]
    if kind == "gla":
        wz_sb = P.sb("wz_sb", [128, 8, 16], BF16)
        P.dma("pool", wz_sb[:, :, :], wz.v(wz.h.rearrange("(c p) n -> p c n", p=128)))
        wga_sb = P.sb("wga_sb", [128, 128], F32)
        P.dma("sp", wga_sb[:, :], wga[:, :])
        zaug = P.sb("zaug", [128, 128], F32)
        P.I("pool", "memset", ap=zaug[:, :], constant=1.0)
        esb = P.sb("esb", [128, 128], F32)
    if kind == "ml":
        wgt_sb = P.sb("wgt_sb", [128, 8, 2 * HPC], BF16)
        P.dma("pool", wgt_sb[:, :, :], wgt.v(wgt.h.rearrange("(c p) n -> p c n", p=128)))
        bgt_sb = P.sb("bgt_sb", [128, 2 * HPC], F32)
        P.dma("sp", bgt_sb[:, :], bgt.v(bgt.h[0:1, :].partition_broadcast(128)))
        gsb = P.sb("gsb", [128, 2 * HPC], F32)
        lf = P.sb("lf", [128, HPC], F32)
        ei = P.sb("ei", [128, HPC], F32)
        dd = P.sb("dd", [128, 1], F32)
    xc = [P.sb(f"xc{i}", [128, 8, 128], BF16) for i in range(2)]
    S = [P.sb(f"S{j}", [128, dvx], F32) for j in range(HPC)]
    Sbf = [P.sb(f"Sbf{j}", [128, dvx], BF16) for j in range(HPC)]
    for j in range(HPC):
        P.I("pool", "memset", ap=S[j][:, :], constant=0.0)
        P.I("pool", "memset", ap=Sbf[j][:, :], constant=0.0)
    eT = P.sb("eT", [128, 128], F32)
    enT = P.sb("enT", [128, 128], F32)
    ent = P.sb("ent", [128, 128], F32)
    tmp = P.sb("tmp", [128, 128], F32)
    qr = P.sb("qr", [128, 128], F32)
    A = P.sb("A", [128, 128], BF16)
    B = P.sb("B", [128, 128], BF16)
    C = P.sb("C", [128, 128], BF16)
    D = P.sb("D", [128, dvx], BF16)
    sT = P.sb("sT", [128, 128], BF16)
    hn = P.sb("hn", [128, dv], F32)
    gate = P.sb("gate", [128, dv], F32)
    st6 = P.sb("st6", [128, 6], F32)
    mv = P.sb("mv", [128, 2], F32)
    rs = P.sb("rs", [128, 1], F32)
    ob = [P.sb(f"ob{i}", [128, HPC * dv], F32) for i in range(2)]
    NBLK = HPC * dv // 128
    oT_sb = [P.sb(f"oT_sb{i}", [128, NBLK, 128], BF16) for i in range(2)]
    pb = P.banks()

    def proj_fm(dst, w_sb, col0, ncol, x):
        for c in range(8):
            P.mm(dst, w_sb[:, c, col0:col0 + ncol], x[:, c, :], start=(c == 0), stop=(c == 7))

    def proj_tm(dst, w_sb, col0, ncol, x):
        for c in range(8):
            P.mm(dst, x[:, c, :], w_sb[:, c, col0:col0 + ncol], start=(c == 0), stop=(c == 7))

    for ci in range(nch):
        x = xc[ci % 2]
        tok = slice(ci * 128, (ci + 1) * 128)
        xq, xv = xsrc(ci)
        P.dma(xq, x[:, :, :], xv)
        if kind == "ret":
            tb = tabs[ci % 2]
            P.dma("sp", tb[0][:, :], cosT[:, tok])
            P.dma("sp", tb[1][:, :], sinT[:, tok])
            P.dma("sp", tb[2][:, :], cosk[tok, :])
            P.dma("sp", tb[3][:, :], sink[tok, :])
        if kind == "ml":
            proj_tm(pb[7][:, 0:2 * HPC], wgt_sb, 0, 2 * HPC, x)
            P.I("dve", "tensor_tensor", out=gsb[:, :], in0=pb[7][:, 0:2 * HPC], in1=bgt_sb[:, :], op=ALU.add)
            P.I("act", "activation", out=ei[:, :], in_=gsb[:, 0:HPC], func=AF.Exp)
            P.I("act", "activation", out=lf[:, :], in_=gsb[:, HPC:2 * HPC], func=AF.Exp, scale=-1.0)
            P.I("act", "activation", out=lf[:, :], in_=lf[:, :], func=AF.Ln, bias=1.0)
            P.I("dve", "tensor_scalar", out=lf[:, :], in0=lf[:, :], scalar1=-1.0, scalar2=None, op0=ALU.mult)
        obuf = ob[ci % 2]
        for j in range(HPC):
            qT_ps, kT_ps = pb[0][0:dk, 0:128], pb[0][0:dk, 128:256]
            kt_ps = pb[1][:, 0:dk]
            vt_ps, gt_ps = pb[2][:, 0:dv], pb[2][:, 256:256 + dv]
            proj_fm(qT_ps, wq_sb, j * dk, dk, x)
            proj_fm(kT_ps, wk_sb, j * dk, dk, x)
            proj_tm(kt_ps, wk_sb, j * dk, dk, x)
            proj_tm(vt_ps, wv_sb, j * dv, dv, x)
            proj_tm(gt_ps, wg_sb, j * dv, dv, x)
            if kind == "ret":
                qsT_ps, ksT_ps = pb[0][0:dk, 256:384], pb[0][0:dk, 384:512]
                kst_ps = pb[1][:, 128:256]
                proj_fm(qsT_ps, wq_sb, (HPC + j) * dk, dk, x)
                proj_fm(ksT_ps, wk_sb, (HPC + j) * dk, dk, x)
                proj_tm(kst_ps, wk_sb, (HPC + j) * dk, dk, x)
                lgv = lgc_sb[:, j * 128:(j + 1) * 128]
            elif kind == "gla":
                proj_fm(pb[7][0:16, 0:128], wz_sb, 0, 16, x)
                P.I("dve", "tensor_copy", out=zaug[0:16, :], in_=pb[7][0:16, 0:128])
                P.mm(pb[7][:, 128:256], zaug[:, :], wga_sb[:, :])
                P.I("act", "activation", out=esb[:, :], in_=pb[7][:, 128:256], func=AF.Exp, scale=-1.0)
                P.I("act", "activation", out=esb[:, :], in_=esb[:, :], func=AF.Ln, bias=1.0)
                P.I("dve", "tensor_scalar", out=lg[:, :], in0=esb[:, :], scalar1=-1.0 / 16.0, scalar2=None,
                    op0=ALU.mult)
                lgv = lg[:, :]
            else:
                P.I("dve", "tensor_copy", out=lg[:, :], in_=lf.v(lf.h[:, j:j + 1].to_broadcast([128, 128])))
                lgv = lg[:, :]
            bT_ps, bt_ps = pb[3][:, 0:128], pb[3][:, 128:256]
            P.mm(bT_ps, lgv, tri_sb[:, :])
            P.mm(bt_ps, tri_sb[:, :], lgv)
            P.I("act", "activation", out=eT[:, :], in_=bT_ps, func=AF.Exp)
            P.I("act", "activation", out=enT[:, :], in_=bT_ps, func=AF.Exp, scale=-1.0)
            P.I("act", "activation", out=ent[:, :], in_=bt_ps, func=AF.Exp, scale=-1.0)
            if kind == "ret":
                P.I("dve", "tensor_tensor", out=tmp[:, :], in0=qsT_ps, in1=tb[1][:, :], op=ALU.mult)
                P.I("dve", "tensor_tensor", out=qr[:, :], in0=qT_ps, in1=tb[0][:, :], op=ALU.mult)
                P.I("pool", "tensor_tensor", out=qr[:, :], in0=qr[:, :], in1=tmp[:, :], op=ALU.add)
                P.I("pool", "tensor_tensor", out=A[:, :], in0=qr[:, :], in1=eT[:, :], op=ALU.mult)
                P.I("dve", "tensor_tensor", out=tmp[:, :], in0=ksT_ps, in1=tb[1][:, :], op=ALU.mult)
                P.I("dve", "tensor_tensor", out=qr[:, :], in0=kT_ps, in1=tb[0][:, :], op=ALU.mult)
                P.I("pool", "tensor_tensor", out=qr[:, :], in0=qr[:, :], in1=tmp[:, :], op=ALU.add)
                P.I("dve", "scalar_tensor_tensor", out=B[:, :], in0=qr[:, :], scalar=cscale, in1=enT[:, :],
                    op0=ALU.mult, op1=ALU.mult)
                P.I("dve", "tensor_tensor", out=tmp[:, :], in0=kst_ps, in1=tb[3][:, :], op=ALU.mult)
                P.I("dve", "tensor_tensor", out=qr[:, :], in0=kt_ps, in1=tb[2][:, :], op=ALU.mult)
                P.I("pool", "tensor_tensor", out=qr[:, :], in0=qr[:, :], in1=tmp[:, :], op=ALU.add)
                P.I("dve", "scalar_tensor_tensor", out=C[:, :], in0=qr[:, :], scalar=cscale, in1=ent[:, :],
                    op0=ALU.mult, op1=ALU.mult)
            else:
                P.I("dve", "tensor_tensor", out=A[0:dk, :], in0=qT_ps, in1=eT[0:dk, :], op=ALU.mult)
                P.I("dve", "scalar_tensor_tensor", out=B[0:dk, :], in0=kT_ps, scalar=cscale, in1=enT[0:dk, :],
                    op0=ALU.mult, op1=ALU.mult)
                P.I("dve", "scalar_tensor_tensor", out=C[:, 0:dk], in0=kt_ps, scalar=cscale, in1=ent[:, 0:dk],
                    op0=ALU.mult, op1=ALU.mult)
            if kind == "ml":
                P.I("dve", "tensor_scalar", out=D[:, 0:dv], in0=vt_ps, scalar1=ei[:, j:j + 1], scalar2=None,
                    op0=ALU.mult)
                P.I("dve", "tensor_copy", out=D[:, dv:dv + 1], in_=ei[:, j:j + 1])
            else:
                P.I("act", "activation", out=D[:, :], in_=vt_ps, func=AF.Copy)
            sT_ps = pb[4][:, 0:128]
            P.mm(sT_ps, B[0:dk, :], A[0:dk, :])
            P.I("dve", "tensor_tensor", out=sT[:, :], in0=sT_ps, in1=tri_sb[:, :], op=ALU.mult)
            o_ps = pb[5][:, 0:dvx]
            P.mm(o_ps, sT[:, :], D[:, :], start=True, stop=False)
            P.mm(o_ps, A[0:dk, :], Sbf[j][0:dk, :], start=False, stop=True)
            U_ps = pb[6][0:dk, 0:dvx]
            P.mm(U_ps, C[:, 0:dk], D[:, :])
            P.I("pool", "tensor_scalar", out=S[j][0:dk, :], in0=S[j][0:dk, :], scalar1=eT[0:dk, 127:128],
                scalar2=None, op0=ALU.mult)
            P.I("dve", "scalar_tensor_tensor", out=S[j][0:dk, :], in0=U_ps, scalar=eT[0:dk, 127:128],
                in1=S[j][0:dk, :], op0=ALU.mult, op1=ALU.add)
            P.I("pool", "tensor_copy", out=Sbf[j][0:dk, :], in_=S[j][0:dk, :])
            if kind == "ml":
                P.I("act", "activation", out=dd[:, :], in_=pb[5][:, dv:dv + 1], func=AF.Abs)
                P.I("dve", "tensor_scalar", out=dd[:, :], in0=dd[:, :], scalar1=1.0, scalar2=None, op0=ALU.max)
                P.I("dve", "reciprocal", out=dd[:, :], in_=dd[:, :])
                P.I("dve", "tensor_scalar", out=hn[:, :], in0=pb[5][:, 0:dv], scalar1=dd[:, 0:1], scalar2=None,
                    op0=ALU.mult)
                P.I("act", "activation", out=gate[:, :], in_=gt_ps, func=AF.Sigmoid)
            else:
                P.I("dve", "tensor_copy", out=hn[:, :], in_=pb[5][:, 0:dv])
                P.I("act", "activation", out=gate[:, :], in_=gt_ps, func=AF.Silu)
            P.I("dve", "bn_stats", out=st6[:, :], in_=hn[:, :])
            P.I("dve", "bn_aggr", out=mv[:, :], in_=st6[:, :])
            P.I("act", "activation", out=rs[:, :], in_=mv[:, 1:2], func=AF.Sqrt, bias=EPS, scale=1.0)
            P.I("dve", "reciprocal", out=rs[:, :], in_=rs[:, :])
            P.I("dve", "tensor_scalar", out=hn[:, :], in0=hn[:, :], scalar1=mv[:, 0:1], scalar2=rs[:, 0:1],
                op0=ALU.subtract, op1=ALU.mult)
            P.I("pool", "tensor_tensor", out=hn[:, :], in0=hn[:, :], in1=ng_sb[:, j * dv:(j + 1) * dv], op=ALU.mult)
            P.I("pool", "tensor_tensor", out=obuf[:, j * dv:(j + 1) * dv], in0=hn[:, :], in1=gate[:, :], op=ALU.mult)
        otb = oT_sb[ci % 2]
        for blk in range(NBLK):
            P.tp(pb[7][:, blk * 128:(blk + 1) * 128], obuf[:, blk * 128:(blk + 1) * 128], idt[:, :])
        P.I("act", "activation", out=otb[:, :, :],
            in_=pb[7].v(pb[7].h[:, 0:NBLK * 128].rearrange("p (c n) -> p c n", c=NBLK)), func=AF.Copy)
        R_ = HPC * dv
        P.dma("sp", oT_loc.v(oT_loc.h[(ci // 4) * R_:(ci // 4 + 1) * R_, (ci % 4) * 128:(ci % 4 + 1) * 128]
                             .rearrange("(c p) t -> p c t", p=128)), otb[:, :, :])


def _c(a):
    return np.ascontiguousarray(a)


def _tri():
    return np.triu(np.ones((128, 128), np.float32))


def l1_in_maps(kind, h, inp, j):
    cfg = L1CFG[kind]
    HPC, dk, dv = cfg["HPC"], cfg["dk"], cfg["dv"]
    maps = []
    for core in range(NCORES):
        b, hb = core // 4, core % 4
        heads = [hb * HPC + i for i in range(HPC)]
        m = {"xT": _c(h[b].T), "tri": _tri()}
        if kind == "ml":
            w = inp["ml_w_in"][j]
            m["wq"] = _c(np.concatenate([w[:, hd * 64:(hd + 1) * 64] for hd in heads], 1))
            m["wk"] = _c(np.concatenate([w[:, 512 + hd * 64:512 + (hd + 1) * 64] for hd in heads], 1))
            m["wv"] = _c(np.concatenate([w[:, 1024 + hd * 128:1024 + (hd + 1) * 128] for hd in heads], 1))
            m["wg"] = _c(np.concatenate([w[:, 2048 + hd * 128:2048 + (hd + 1) * 128] for hd in heads], 1))
            gi = [3072 + hd for hd in heads] + [3080 + hd for hd in heads]
            m["wgt"] = _c(w[:, gi])
            m["bgt"] = _c(inp["ml_b_gates"][j][[hd for hd in heads] + [8 + hd for hd in heads]][None, :])
            m["ng"] = _c(np.concatenate([inp["ml_norm_g"][j][hd * 128:(hd + 1) * 128] for hd in heads])[None, :])
        elif kind == "ret":
            w = inp["ret_w_in"][j]

            def sw(c0):
                return np.concatenate([w[:, c0 + 64:c0 + 128], w[:, c0:c0 + 64]], 1)
            m["wq"] = _c(np.concatenate([w[:, hd * 128:(hd + 1) * 128] for hd in heads] + [sw(hd * 128) for hd in heads], 1))
            m["wk"] = _c(np.concatenate([w[:, 1024 + hd * 128:1024 + (hd + 1) * 128] for hd in heads]
                                        + [sw(1024 + hd * 128) for hd in heads], 1))
            m["wv"] = _c(np.concatenate([w[:, 2048 + hd * 256:2048 + (hd + 1) * 256] for hd in heads], 1))
            m["wg"] = _c(np.concatenate([w[:, 4096 + hd * 256:4096 + (hd + 1) * 256] for hd in heads], 1))
            m["ng"] = _c(np.concatenate([inp["ret_norm_g"][j][hd * 256:(hd + 1) * 256] for hd in heads])[None, :])
            inv = (10000.0 ** (-np.arange(0, 128, 2, dtype=np.float32) / 128.0)).astype(np.float32)
            ang = np.arange(SEQ, dtype=np.float32)[:, None] * inv[None, :]
            cos, sin = np.cos(ang).astype(np.float32), np.sin(ang).astype(np.float32)
            cosk = np.concatenate([cos, cos], 1)
            sink = np.concatenate([-sin, sin], 1)
            m["cosk"], m["sink"] = _c(cosk), _c(sink)
            m["cosT"], m["sinT"] = _c(cosk.T), _c(sink.T)
            lgam = np.log1p(-(2.0 ** (-5.0 - np.arange(8, dtype=np.float32)))).astype(np.float32)
            m["lgc"] = _c(np.concatenate([np.full((128, 128), lgam[hd], np.float32) for hd in heads], 1))
        else:
            w = inp["gla_w_in"][j]
            hd = heads[0]
            m["wq"] = _c(w[:, hd * 128:(hd + 1) * 128])
            m["wk"] = _c(w[:, 512 + hd * 128:512 + (hd + 1) * 128])
            m["wv"] = _c(w[:, 1024 + hd * 256:1024 + (hd + 1) * 256])
            m["wg"] = _c(w[:, 2048 + hd * 256:2048 + (hd + 1) * 256])
            m["wz"] = _c(w[:, 3072:3088])
            wga = np.zeros((128, 128), np.float32)
            wga[0:16] = inp["gla_w_gate"][j][:, hd * 128:(hd + 1) * 128]
            wga[16] = inp["gla_b_gate"][j][hd * 128:(hd + 1) * 128]
            m["wga"] = wga
            m["ng"] = _c(inp["gla_norm_g"][j][hd * 256:(hd + 1) * 256][None, :])
        maps.append(m)
    return maps


def l1_gather(kind, results):
    outs = []
    for b in range(2):
        outs.append(np.concatenate([np.asarray(results[b * 4 + hb]["o"]) for hb in range(4)], 1))
    return np.stack(outs, 0)


def stage_s5(P, D, xsrc, yT, T=SEQ):
    nc = P.nc
    NBK = T // 512
    NK = int(np.log2(T))
    w_in = D("s5_w_in", [1024, 256], F32)
    lre = D("s5_lre", [128, 8], F32)
    lim = D("s5_lim", [128, 8], F32)
    ldt = D("s5_ldt", [128, 8], F32)
    bbr = D("s5_bbr", [32, 8, 128], F32)
    bbi = D("s5_bbi", [32, 8, 128], F32)
    ccr = D("s5_ccr", [128, 8, 32], F32)
    cci = D("s5_cci", [128, 8, 32], F32)
    ddg = D("s5_ddg", [32, 8, 32], F32)

    w_sb = P.sb("w_sb", [128, 8, 256], BF16)
    P.dma("pool", w_sb[:, :, :], w_in.v(w_in.h.rearrange("(c p) n -> p c n", p=128)))
    bbr_sb = P.sb("bbr_sb", [32, 8, 128], BF16)
    bbi_sb = P.sb("bbi_sb", [32, 8, 128], BF16)
    ddg_sb = P.sb("ddg_sb", [32, 8, 32], BF16)
    P.dma("pool", bbr_sb[:, :, :], bbr[:, :, :])
    P.dma("pool", bbi_sb[:, :, :], bbi[:, :, :])
    P.dma("pool", ddg_sb[:, :, :], ddg[:, :, :])
    ccr_sb = P.sb("ccr_sb", [128, 8, 32], F32)
    cci_sb = P.sb("cci_sb", [128, 8, 32], F32)
    P.dma("sp", ccr_sb[:, :, :], ccr[:, :, :])
    P.dma("sp", cci_sb[:, :, :], cci[:, :, :])
    P.I("dve", "tensor_scalar", out=cci_sb[:, :, :], in0=cci_sb[:, :, :], scalar1=-1.0, scalar2=None, op0=ALU.mult)

    def small(name, n=8):
        return P.sb(name, [128, n], F32)

    lr, li, dt = small("lr"), small("li"), small("dt")
    P.dma("sp", lr[:, :], lre[:, :])
    P.dma("sp", li[:, :], lim[:, :])
    P.dma("sp", dt[:, :], ldt[:, :])
    P.I("act", "activation", out=dt[:, :], in_=dt[:, :], func=AF.Exp)
    rr, th, cs, sn, t1, t2 = small("rr"), small("th"), small("cs"), small("sn"), small("t1"), small("t2")

    def tt(out, a, b, op, eng="dve"):
        P.I(eng, "tensor_tensor", out=out, in0=a, in1=b, op=op)

    tt(rr[:, :], lr[:, :], dt[:, :], ALU.mult)
    P.I("act", "activation", out=rr[:, :], in_=rr[:, :], func=AF.Exp)
    tt(th[:, :], li[:, :], dt[:, :], ALU.mult)
    P.I("act", "activation", out=sn[:, :], in_=th[:, :], func=AF.Sin, scale=1.0 / 16.0)
    hp = small("hp", 1)
    P.I("pool", "memset", ap=hp[:, :], constant=float(np.pi / 2))
    P.I("act", "activation", out=cs[:, :], in_=th[:, :], func=AF.Sin, scale=1.0 / 16.0, bias=hp[:, 0:1])

    def csq(c, s):
        tt(t1[:, :], c, c, ALU.mult)
        tt(t2[:, :], s, s, ALU.mult)
        tt(s, c, s, ALU.mult)
        P.I("dve", "tensor_scalar", out=s, in0=s, scalar1=2.0, scalar2=None, op0=ALU.mult)
        tt(c, t1[:, :], t2[:, :], ALU.subtract)

    for _ in range(4):
        csq(cs[:, :], sn[:, :])
    ar = P.sb("ar", [128, NK, 8], F32)
    ai = P.sb("ai", [128, NK, 8], F32)
    nai = P.sb("nai", [128, NK, 8], F32)
    tt(ar[:, 0, :], rr[:, :], cs[:, :], ALU.mult)
    tt(ai[:, 0, :], rr[:, :], sn[:, :], ALU.mult)
    for k in range(1, NK):
        tt(t1[:, :], ar[:, k - 1, :], ar[:, k - 1, :], ALU.mult)
        tt(t2[:, :], ai[:, k - 1, :], ai[:, k - 1, :], ALU.mult)
        tt(ar[:, k, :], t1[:, :], t2[:, :], ALU.subtract)
        tt(t1[:, :], ar[:, k - 1, :], ai[:, k - 1, :], ALU.mult)
        P.I("dve", "tensor_scalar", out=ai[:, k, :], in0=t1[:, :], scalar1=2.0, scalar2=None, op0=ALU.mult)
    P.I("dve", "tensor_scalar", out=nai[:, :, :], in0=ai[:, :, :], scalar1=-1.0, scalar2=None, op0=ALU.mult)
    cr, ci, nci, m2 = small("cr"), small("ci"), small("nci"), small("m2")
    am1 = small("am1")
    P.I("dve", "tensor_scalar", out=am1[:, :], in0=ar[:, 0, :], scalar1=-1.0, scalar2=None, op0=ALU.add)
    tt(t1[:, :], lr[:, :], lr[:, :], ALU.mult)
    tt(t2[:, :], li[:, :], li[:, :], ALU.mult)
    tt(m2[:, :], t1[:, :], t2[:, :], ALU.add)
    P.I("dve", "reciprocal", out=m2[:, :], in_=m2[:, :])
    tt(t1[:, :], am1[:, :], lr[:, :], ALU.mult)
    tt(t2[:, :], ai[:, 0, :], li[:, :], ALU.mult)
    tt(cr[:, :], t1[:, :], t2[:, :], ALU.add)
    tt(cr[:, :], cr[:, :], m2[:, :], ALU.mult)
    tt(t1[:, :], ai[:, 0, :], lr[:, :], ALU.mult)
    tt(t2[:, :], am1[:, :], li[:, :], ALU.mult)
    tt(ci[:, :], t1[:, :], t2[:, :], ALU.subtract)
    tt(ci[:, :], ci[:, :], m2[:, :], ALU.mult)
    P.I("dve", "tensor_scalar", out=nci[:, :], in0=ci[:, :], scalar1=-1.0, scalar2=None, op0=ALU.mult)

    X = [[P.sb(f"X{a}{b}", [128, T], F32) for b in range(2)] for a in range(2)]
    uT = P.sb("uT", [32, T], BF16)
    xc = [P.sb(f"xc{i}", [128, 8, 512], BF16) for i in range(2)]
    g1 = [P.sb(f"g1_{i}", [32, 512], F32) for i in range(2)]
    g2 = [P.sb(f"g2_{i}", [32, 512], F32) for i in range(2)]
    yo = [P.sb(f"yo{i}", [32, 512], BF16) for i in range(2)]
    pb = P.banks()

    it = 0
    for pp in range(8):
        cur = X[0]
        for tb in range(NBK):
            x = xc[it % 2]
            tok = slice(tb * 512, (tb + 1) * 512)
            xsrc(tb, x)
            ups = pb[it % 2][0:32, :]
            for c in range(8):
                P.mm(ups, w_sb[:, c, pp * 32:(pp + 1) * 32], x[:, c, :], start=(c == 0), stop=(c == 7))
            P.I("act", "activation", out=uT[:, tok], in_=ups, func=AF.Copy)
            br_ps, bi_ps = pb[2 + it % 2], pb[4 + it % 2]
            P.mm(br_ps[:, :], bbr_sb[:, pp, :], uT[:, tok])
            P.mm(bi_ps[:, :], bbi_sb[:, pp, :], uT[:, tok])
            P.I("dve", "tensor_scalar", out=cur[0][:, tok], in0=br_ps[:, :], scalar1=cr[:, pp:pp + 1], scalar2=None,
                op0=ALU.mult)
            P.I("dve", "scalar_tensor_tensor", out=cur[0][:, tok], in0=bi_ps[:, :], scalar=nci[:, pp:pp + 1],
                in1=cur[0][:, tok], op0=ALU.mult, op1=ALU.add)
            P.I("dve", "tensor_scalar", out=cur[1][:, tok], in0=bi_ps[:, :], scalar1=cr[:, pp:pp + 1], scalar2=None,
                op0=ALU.mult)
            P.I("dve", "scalar_tensor_tensor", out=cur[1][:, tok], in0=br_ps[:, :], scalar=ci[:, pp:pp + 1],
                in1=cur[1][:, tok], op0=ALU.mult, op1=ALU.add)
            it += 1
        src_i = 0
        for k in range(NK):
            d = 1 << k
            s, o2 = X[src_i], X[1 - src_i]
            a_r, a_i, na_i = ar[:, k, pp:pp + 1], ai[:, k, pp:pp + 1], nai[:, k, pp:pp + 1]
            P.I("act", "activation", out=o2[0][:, 0:d], in_=s[0][:, 0:d], func=AF.Copy)
            P.I("act", "activation", out=o2[1][:, 0:d], in_=s[1][:, 0:d], func=AF.Copy)
            P.I("dve", "scalar_tensor_tensor", out=o2[0][:, d:T], in0=s[0][:, 0:T - d], scalar=a_r, in1=s[0][:, d:T],
                op0=ALU.mult, op1=ALU.add)
            P.I("dve", "scalar_tensor_tensor", out=o2[0][:, d:T], in0=s[1][:, 0:T - d], scalar=na_i, in1=o2[0][:, d:T],
                op0=ALU.mult, op1=ALU.add)
            P.I("dve", "scalar_tensor_tensor", out=o2[1][:, d:T], in0=s[1][:, 0:T - d], scalar=a_r, in1=s[1][:, d:T],
                op0=ALU.mult, op1=ALU.add)
            P.I("dve", "scalar_tensor_tensor", out=o2[1][:, d:T], in0=s[0][:, 0:T - d], scalar=a_i, in1=o2[1][:, d:T],
                op0=ALU.mult, op1=ALU.add)
            src_i = 1 - src_i
        fin = X[src_i]
        for tb in range(NBK):
            tok = slice(tb * 512, (tb + 1) * 512)
            yps = pb[6 + tb % 2][0:32, :]
            P.mm(yps, ccr_sb[:, pp, :], fin[0][:, tok], start=True, stop=False)
            P.mm(yps, cci_sb[:, pp, :], fin[1][:, tok], start=False, stop=True)
            dps = pb[tb % 2][0:32, :]
            P.mm(dps, ddg_sb[:, pp, :], uT[:, tok])
            a1, a2, yb = g1[tb % 2], g2[tb % 2], yo[tb % 2]
            P.I("act", "activation", out=a1[:, :], in_=yps, func=AF.Copy)
            P.I("dve", "tensor_tensor", out=a1[:, :], in0=a1[:, :], in1=dps, op=ALU.add)
            P.I("pool", "tensor_tensor", out=a2[:, :], in0=a1[:, :], in1=a1[:, :], op=ALU.mult)
            P.I("pool", "tensor_scalar", out=a2[:, :], in0=a2[:, :], scalar1=0.044715, scalar2=1.0,
                op0=ALU.mult, op1=ALU.add)
            P.I("pool", "tensor_tensor", out=a2[:, :], in0=a2[:, :], in1=a1[:, :], op=ALU.mult)
            P.I("act", "activation", out=a2[:, :], in_=a2[:, :], func=AF.Tanh, scale=0.7978845608028654)
            P.I("pool", "tensor_scalar", out=a2[:, :], in0=a2[:, :], scalar1=1.0, scalar2=0.5,
                op0=ALU.add, op1=ALU.mult)
            P.I("pool", "tensor_tensor", out=yb[:, :], in0=a2[:, :], in1=a1[:, :], op=ALU.mult)
            P.dma("sp", yT[tb * 256 + pp * 32:tb * 256 + (pp + 1) * 32, :], yb[:, :])


def s5_in_maps(h, inp, j):
    maps = []
    for core in range(NCORES):
        b, hb = core // 4, core % 4
        g0 = hb * 16
        m = {"xT": _c(h[b].T), "w_in": _c(inp["s5_w_in"][j][:, g0 * 16:(g0 + 16) * 16])}
        lre = inp["s5_lam_re"][j][g0:g0 + 16]
        lim = inp["s5_lam_im"][j][g0:g0 + 16]
        ldt = np.repeat(inp["s5_log_dt"][j][g0:g0 + 16][:, None], 64, 1)

        def lay(a):
            return _c(a.reshape(8, 2, 64).transpose(1, 2, 0).reshape(128, 8))
        m["lre"], m["lim"], m["ldt"] = lay(lre), lay(lim), lay(ldt)
        bre = inp["s5_b_re"][j][g0:g0 + 16]
        bim = inp["s5_b_im"][j][g0:g0 + 16]
        cre = inp["s5_c_re"][j][g0:g0 + 16]
        cim = inp["s5_c_im"][j][g0:g0 + 16]
        dsk = inp["s5_d"][j][g0 * 16:(g0 + 16) * 16]
        bbr = np.zeros((32, 8, 128), np.float32)
        bbi = np.zeros((32, 8, 128), np.float32)
        ccr = np.zeros((128, 8, 32), np.float32)
        cci = np.zeros((128, 8, 32), np.float32)
        ddg = np.zeros((32, 8, 32), np.float32)
        for pp in range(8):
            for g2 in range(2):
                g = pp * 2 + g2
                bbr[g2 * 16:(g2 + 1) * 16, pp, g2 * 64:(g2 + 1) * 64] = bre[g].T
                bbi[g2 * 16:(g2 + 1) * 16, pp, g2 * 64:(g2 + 1) * 64] = bim[g].T
                ccr[g2 * 64:(g2 + 1) * 64, pp, g2 * 16:(g2 + 1) * 16] = cre[g].T
                cci[g2 * 64:(g2 + 1) * 64, pp, g2 * 16:(g2 + 1) * 16] = cim[g].T
            idx = np.arange(32)
            ddg[idx, pp, idx] = dsk[pp * 32:(pp + 1) * 32]
        m.update(bbr=bbr, bbi=bbi, ccr=ccr, cci=cci, ddg=ddg)
        maps.append(m)
    return maps


def s5_gather(results):
    outs = []
    for b in range(2):
        yT = np.concatenate([np.asarray(results[b * 4 + hb]["yT"]) for hb in range(4)], 0)
        outs.append(yT.T)
    return np.stack(outs, 0)


KINDS = ("ml", "ret", "gla", "s5")


def build_fused(n_exp=NE, nlayers=4, stop=0):
    nc = bass.Bass("TRN2", target_bir_lowering=False)
    P = Prog(nc)
    ext = {}

    def D(name, shape, dtype):
        if name not in ext:
            ext[name] = P.dram(name, shape, dtype, kind="ExternalInput")
        return ext[name]

    xT0 = D("xT0", [1024, SEQ], F32)
    hin0 = D("hin0", [2048, 1024], F32)
    hout = P.dram("hout", [2048, 1024], F32, kind="ExternalOutput")
    h_loc = [P.dram(f"h_loc{i}", [2048, 1024], F32) for i in range(2)]
    hT_loc = P.dram("hT_loc", [8 * 1024, 256], BF16)
    hT_all = P.dram("hT_all", [8 * 4096, 256], BF16)

    def hT_src(t0, n):
        r, tl = t0 // 2048, t0 % 2048
        k, col = tl // 256, tl % 256
        return hT_all.v(hT_all.h[k * 4096 + r * 1024:k * 4096 + (r + 1) * 1024, col:col + n]
                        .rearrange("(c p) t -> p c t", p=128))

    for layer in range(nlayers):
        kind = KINDS[layer % 4]
        rows = 256 if kind == "s5" else L1CFG[kind]["HPC"] * L1CFG[kind]["dv"]
        oT_loc = P.dram(f"oT_loc{layer}", [16 * rows, 512], BF16)
        oT_all = P.dram(f"oT_all{layer}", [16 * 2048, 512], BF16)
        P.sb_reset()
        if kind == "s5":
            def xsrc(tb, x):
                for hh in range(2):
                    P.dma("sp", x[:, :, hh * 256:(hh + 1) * 256], hT_src(tb * 512 + hh * 256, 256))
            stage_s5(P, D, xsrc, oT_loc)
        else:
            if layer == 0:
                def xsrc(ci):
                    return "pool", xT0.v(xT0.h[:, ci * 128:(ci + 1) * 128].rearrange("(c p) t -> p c t", p=128))
            else:
                def xsrc(ci):
                    return "sp", hT_src(ci * 128, 128)
            stage_l1(P, D, kind, xsrc, oT_loc)
        if stop == 10 * layer + 1:
            break
        for k in range(16):
            P.coll("AllGather", oT_all.v(oT_all.h[k * 2048:k * 2048 + 4 * rows, :]),
                   oT_loc.v(oT_loc.h[k * rows:(k + 1) * rows, :]), GROUPS)
        P.barrier()
        P.new_epoch()
        if stop == 10 * layer + 2:
            break
        P.sb_reset()
        last = layer == nlayers - 1
        hin = hin0 if layer == 0 else h_loc[(layer - 1) % 2]
        ho = hout if last else h_loc[layer % 2]
        stage_l2(P, D, layer, 4 * rows // 128, oT_all, hin, ho, None if last else hT_loc, n_exp=n_exp,
                 glu=(kind == "s5"))
        if stop == 10 * layer + 3:
            break
        if not last:
            for k in range(8):
                P.coll("AllGather", hT_all.v(hT_all.h[k * 4096:(k + 1) * 4096, :]),
                       hT_loc.v(hT_loc.h[k * 1024:(k + 1) * 1024, :]), GROUPS)
        P.barrier()
        P.new_epoch()
        if stop == 10 * layer + 4:
            break
    P.emit()
    return nc, list(ext.keys())


def fused_in_maps(inp, names, n_exp=NE):
    x = np.asarray(inp["x"], np.float32)
    xf = x.reshape(-1, 1024)
    shared = {"tri": _tri(), "idn": np.eye(128, dtype=np.float32)}
    for layer in range(4):
        L = f"_{layer}"
        kind = KINDS[layer % 4]
        j = layer // 4
        shared["w_out" + L] = _c(inp[{"ml": "ml_w_out", "ret": "ret_w_out", "gla": "gla_w_out", "s5": "s5_w_out"}[kind]][j])
        shared["lng" + L] = _c(inp["ln_g"][layer])
        shared["lnb" + L] = _c(inp["ln_b"][layer])
        shared["w_r" + L] = _c(inp["moe_w_router"][layer])
        shared["b_r" + L] = _c(inp["moe_b_router"][layer][None, :])
        shared["w_gu" + L] = _c(inp["moe_w_gate_up"][layer][:max(n_exp, 1)])
        shared["b_gu" + L] = _c(inp["moe_b_gate_up"][layer].reshape(32, 16, 128).transpose(2, 0, 1))
        shared["w_d" + L] = _c(inp["moe_w_down"][layer][:max(n_exp, 1)])
        shared["b_d" + L] = _c(inp["moe_b_down"][layer])
        if kind == "s5":
            shared["w_glu" + L] = _c(inp["s5_w_glu"][j])
            shared["b_glu" + L] = _c(inp["s5_b_glu"][j].reshape(8, 128).T)
    per_kind = {k: l1_in_maps(k, x, inp, 0) for k in ("ml", "ret", "gla")}
    s5m = s5_in_maps(x, inp, 0)
    maps = []
    for c in range(NCORES):
        b = c // 4
        m = dict(shared)
        m["xT0"] = _c(x[b].T)
        m["hin0"] = _c(xf[c * 2048:(c + 1) * 2048])
        for k in ("ml", "ret", "gla"):
            for key, val in per_kind[k][c].items():
                if key in ("xT", "tri"):
                    continue
                m[key if key in ("cosT", "sinT", "cosk", "sink", "lgc") else k + "_" + key] = val
        for key, val in s5m[c].items():
            if key != "xT":
                m["s5_" + key] = val
        maps.append({k: m[k] for k in names})
    return maps


_FUSED = {}


def kernel(**inp):
    inp = {k: np.asarray(v) for k, v in inp.items()}
    if "p" not in _FUSED:
        _FUSED["p"] = build_fused()
    nc, names = _FUSED["p"]
    res = run_bass_kernel_spmd(nc, fused_in_maps(inp, names), core_ids=list(range(NCORES))).results
    out = np.concatenate([np.asarray(r["hout"]) for r in res], 0).reshape(2, SEQ, 1024)
    return out.astype(np.float32)
```

```python
import contextlib
import numpy as np
import ml_dtypes
import concourse.bass as bass
import concourse.mybir as mybir
from concourse.bass_utils import run_bass_kernel_spmd

F32 = mybir.dt.float32
BF16 = mybir.dt.bfloat16
AF = mybir.ActivationFunctionType
ALU = mybir.AluOpType
AX = mybir.AxisListType
NPBF = ml_dtypes.bfloat16

NCORES = 8
SB_BASE = 16512
SB_TOP = 229344
GROUPS = [[0, 1, 2, 3], [4, 5, 6, 7]]
ALPHA = 8.0 ** 0.25
EPS = 1e-5


class Tr:
    __slots__ = ("w", "r")

    def __init__(self):
        self.w = None
        self.r = {}


class V:
    __slots__ = ("ap", "trs")

    def __init__(self, ap, trs):
        self.ap = ap
        self.trs = trs


class Buf:
    def __init__(self, handle, trs=None):
        self.h = handle
        self.trs = trs if trs is not None else (Tr(),)

    def __getitem__(self, key):
        return V(self.h[key], self.trs)

    def v(self, ap):
        return V(ap, self.trs)


WRITE_KW = ("out", "accum_out", "ap", "out_ap")


class Prog:
    ENG = ("pe", "dve", "act", "pool", "sp")
    DMAQ = ("sp", "pool", "act")

    def __init__(self, nc, ndma=6):
        self.nc = nc
        self.es = contextlib.ExitStack()
        self.stream = {e: [] for e in self.ENG}
        self.cnt = {e: 0 for e in self.ENG}
        self.sem = {}
        self.epoch = 0
        self.ck = {}
        for e in self.ENG:
            self.ck[e] = "c_" + e
            self.sem["c_" + e] = self.es.enter_context(nc.semaphore("c_" + e))
        self.known = {e: {} for e in self.ENG}
        self.ndma = ndma
        for q in self.DMAQ:
            for i in range(ndma):
                self.sem[f"d_{q}{i}"] = self.es.enter_context(nc.semaphore(f"d_{q}{i}"))
        self.dval = {q: [0] * ndma for q in self.DMAQ}
        self.dnext = {q: 0 for q in self.DMAQ}
        self.sb_off = SB_BASE
        self.dyn = {}
        self.nname = 0
        self.pb = None

    def banks(self):
        if self.pb is None:
            self.pb = [self.ps(f"pb{i}", [128, 512], F32) for i in range(8)]
        return self.pb

    def sb(self, name, shape, dtype):
        nbytes = int(np.prod(shape[1:])) * (4 if dtype == F32 else 2)
        nbytes = (nbytes + 63) // 64 * 64
        off = self.sb_off
        assert off + nbytes <= SB_TOP, f"out of SBUF for {name}: {off}+{nbytes}"
        self.sb_off = off + nbytes
        self.nname += 1
        return Buf(self.nc.alloc_sbuf_tensor_at(f"{name}_{self.nname}", list(shape), dtype, offset=off))

    def sb_reset(self, off=None):
        self.sb_off = SB_BASE if off is None else off

    def barrier(self):
        allv = [(self.ck[e], self.cnt[e]) for e in self.ENG if self.cnt[e] > 0]
        for q in self.DMAQ:
            for i in range(self.ndma):
                if self.dval[q][i] > 0:
                    allv.append((f"d_{q}{i}", self.dval[q][i]))
        if "cc" in self.sem and self.ccval > 0:
            allv.append(("cc", self.ccval))
        for e in self.ENG:
            waits = self._waits(e, [d for d in allv if d[0] != self.ck[e]])
            if waits:
                self.stream[e].append((waits, None, None))

    def new_epoch(self):
        self.epoch += 1
        for e in self.ENG:
            k = f"c_{e}_{self.epoch}"
            self.ck[e] = k
            self.sem[k] = self.es.enter_context(self.nc.semaphore(k))
            self.cnt[e] = 0

    def ps(self, name, shape, dtype=F32):
        return Buf(self.nc.alloc_psum_tensor(name, list(shape), dtype))

    def dram(self, name, shape, dtype, kind="Internal"):
        return Buf(self.nc.dram_tensor(name, list(shape), dtype, kind=kind))

    def _deps(self, reads, writes):
        deps = []
        for t in reads:
            if t.w is not None:
                deps.append(t.w)
        for t in writes:
            if t.w is not None:
                deps.append(t.w)
            deps.extend(t.r.items())
        return deps

    def _waits(self, eng, deps, skip_self=False):
        kn = self.known[eng]
        need = {}
        own = self.ck[eng]
        for key, val in deps:
            if skip_self and key == own:
                continue
            if kn.get(key, 0) >= val:
                continue
            if need.get(key, 0) < val:
                need[key] = val
        for key, val in need.items():
            kn[key] = val
        return [(self.sem[k], v) for k, v in need.items()]

    def _mark(self, tok, reads, writes):
        for t in reads:
            if t.r.get(tok[0], 0) < tok[1]:
                t.r[tok[0]] = tok[1]
        for t in writes:
            t.w = tok
            t.r = {}

    def op(self, eng, fn, reads=(), writes=(), skip_self=False, noinc=False):
        deps = self._deps(reads, writes)
        waits = self._waits(eng, deps, skip_self)
        if noinc:
            tok = (self.ck[eng], self.cnt[eng] + 1)
            self.stream[eng].append((waits, fn, None))
        else:
            self.cnt[eng] += 1
            tok = (self.ck[eng], self.cnt[eng])
            self.stream[eng].append((waits, fn, (self.sem[tok[0]], 1)))
        self._mark(tok, reads, writes)
        return tok

    def I(self, eng, name, **kw):
        reads, writes, args = [], [], {}
        for k, v in kw.items():
            if isinstance(v, V):
                args[k] = v.ap
                (writes if k in WRITE_KW else reads).extend(v.trs)
            else:
                args[k] = v
        return self.op(eng, lambda e: getattr(e, name)(**args), reads, writes)

    def mm(self, out, lhsT, rhs, start=True, stop=True):
        o, l, r = out.ap, lhsT.ap, rhs.ap
        return self.op("pe", lambda e: e.matmul(o, l, r, start=start, stop=stop),
                       list(lhsT.trs) + list(rhs.trs), list(out.trs), skip_self=True, noinc=not stop)

    def tp(self, out, in_, ident):
        o, i, d = out.ap, in_.ap, ident.ap
        return self.op("pe", lambda e: e.transpose(o, i, d),
                       list(in_.trs) + list(ident.trs), list(out.trs), skip_self=True)

    def dma(self, q, out, in_, **kw):
        reads, writes = list(in_.trs), list(out.trs)
        deps = self._deps(reads, writes)
        i = self.dnext[q]
        self.dnext[q] = (i + 1) % self.ndma
        key = f"d_{q}{i}"
        prev = self.dval[q][i]
        if prev > 0:
            deps.append((key, prev))
        self.dval[q][i] = prev + 16
        tok = (key, prev + 16)
        waits = self._waits(q, deps)
        o, a = out.ap, in_.ap

        def fn(e):
            return e.dma_start(out=(o(e) if callable(o) else o), in_=(a(e) if callable(a) else a), **kw)

        self.stream[q].append((waits, fn, (self.sem[key], 16)))
        self._mark(tok, reads, writes)
        return tok

    def coll(self, kind, out, in_, groups):
        q = "pool"
        if "cc" not in self.sem:
            self.sem["cc"] = self.es.enter_context(self.nc.semaphore("cc"))
            self.ccval = 0
        reads, writes = list(in_.trs), list(out.trs)
        deps = self._deps(reads, writes)
        if self.ccval > 0:
            deps.append(("cc", self.ccval))
        self.ccval += 1
        tok = ("cc", self.ccval)
        waits = self._waits(q, deps)
        o, a = out.ap, in_.ap
        self.stream[q].append((waits, lambda e: e.collective_compute(kind, ALU.bypass, groups, [a], [o]),
                               (self.sem["cc"], 1)))
        self._mark(tok, reads, writes)
        return tok

    def emit(self):
        waits = []
        for q in self.DMAQ:
            for i in range(self.ndma):
                if self.dval[q][i] > 0:
                    waits.append((self.sem[f"d_{q}{i}"], self.dval[q][i]))
        for e in self.ENG:
            if e != "sp" and self.cnt[e] > 0:
                waits.append((self.sem[self.ck[e]], self.cnt[e]))
        if "cc" in self.sem and self.ccval > 0:
            waits.append((self.sem["cc"], self.ccval))
        self.stream["sp"].append((waits, None, None))
        with self.nc.Block() as block:
            decos = {"pe": block.tensor, "dve": block.vector, "act": block.scalar,
                     "pool": block.gpsimd, "sp": block.sync}
            for e in self.ENG:
                items = self.stream[e]
                if not items:
                    continue

                def body(engine, items=items):
                    for waits, fn, inc in items:
                        for sem, val in waits:
                            engine.wait_ge(sem, val)
                        if fn is not None:
                            ins = fn(engine)
                            if inc is not None:
                                ins.then_inc(inc[0], inc[1])

                decos[e](body)
        self.es.close()
        return self.nc


def layer_norm_tile(P, src, dst, gt, bt, st, mv, rs, eng2="pool"):
    for i in range(2):
        P.I("dve", "bn_stats", out=st[:, i, :], in_=src(slice(i * 512, (i + 1) * 512)))
    P.I("dve", "bn_aggr", out=mv[:, :], in_=st[:, :, :])
    P.I("act", "activation", out=rs[:, :], in_=mv[:, 1:2], func=AF.Sqrt, bias=EPS, scale=1.0)
    P.I("dve", "reciprocal", out=rs[:, :], in_=rs[:, :])
    full = slice(0, 1024)
    P.I("dve", "tensor_scalar", out=dst(full), in0=src(full), scalar1=mv[:, 0:1], scalar2=rs[:, 0:1],
        op0=ALU.subtract, op1=ALU.mult)
    P.I(eng2, "tensor_tensor", out=dst(full), in0=dst(full), in1=gt[:, :], op=ALU.mult)
    P.I(eng2, "tensor_tensor", out=dst(full), in0=dst(full), in1=bt[:, :], op=ALU.add)


NT = 16
NE = 32
NQ = 4
NB = 4
POOL_EVAC = ()
NOLOAD_DBG = False


def stage_l2(P, D, layer, KC, oT_all, hin, hout, hT_loc, n_exp=NE, glu=False):
    nc = P.nc
    dbg = 0
    Dm = KC * 128
    IN = dict(kind="ExternalInput")
    L = f"_{layer}"
    w_out = D("w_out" + L, [Dm, 1024], F32)
    lng = D("lng" + L, [2, 1024], F32)
    lnb = D("lnb" + L, [2, 1024], F32)
    w_r = D("w_r" + L, [1024, 32], F32)
    b_r = D("b_r" + L, [1, 32], F32)
    w_gu = D("w_gu" + L, [max(n_exp, 1), 1024, 2048], F32)
    b_gu = D("b_gu" + L, [128, NE, 16], F32)
    w_d = D("w_d" + L, [max(n_exp, 1), 1024, 1024], F32)
    b_d = D("b_d" + L, [NE, 1024], F32)
    idn = D("idn", [128, 128], F32)
    if glu:
        w_glu = D("w_glu" + L, [1024, 1024], F32)
        b_glu = D("b_glu" + L, [128, 8], F32)

    acc_all = P.sb("acc", [128, NT, 1024], F32)
    acc_t = [Buf(acc_all.h) for _ in range(NT)]

    class _Acc:
        def __getitem__(self, key):
            return acc_t[key[1]][key]

    acc = _Acc()
    xT = P.sb("xT", [128, 8, 2048], BF16)
    G = P.sb("G", [128, NT, 32], F32)
    PIECE = 8 * 512 + 2 * 1024
    slot_tr = [Tr(), Tr(), Tr()]
    arena_h = P.sb("arena", [128, 3 * PIECE], BF16).h
    wout_sb = Buf(arena_h, tuple(slot_tr))
    slots = [Buf(arena_h, (slot_tr[i],)) for i in range(3)]
    idt = P.sb("idt", [128, 128], F32)
    gt = P.sb("gt", [128, 1024], F32)
    bt = P.sb("bt", [128, 1024], F32)
    wr_sb = P.sb("wr_sb", [128, 8, 32], F32)
    br_sb = P.sb("br_sb", [128, 32], F32)
    bgu_sb = P.sb("bgu_sb", [128, NE, 16], F32)
    bd_sb = P.sb("bd_sb", [128, 1024], F32)
    Gpad = P.sb("Gpad", [128, 128], F32)
    mix_sb = [P.sb(f"mix_sb{i}", [128, KC, 128], BF16) for i in range(2)]
    hin_sb = [P.sb(f"hin_sb{i}", [128, 1024], F32) for i in range(1)]
    rbuf = [P.sb(f"rbuf{i}", [128, 1024], F32) for i in range(1)]
    hm = [P.sb(f"hm{i}", [128, 1024], F32) for i in range(2)]
    hT32 = P.sb("hT32", [128, 8, 128], F32)
    st = P.sb("st", [128, 2, 6], F32)
    mv = P.sb("mv", [128, 2], F32)
    rs = P.sb("rs", [128, 1], F32)
    lg = P.sb("lg", [128, 32], F32)
    m8 = P.sb("m8", [128, 8], F32)
    msk = P.sb("msk", [128, 32], F32)
    nmx = P.sb("nmx", [128, 1], F32)
    ex = P.sb("ex", [128, 32], F32)
    den = P.sb("den", [128, 1], F32)
    GT = P.sb("GT", [128, 128], F32)
    gbuf = [P.sb(f"gbuf{i}", [128, 512], F32) for i in range(2)]
    sgbuf = [P.sb(f"sgbuf{i}", [128, 512], F32) for i in range(2)]
    tbuf = [P.sb(f"tbuf{i}", [128, 512], F32) for i in range(2)]
    actT = [P.sb(f"actT{i}", [128, 2, 512], BF16) for i in range(2)]
    ev = [P.sb(f"ev{i}", [128, 512], F32) for i in range(2)]
    pb = P.banks()

    P.dma("sp", idt[:, :], idn[:, :])
    P.dma("sp", gt[:, :], lng.v(lng.h[0:1, :].partition_broadcast(128)))
    P.dma("sp", bt[:, :], lnb.v(lnb.h[0:1, :].partition_broadcast(128)))
    P.dma("sp", wr_sb[:, :, :], w_r.v(w_r.h.rearrange("(c p) n -> p c n", p=128)))
    P.dma("sp", br_sb[:, :], b_r.v(b_r.h[0:1, :].partition_broadcast(128)))
    P.dma("sp", bgu_sb[:, :, :], b_gu[:, :, :])
    P.I("pool", "memset", ap=bd_sb[:, :], constant=0.0)
    P.I("pool", "memset", ap=Gpad[:, :], constant=0.0)
    P.dma("sp", bd_sb[0:32, :], b_d[:, :])
    wout_v = wout_sb.v(arena_h[:, 0:KC * 1024].rearrange("p (c n) -> p c n", c=KC))
    for c0 in range(0, KC, 4):
        P.dma("pool", wout_sb.v(arena_h[:, c0 * 1024:(c0 + 4) * 1024].rearrange("p (c n) -> p c n", c=4)),
              w_out.v(w_out.h[c0 * 128:(c0 + 4) * 128, :].rearrange("(c p) n -> p c n", p=128)))
    if glu:
        wglu_v = wout_sb.v(arena_h[:, 8192:16384].rearrange("p (c n) -> p c n", c=8))
        for c0 in range(0, 8, 4):
            P.dma("pool", wout_sb.v(arena_h[:, 8192 + c0 * 1024:8192 + (c0 + 4) * 1024].rearrange("p (c n) -> p c n", c=4)),
                  w_glu.v(w_glu.h[c0 * 128:(c0 + 4) * 128, :].rearrange("(c p) n -> p c n", p=128)))
        bglu_sb = P.sb("bglu_sb", [128, 8], F32)
        P.dma("sp", bglu_sb[:, :], b_glu[:, :])
        sgl = P.sb("sgl", [128, 128], F32)
        gms = [P.sb(f"gms{i}", [128, 8, 128], BF16) for i in range(1)]
    P.I("dve", "tensor_scalar", out=bgu_sb[:, :, 8:16], in0=bgu_sb[:, :, 8:16], scalar1=1.0, scalar2=None,
        op0=ALU.add)

    for i in range(NT):
        ms, hs, rb, hmt = mix_sb[i % 2], hin_sb[0], rbuf[0], hm[i % 2]
        tok = slice(i * 128, (i + 1) * 128)
        def mix_src(e, i=i):
            if "hb" not in P.dyn:
                P.dyn["hb"] = e.snap(e.partition_id() % 4, min_val=0, max_val=3)
            key = ("off", i // 4)
            if key not in P.dyn:
                P.dyn[key] = e.snap((P.dyn["hb"] * 4 + i // 4) * 2048, min_val=0, max_val=15 * 2048)
            return oT_all.h[bass.ds(P.dyn[key], Dm), (i % 4) * 128:(i % 4 + 1) * 128] \
                .rearrange("(c p) t -> p c t", p=128)

        P.dma("sp", ms[:, :, :], oT_all.v(mix_src))
        P.dma("sp", hs[:, :], hin[tok, :])
        if glu:
            gm = gms[0]
            for n in range(8):
                zps = pb[6 + n % 2][:, 0:128]
                for c in range(8):
                    P.mm(zps, wout_sb.v(wglu_v.ap[:, c, n * 128:(n + 1) * 128]), ms[:, c, :],
                         start=(c == 0), stop=(c == 7))
                P.I("act", "activation", out=sgl[:, :], in_=zps, func=AF.Sigmoid, bias=bglu_sb[:, n:n + 1], scale=1.0)
                P.I("dve", "tensor_tensor", out=gm[:, n, :], in0=sgl[:, :], in1=ms[:, n, :], op=ALU.mult)
            ms = gm
        for nh in range(2):
            for c in range(KC):
                P.mm(pb[nh][:, :], ms[:, c, :], wout_sb.v(wout_v.ap[:, c, nh * 512:(nh + 1) * 512]),
                     start=(c == 0), stop=(c == KC - 1))
            P.I("dve", "scalar_tensor_tensor", out=rb[:, nh * 512:(nh + 1) * 512],
                in0=hs[:, nh * 512:(nh + 1) * 512], scalar=ALPHA, in1=pb[nh][:, :],
                op0=ALU.mult, op1=ALU.add)
        layer_norm_tile(P, lambda s: rb[:, s], lambda s: hmt[:, s], gt, bt, st, mv, rs)
        if dbg == 1:
            P.dma("sp", hout[i * 128:(i + 1) * 128, :], hmt[:, :])
            continue
        P.I("act", "activation", out=acc[:, i, :], in_=hmt[:, :], func=AF.Copy, scale=ALPHA)
        for c in range(8):
            P.tp(pb[2 + c // 4][:, (c % 4) * 128:(c % 4 + 1) * 128], hmt[:, c * 128:(c + 1) * 128], idt[:, :])
        for half in range(2):
            src = pb[2 + half].v(pb[2 + half].h[:, :].rearrange("p (c n) -> p c n", c=4))
            P.I("act", "activation", out=hT32[:, half * 4:(half + 1) * 4, :], in_=src, func=AF.Copy)
            P.I("dve", "tensor_copy", out=xT[:, half * 4:(half + 1) * 4, tok], in_=hT32[:, half * 4:(half + 1) * 4, :])
        if dbg == 2:
            continue
        for c in range(8):
            P.mm(pb[4][:, 0:32], hT32[:, c, :], wr_sb[:, c, :], start=(c == 0), stop=(c == 7))
        P.I("dve", "tensor_tensor", out=lg[:, :], in0=pb[4][:, 0:32], in1=br_sb[:, :], op=ALU.add)
        P.I("dve", "max", out=m8[:, :], in_=lg[:, :])
        P.I("dve", "tensor_scalar", out=msk[:, :], in0=lg[:, :], scalar1=m8[:, 3:4], scalar2=None, op0=ALU.is_ge)
        P.I("dve", "tensor_scalar", out=nmx[:, :], in0=m8[:, 0:1], scalar1=-1.0, scalar2=None, op0=ALU.mult)
        P.I("act", "activation", out=ex[:, :], in_=lg[:, :], func=AF.Exp, bias=nmx[:, 0:1], scale=1.0)
        P.I("dve", "tensor_tensor", out=ex[:, :], in0=ex[:, :], in1=msk[:, :], op=ALU.mult)
        P.I("dve", "reduce_sum", out=den[:, :], in_=ex[:, :], axis=AX.X)
        P.I("dve", "reciprocal", out=den[:, :], in_=den[:, :])
        P.I("dve", "tensor_scalar", out=G[:, i, :], in0=ex[:, :], scalar1=den[:, 0:1], scalar2=None, op0=ALU.mult)
        if dbg == 3:
            continue
        P.I("dve", "tensor_copy", out=Gpad[:, 0:32], in_=G[:, i, :])
        P.tp(pb[5][:, 0:128], Gpad[:, :], idt[:, :])
        P.I("dve", "tensor_copy", out=GT[:, :], in_=pb[5][:, 0:128])
        for nh in range(2):
            P.mm(pb[6 + nh][:, :], GT[:, :], bd_sb[:, nh * 512:(nh + 1) * 512])
            P.I("dve", "tensor_tensor", out=acc[:, i, nh * 512:(nh + 1) * 512],
                in0=acc[:, i, nh * 512:(nh + 1) * 512], in1=pb[6 + nh][:, :], op=ALU.add)

    P.dma("sp", gt[:, :], lng.v(lng.h[1:2, :].partition_broadcast(128)))
    P.dma("sp", bt[:, :], lnb.v(lnb.h[1:2, :].partition_broadcast(128)))

    pieces = [(e, q) for e in range(n_exp) for q in range(NQ)]

    def load_piece(k):
        e, q = pieces[k]
        sl = slots[k % 3]
        gu_v = sl.v(arena_h[:, (k % 3) * PIECE:(k % 3) * PIECE + 4096].rearrange("p (c n) -> p c n", c=8))
        d_v = sl.v(arena_h[:, (k % 3) * PIECE + 4096:(k % 3 + 1) * PIECE].rearrange("p (c n) -> p c n", c=2))
        if NOLOAD_DBG and k >= 3:
            return gu_v, d_v
        P.dma("pool", sl.v(gu_v.ap[:, :, 0:256]),
              w_gu.v(w_gu.h[e, :, q * 256:(q + 1) * 256].rearrange("(c p) n -> p c n", p=128)))
        P.dma("pool", sl.v(gu_v.ap[:, :, 256:512]),
              w_gu.v(w_gu.h[e, :, 1024 + q * 256:1024 + (q + 1) * 256].rearrange("(c p) n -> p c n", p=128)))
        P.dma("pool", d_v, w_d.v(w_d.h[e, q * 256:(q + 1) * 256, :].rearrange("(c p) n -> p c n", p=128)))
        return gu_v, d_v

    views = {}
    for k0 in range(min(2, len(pieces))):
        views[k0] = load_piece(k0)
    it = 0
    evi = [0]

    def down_proj(at, sl, d_v, e, b):
        for tt in range(4):
            ti = b * 4 + tt
            for nh in range(2):
                yps = pb[4 + evi[0] % 4]
                evb = ev[evi[0] % 2]
                evi[0] += 1
                for jj in range(2):
                    P.mm(yps[:, :], at[:, jj, tt * 128:(tt + 1) * 128], sl.v(d_v.ap[:, jj, nh * 512:(nh + 1) * 512]),
                         start=(jj == 0), stop=(jj == 1))
                if (tt * 2 + nh) in POOL_EVAC:
                    P.I("act", "activation", out=evb[:, :], in_=yps[:, :], func=AF.Copy, scale=G[:, ti, e:e + 1])
                    P.I("pool", "tensor_tensor", out=acc[:, ti, nh * 512:(nh + 1) * 512],
                        in0=acc[:, ti, nh * 512:(nh + 1) * 512], in1=evb[:, :], op=ALU.add)
                else:
                    P.I("dve", "scalar_tensor_tensor", out=acc[:, ti, nh * 512:(nh + 1) * 512], in0=yps[:, :],
                        scalar=G[:, ti, e:e + 1], in1=acc[:, ti, nh * 512:(nh + 1) * 512],
                        op0=ALU.mult, op1=ALU.add)

    pending = None
    for k, (e, q) in enumerate(pieces):
        gu_v, d_v = views.pop(k)
        sl = slots[k % 3]
        for b in range(NB):
            at = actT[it % 2]
            tokb = slice(b * 512, (b + 1) * 512)
            for jj in range(2):
                gps, ups = pb[jj], pb[2 + jj]
                gb, sgb, tb = gbuf[jj], sgbuf[jj], tbuf[jj]
                for c in range(8):
                    P.mm(gps[:, :], sl.v(gu_v.ap[:, c, jj * 128:(jj + 1) * 128]), xT[:, c, tokb],
                         start=(c == 0), stop=(c == 7))
                for c in range(8):
                    P.mm(ups[:, :], sl.v(gu_v.ap[:, c, 256 + jj * 128:256 + (jj + 1) * 128]), xT[:, c, tokb],
                         start=(c == 0), stop=(c == 7))
                ch = q * 2 + jj
                P.I("dve", "tensor_scalar", out=gb[:, :], in0=gps[:, :], scalar1=bgu_sb[:, e, ch:ch + 1],
                    scalar2=7.0, op0=ALU.add, op1=ALU.min)
                P.I("act", "activation", out=sgb[:, :], in_=gb[:, :], func=AF.Sigmoid, scale=1.702)
                P.I("dve", "tensor_tensor", out=sgb[:, :], in0=sgb[:, :], in1=gb[:, :], op=ALU.mult)
                P.I("dve", "tensor_scalar", out=tb[:, :], in0=ups[:, :], scalar1=bgu_sb[:, e, 8 + ch:9 + ch],
                    scalar2=8.0, op0=ALU.add, op1=ALU.min)
                P.I("dve", "scalar_tensor_tensor", out=at[:, jj, :], in0=tb[:, :], scalar=-6.0, in1=sgb[:, :],
                    op0=ALU.max, op1=ALU.mult)
            if pending is not None:
                down_proj(*pending)
            pending = (at, sl, d_v, e, b)
            if b == 0 and k + 2 < len(pieces):
                views[k + 2] = load_piece(k + 2)
            it += 1
    if pending is not None:
        down_proj(*pending)

    for i in range(NT):
        ob = hm[i % 2]
        layer_norm_tile(P, lambda s: acc[:, i, s], lambda s: ob[:, s], gt, bt, st, mv, rs)
        P.dma("sp", hout[i * 128:(i + 1) * 128, :], ob[:, :])
        if hT_loc is not None:
            for c in range(8):
                P.tp(pb[2 + c // 4][:, (c % 4) * 128:(c % 4 + 1) * 128], ob[:, c * 128:(c + 1) * 128], idt[:, :])
            hb16 = mix_sb[i % 2]
            for half in range(2):
                src = pb[2 + half].v(pb[2 + half].h[:, :].rearrange("p (c n) -> p c n", c=4))
                P.I("act", "activation", out=hb16[:, half * 4:(half + 1) * 4, :], in_=src, func=AF.Copy)
            P.dma("sp", hT_loc.v(hT_loc.h[(i // 2) * 1024:(i // 2 + 1) * 1024, (i % 2) * 128:(i % 2 + 1) * 128]
                                 .rearrange("(c p) t -> p c t", p=128)), hb16[:, 0:8, :])


L1CFG = {"ml": dict(HPC=2, dk=64, dv=128), "ret": dict(HPC=2, dk=128, dv=256), "gla": dict(HPC=1, dk=128, dv=256)}
SEQ = 8192
NCH = SEQ // 128


def stage_l1(P, D, kind, xsrc, oT_loc, nch=NCH):
    cfg = L1CFG[kind]
    HPC, dk, dv = cfg["HPC"], cfg["dk"], cfg["dv"]
    dvx = dv + 1 if kind == "ml" else dv
    cscale = float(dk) ** -0.5
    nc = P.nc
    K_ = kind + "_"
    nq = 2 * HPC * dk if kind == "ret" else HPC * dk
    wq = D(K_ + "wq", [1024, nq], F32)
    wk = D(K_ + "wk", [1024, nq], F32)
    wv = D(K_ + "wv", [1024, HPC * dv], F32)
    wg = D(K_ + "wg", [1024, HPC * dv], F32)
    ng = D(K_ + "ng", [1, HPC * dv], F32)
    tri = D("tri", [128, 128], F32)
    idn = D("idn", [128, 128], F32)
    if kind == "ret":
        cosT = D("cosT", [128, SEQ], F32)
        sinT = D("sinT", [128, SEQ], F32)
        cosk = D("cosk", [SEQ, 128], F32)
        sink = D("sink", [SEQ, 128], F32)
        lgc = D("lgc", [128, HPC * 128], F32)
    if kind == "gla":
        wz = D(K_ + "wz", [1024, 16], F32)
        wga = D(K_ + "wga", [128, 128], F32)
    if kind == "ml":
        wgt = D(K_ + "wgt", [1024, 2 * HPC], F32)
        bgt = D(K_ + "bgt", [1, 2 * HPC], F32)

    idt = P.sb("idt", [128, 128], F32)
    P.dma("sp", idt[:, :], idn[:, :])
    wq_sb = P.sb("wq_sb", [128, 8, nq], BF16)
    wk_sb = P.sb("wk_sb", [128, 8, nq], BF16)
    wv_sb = P.sb("wv_sb", [128, 8, HPC * dv], BF16)
    wg_sb = P.sb("wg_sb", [128, 8, HPC * dv], BF16)
    ng_sb = P.sb("ng_sb", [128, HPC * dv], F32)
    tri_sb = P.sb("tri_sb", [128, 128], F32)
    for dst, src in ((wq_sb, wq), (wk_sb, wk), (wv_sb, wv), (wg_sb, wg)):
        P.dma("pool", dst[:, :, :], src.v(src.h.rearrange("(c p) n -> p c n", p=128)))
    P.dma("sp", ng_sb[:, :], ng.v(ng.h[0:1, :].partition_broadcast(128)))
    P.dma("sp", tri_sb[:, :], tri[:, :])
    lg = P.sb("lg", [128, 128], F32)
    if kind == "ret":
        lgc_sb = P.sb("lgc_sb", [128, HPC * 128], F32)
        P.dma("sp", lgc_sb[:, :], lgc[:, :])
        tabs = [[P.sb(f"tab{i}_{j}", [128, 128], F32) for j in range(4)] for i in range(2)]
    if kind == "gla":
        wz_sb = P.sb("wz_sb", [128, 8, 16], BF16)
        P.dma("pool", wz_sb[:, :, :], wz.v(wz.h.rearrange("(c p) n -> p c n", p=128)))
        wga_sb = P.sb("wga_sb", [128, 128], F32)
        P.dma("sp", wga_sb[:, :], wga[:, :])
        zaug = P.sb("zaug", [128, 128], F32)
        P.I("pool", "memset", ap=zaug[:, :], constant=1.0)
        esb = P.sb("esb", [128, 128], F32)
    if kind == "ml":
        wgt_sb = P.sb("wgt_sb", [128, 8, 2 * HPC], BF16)
        P.dma("pool", wgt_sb[:, :, :], wgt.v(wgt.h.rearrange("(c p) n -> p c n", p=128)))
        bgt_sb = P.sb("bgt_sb", [128, 2 * HPC], F32)
        P.dma("sp", bgt_sb[:, :], bgt.v(bgt.h[0:1, :].partition_broadcast(128)))
        gsb = P.sb("gsb", [128, 2 * HPC], F32)
        lf = P.sb("lf", [128, HPC], F32)
        ei = P.sb("ei", [128, HPC], F32)
        dd = P.sb("dd", [128, 1], F32)
    xc = [P.sb(f"xc{i}", [128, 8, 128], BF16) for i in range(2)]
    S = [P.sb(f"S{j}", [128, dvx], F32) for j in range(HPC)]
    Sbf = [P.sb(f"Sbf{j}", [128, dvx], BF16) for j in range(HPC)]
    for j in range(HPC):
        P.I("pool", "memset", ap=S[j][:, :], constant=0.0)
        P.I("pool", "memset", ap=Sbf[j][:, :], constant=0.0)
    eT = P.sb("eT", [128, 128], F32)
    enT = P.sb("enT", [128, 128], F32)
    ent = P.sb("ent", [128, 128], F32)
    tmp = P.sb("tmp", [128, 128], F32)
    qr = P.sb("qr", [128, 128], F32)
    A = P.sb("A", [128, 128], BF16)
    B = P.sb("B", [128, 128], BF16)
    C = P.sb("C", [128, 128], BF16)
    D = P.sb("D", [128, dvx], BF16)
    sT = P.sb("sT", [128, 128], BF16)
    hn = P.sb("hn", [128, dv], F32)
    gate = P.sb("gate", [128, dv], F32)
    st6 = P.sb("st6", [128, 6], F32)
    mv = P.sb("mv", [128, 2], F32)
    rs = P.sb("rs", [128, 1], F32)
    ob = [P.sb(f"ob{i}", [128, HPC * dv], F32) for i in range(2)]
    NBLK = HPC * dv // 128
    oT_sb = [P.sb(f"oT_sb{i}", [128, NBLK, 128], BF16) for i in range(2)]
    pb = P.banks()

    def proj_fm(dst, w_sb, col0, ncol, x):
        for c in range(8):
            P.mm(dst, w_sb[:, c, col0:col0 + ncol], x[:, c, :], start=(c == 0), stop=(c == 7))

    def proj_tm(dst, w_sb, col0, ncol, x):
        for c in range(8):
            P.mm(dst, x[:, c, :], w_sb[:, c, col0:col0 + ncol], start=(c == 0), stop=(c == 7))

    for ci in range(nch):
        x = xc[ci % 2]
        tok = slice(ci * 128, (ci + 1) * 128)
        xq, xv = xsrc(ci)
        P.dma(xq, x[:, :, :], xv)
        if kind == "ret":
            tb = tabs[ci % 2]
            P.dma("sp", tb[0][:, :], cosT[:, tok])
            P.dma("sp", tb[1][:, :], sinT[:, tok])
            P.dma("sp", tb[2][:, :], cosk[tok, :])
            P.dma("sp", tb[3][:, :], sink[tok, :])
        if kind == "ml":
            proj_tm(pb[7][:, 0:2 * HPC], wgt_sb, 0, 2 * HPC, x)
            P.I("dve", "tensor_tensor", out=gsb[:, :], in0=pb[7][:, 0:2 * HPC], in1=bgt_sb[:, :], op=ALU.add)
            P.I("act", "activation", out=ei[:, :], in_=gsb[:, 0:HPC], func=AF.Exp)
            P.I("act", "activation", out=lf[:, :], in_=gsb[:, HPC:2 * HPC], func=AF.Exp, scale=-1.0)
            P.I("act", "activation", out=lf[:, :], in_=lf[:, :], func=AF.Ln, bias=1.0)
            P.I("dve", "tensor_scalar", out=lf[:, :], in0=lf[:, :], scalar1=-1.0, scalar2=None, op0=ALU.mult)
        obuf = ob[ci % 2]
        for j in range(HPC):
            qT_ps, kT_ps = pb[0][0:dk, 0:128], pb[0][0:dk, 128:256]
            kt_ps = pb[1][:, 0:dk]
            vt_ps, gt_ps = pb[2][:, 0:dv], pb[2][:, 256:256 + dv]
            proj_fm(qT_ps, wq_sb, j * dk, dk, x)
            proj_fm(kT_ps, wk_sb, j * dk, dk, x)
            proj_tm(kt_ps, wk_sb, j * dk, dk, x)
            proj_tm(vt_ps, wv_sb, j * dv, dv, x)
            proj_tm(gt_ps, wg_sb, j * dv, dv, x)
            if kind == "ret":
                qsT_ps, ksT_ps = pb[0][0:dk, 256:384], pb[0][0:dk, 384:512]
                kst_ps = pb[1][:, 128:256]
                proj_fm(qsT_ps, wq_sb, (HPC + j) * dk, dk, x)
                proj_fm(ksT_ps, wk_sb, (HPC + j) * dk, dk, x)
                proj_tm(kst_ps, wk_sb, (HPC + j) * dk, dk, x)
                lgv = lgc_sb[:, j * 128:(j + 1) * 128]
            elif kind == "gla":
                proj_fm(pb[7][0:16, 0:128], wz_sb, 0, 16, x)
                P.I("dve", "tensor_copy", out=zaug[0:16, :], in_=pb[7][0:16, 0:128])
                P.mm(pb[7][:, 128:256], zaug[:, :], wga_sb[:, :])
                P.I("act", "activation", out=esb[:, :], in_=pb[7][:, 128:256], func=AF.Exp, scale=-1.0)
                P.I("act", "activation", out=esb[:, :], in_=esb[:, :], func=AF.Ln, bias=1.0)
                P.I("dve", "tensor_scalar", out=lg[:, :], in0=esb[:, :], scalar1=-1.0 / 16.0, scalar2=None,
                    op0=ALU.mult)
                lgv = lg[:, :]
            else:
                P.I("dve", "tensor_copy", out=lg[:, :], in_=lf.v(lf.h[:, j:j + 1].to_broadcast([128, 128])))
                lgv = lg[:, :]
            bT_ps, bt_ps = pb[3][:, 0:128], pb[3][:, 128:256]
            P.mm(bT_ps, lgv, tri_sb[:, :])
            P.mm(bt_ps, tri_sb[:, :], lgv)
            P.I("act", "activation", out=eT[:, :], in_=bT_ps, func=AF.Exp)
            P.I("act", "activation", out=enT[:, :], in_=bT_ps, func=AF.Exp, scale=-1.0)
            P.I("act", "activation", out=ent[:, :], in_=bt_ps, func=AF.Exp, scale=-1.0)
            if kind == "ret":
                P.I("dve", "tensor_tensor", out=tmp[:, :], in0=qsT_ps, in1=tb[1][:, :], op=ALU.mult)
                P.I("dve", "tensor_tensor", out=qr[:, :], in0=qT_ps, in1=tb[0][:, :], op=ALU.mult)
                P.I("pool", "tensor_tensor", out=qr[:, :], in0=qr[:, :], in1=tmp[:, :], op=ALU.add)
                P.I("pool", "tensor_tensor", out=A[:, :], in0=qr[:, :], in1=eT[:, :], op=ALU.mult)
                P.I("dve", "tensor_tensor", out=tmp[:, :], in0=ksT_ps, in1=tb[1][:, :], op=ALU.mult)
                P.I("dve", "tensor_tensor", out=qr[:, :], in0=kT_ps, in1=tb[0][:, :], op=ALU.mult)
                P.I("pool", "tensor_tensor", out=qr[:, :], in0=qr[:, :], in1=tmp[:, :], op=ALU.add)
                P.I("dve", "scalar_tensor_tensor", out=B[:, :], in0=qr[:, :], scalar=cscale, in1=enT[:, :],
                    op0=ALU.mult, op1=ALU.mult)
                P.I("dve", "tensor_tensor", out=tmp[:, :], in0=kst_ps, in1=tb[3][:, :], op=ALU.mult)
                P.I("dve", "tensor_tensor", out=qr[:, :], in0=kt_ps, in1=tb[2][:, :], op=ALU.mult)
                P.I("pool", "tensor_tensor", out=qr[:, :], in0=qr[:, :], in1=tmp[:, :], op=ALU.add)
                P.I("dve", "scalar_tensor_tensor", out=C[:, :], in0=qr[:, :], scalar=cscale, in1=ent[:, :],
                    op0=ALU.mult, op1=ALU.mult)
            else:
                P.I("dve", "tensor_tensor", out=A[0:dk, :], in0=qT_ps, in1=eT[0:dk, :], op=ALU.mult)
                P.I("dve", "scalar_tensor_tensor", out=B[0:dk, :], in0=kT_ps, scalar=cscale, in1=enT[0:dk, :],
                    op0=ALU.mult, op1=ALU.mult)
                P.I("dve", "scalar_tensor_tensor", out=C[:, 0:dk], in0=kt_ps, scalar=cscale, in1=ent[:, 0:dk],
                    op0=ALU.mult, op1=ALU.mult)
            if kind == "ml":
                P.I("dve", "tensor_scalar", out=D[:, 0:dv], in0=vt_ps, scalar1=ei[:, j:j + 1], scalar2=None,
                    op0=ALU.mult)
                P.I("dve", "tensor_copy", out=D[:, dv:dv + 1], in_=ei[:, j:j + 1])
            else:
                P.I("act", "activation", out=D[:, :], in_=vt_ps, func=AF.Copy)
            sT_ps = pb[4][:, 0:128]
            P.mm(sT_ps, B[0:dk, :], A[0:dk, :])
            P.I("dve", "tensor_tensor", out=sT[:, :], in0=sT_ps, in1=tri_sb[:, :], op=ALU.mult)
            o_ps = pb[5][:, 0:dvx]
            P.mm(o_ps, sT[:, :], D[:, :], start=True, stop=False)
            P.mm(o_ps, A[0:dk, :], Sbf[j][0:dk, :], start=False, stop=True)
            U_ps = pb[6][0:dk, 0:dvx]
            P.mm(U_ps, C[:, 0:dk], D[:, :])
            P.I("pool", "tensor_scalar", out=S[j][0:dk, :], in0=S[j][0:dk, :], scalar1=eT[0:dk, 127:128],
                scalar2=None, op0=ALU.mult)
            P.I("dve", "scalar_tensor_tensor", out=S[j][0:dk, :], in0=U_ps, scalar=eT[0:dk, 127:128],
                in1=S[j][0:dk, :], op0=ALU.mult, op1=ALU.add)
            P.I("pool", "tensor_copy", out=Sbf[j][0:dk, :], in_=S[j][0:dk, :])
            if kind == "ml":
                P.I("act", "activation", out=dd[:, :], in_=pb[5][:, dv:dv + 1], func=AF.Abs)
                P.I("dve", "tensor_scalar", out=dd[:, :], in0=dd[:, :], scalar1=1.0, scalar2=None, op0=ALU.max)
                P.I("dve", "reciprocal", out=dd[:, :], in_=dd[:, :])
                P.I("dve", "tensor_scalar", out=hn[:, :], in0=pb[5][:, 0:dv], scalar1=dd[:, 0:1], scalar2=None,
                    op0=ALU.mult)
                P.I("act", "activation", out=gate[:, :], in_=gt_ps, func=AF.Sigmoid)
            else:
                P.I("dve", "tensor_copy", out=hn[:, :], in_=pb[5][:, 0:dv])
                P.I("act", "activation", out=gate[:, :], in_=gt_ps, func=AF.Silu)
            P.I("dve", "bn_stats", out=st6[:, :], in_=hn[:, :])
            P.I("dve", "bn_aggr", out=mv[:, :], in_=st6[:, :])
            P.I("act", "activation", out=rs[:, :], in_=mv[:, 1:2], func=AF.Sqrt, bias=EPS, scale=1.0)
            P.I("dve", "reciprocal", out=rs[:, :], in_=rs[:, :])
            P.I("dve", "tensor_scalar", out=hn[:, :], in0=hn[:, :], scalar1=mv[:, 0:1], scalar2=rs[:, 0:1],
                op0=ALU.subtract, op1=ALU.mult)
            P.I("pool", "tensor_tensor", out=hn[:, :], in0=hn[:, :], in1=ng_sb[:, j * dv:(j + 1) * dv], op=ALU.mult)
            P.I("pool", "tensor_tensor", out=obuf[:, j * dv:(j + 1) * dv], in0=hn[:, :], in1=gate[:, :], op=ALU.mult)
        otb = oT_sb[ci % 2]
        for blk in range(NBLK):
            P.tp(pb[7][:, blk * 128:(blk + 1) * 128], obuf[:, blk * 128:(blk + 1) * 128], idt[:, :])
        P.I("act", "activation", out=otb[:, :, :],
            in_=pb[7].v(pb[7].h[:, 0:NBLK * 128].rearrange("p (c n) -> p c n", c=NBLK)), func=AF.Copy)
        R_ = HPC * dv
        P.dma("sp", oT_loc.v(oT_loc.h[(ci // 4) * R_:(ci // 4 + 1) * R_, (ci % 4) * 128:(ci % 4 + 1) * 128]
                             .rearrange("(c p) t -> p c t", p=128)), otb[:, :, :])


def _c(a):
    return np.ascontiguousarray(a)


def _tri():
    return np.triu(np.ones((128, 128), np.float32))


def l1_in_maps(kind, h, inp, j):
    cfg = L1CFG[kind]
    HPC, dk, dv = cfg["HPC"], cfg["dk"], cfg["dv"]
    maps = []
    for core in range(NCORES):
        b, hb = core // 4, core % 4
        heads = [hb * HPC + i for i in range(HPC)]
        m = {"xT": _c(h[b].T), "tri": _tri()}
        if kind == "ml":
            w = inp["ml_w_in"][j]
            m["wq"] = _c(np.concatenate([w[:, hd * 64:(hd + 1) * 64] for hd in heads], 1))
            m["wk"] = _c(np.concatenate([w[:, 512 + hd * 64:512 + (hd + 1) * 64] for hd in heads], 1))
            m["wv"] = _c(np.concatenate([w[:, 1024 + hd * 128:1024 + (hd + 1) * 128] for hd in heads], 1))
            m["wg"] = _c(np.concatenate([w[:, 2048 + hd * 128:2048 + (hd + 1) * 128] for hd in heads], 1))
            gi = [3072 + hd for hd in heads] + [3080 + hd for hd in heads]
            m["wgt"] = _c(w[:, gi])
            m["bgt"] = _c(inp["ml_b_gates"][j][[hd for hd in heads] + [8 + hd for hd in heads]][None, :])
            m["ng"] = _c(np.concatenate([inp["ml_norm_g"][j][hd * 128:(hd + 1) * 128] for hd in heads])[None, :])
        elif kind == "ret":
            w = inp["ret_w_in"][j]

            def sw(c0):
                return np.concatenate([w[:, c0 + 64:c0 + 128], w[:, c0:c0 + 64]], 1)
            m["wq"] = _c(np.concatenate([w[:, hd * 128:(hd + 1) * 128] for hd in heads] + [sw(hd * 128) for hd in heads], 1))
            m["wk"] = _c(np.concatenate([w[:, 1024 + hd * 128:1024 + (hd + 1) * 128] for hd in heads]
                                        + [sw(1024 + hd * 128) for hd in heads], 1))
            m["wv"] = _c(np.concatenate([w[:, 2048 + hd * 256:2048 + (hd + 1) * 256] for hd in heads], 1))
            m["wg"] = _c(np.concatenate([w[:, 4096 + hd * 256:4096 + (hd + 1) * 256] for hd in heads], 1))
            m["ng"] = _c(np.concatenate([inp["ret_norm_g"][j][hd * 256:(hd + 1) * 256] for hd in heads])[None, :])
            inv = (10000.0 ** (-np.arange(0, 128, 2, dtype=np.float32) / 128.0)).astype(np.float32)
            ang = np.arange(SEQ, dtype=np.float32)[:, None] * inv[None, :]
            cos, sin = np.cos(ang).astype(np.float32), np.sin(ang).astype(np.float32)
            cosk = np.concatenate([cos, cos], 1)
            sink = np.concatenate([-sin, sin], 1)
            m["cosk"], m["sink"] = _c(cosk), _c(sink)
            m["cosT"], m["sinT"] = _c(cosk.T), _c(sink.T)
            lgam = np.log1p(-(2.0 ** (-5.0 - np.arange(8, dtype=np.float32)))).astype(np.float32)
            m["lgc"] = _c(np.concatenate([np.full((128, 128), lgam[hd], np.float32) for hd in heads], 1))
        else:
            w = inp["gla_w_in"][j]
            hd = heads[0]
            m["wq"] = _c(w[:, hd * 128:(hd + 1) * 128])
            m["wk"] = _c(w[:, 512 + hd * 128:512 + (hd + 1) * 128])
            m["wv"] = _c(w[:, 1024 + hd * 256:1024 + (hd + 1) * 256])
            m["wg"] = _c(w[:, 2048 + hd * 256:2048 + (hd + 1) * 256])
            m["wz"] = _c(w[:, 3072:3088])
            wga = np.zeros((128, 128), np.float32)
            wga[0:16] = inp["gla_w_gate"][j][:, hd * 128:(hd + 1) * 128]
            wga[16] = inp["gla_b_gate"][j][hd * 128:(hd + 1) * 128]
            m["wga"] = wga
            m["ng"] = _c(inp["gla_norm_g"][j][hd * 256:(hd + 1) * 256][None, :])
        maps.append(m)
    return maps


def l1_gather(kind, results):
    outs = []
    for b in range(2):
        outs.append(np.concatenate([np.asarray(results[b * 4 + hb]["o"]) for hb in range(4)], 1))
    return np.stack(outs, 0)


def stage_s5(P, D, xsrc, yT, T=SEQ):
    nc = P.nc
    NBK = T // 512
    NK = int(np.log2(T))
    w_in = D("s5_w_in", [1024, 256], F32)
    lre = D("s5_lre", [128, 8], F32)
    lim = D("s5_lim", [128, 8], F32)
    ldt = D("s5_ldt", [128, 8], F32)
    bbr = D("s5_bbr", [32, 8, 128], F32)
    bbi = D("s5_bbi", [32, 8, 128], F32)
    ccr = D("s5_ccr", [128, 8, 32], F32)
    cci = D("s5_cci", [128, 8, 32], F32)
    ddg = D("s5_ddg", [32, 8, 32], F32)

    w_sb = P.sb("w_sb", [128, 8, 256], BF16)
    P.dma("pool", w_sb[:, :, :], w_in.v(w_in.h.rearrange("(c p) n -> p c n", p=128)))
    bbr_sb = P.sb("bbr_sb", [32, 8, 128], BF16)
    bbi_sb = P.sb("bbi_sb", [32, 8, 128], BF16)
    ddg_sb = P.sb("ddg_sb", [32, 8, 32], BF16)
    P.dma("pool", bbr_sb[:, :, :], bbr[:, :, :])
    P.dma("pool", bbi_sb[:, :, :], bbi[:, :, :])
    P.dma("pool", ddg_sb[:, :, :], ddg[:, :, :])
    ccr_sb = P.sb("ccr_sb", [128, 8, 32], F32)
    cci_sb = P.sb("cci_sb", [128, 8, 32], F32)
    P.dma("sp", ccr_sb[:, :, :], ccr[:, :, :])
    P.dma("sp", cci_sb[:, :, :], cci[:, :, :])
    P.I("dve", "tensor_scalar", out=cci_sb[:, :, :], in0=cci_sb[:, :, :], scalar1=-1.0, scalar2=None, op0=ALU.mult)

    def small(name, n=8):
        return P.sb(name, [128, n], F32)

    lr, li, dt = small("lr"), small("li"), small("dt")
    P.dma("sp", lr[:, :], lre[:, :])
    P.dma("sp", li[:, :], lim[:, :])
    P.dma("sp", dt[:, :], ldt[:, :])
    P.I("act", "activation", out=dt[:, :], in_=dt[:, :], func=AF.Exp)
    rr, th, cs, sn, t1, t2 = small("rr"), small("th"), small("cs"), small("sn"), small("t1"), small("t2")

    def tt(out, a, b, op, eng="dve"):
        P.I(eng, "tensor_tensor", out=out, in0=a, in1=b, op=op)

    tt(rr[:, :], lr[:, :], dt[:, :], ALU.mult)
    P.I("act", "activation", out=rr[:, :], in_=rr[:, :], func=AF.Exp)
    tt(th[:, :], li[:, :], dt[:, :], ALU.mult)
    P.I("act", "activation", out=sn[:, :], in_=th[:, :], func=AF.Sin, scale=1.0 / 16.0)
    hp = small("hp", 1)
    P.I("pool", "memset", ap=hp[:, :], constant=float(np.pi / 2))
    P.I("act", "activation", out=cs[:, :], in_=th[:, :], func=AF.Sin, scale=1.0 / 16.0, bias=hp[:, 0:1])

    def csq(c, s):
        tt(t1[:, :], c, c, ALU.mult)
        tt(t2[:, :], s, s, ALU.mult)
        tt(s, c, s, ALU.mult)
        P.I("dve", "tensor_scalar", out=s, in0=s, scalar1=2.0, scalar2=None, op0=ALU.mult)
        tt(c, t1[:, :], t2[:, :], ALU.subtract)

    for _ in range(4):
        csq(cs[:, :], sn[:, :])
    ar = P.sb("ar", [128, NK, 8], F32)
    ai = P.sb("ai", [128, NK, 8], F32)
    nai = P.sb("nai", [128, NK, 8], F32)
    tt(ar[:, 0, :], rr[:, :], cs[:, :], ALU.mult)
    tt(ai[:, 0, :], rr[:, :], sn[:, :], ALU.mult)
    for k in range(1, NK):
        tt(t1[:, :], ar[:, k - 1, :], ar[:, k - 1, :], ALU.mult)
        tt(t2[:, :], ai[:, k - 1, :], ai[:, k - 1, :], ALU.mult)
        tt(ar[:, k, :], t1[:, :], t2[:, :], ALU.subtract)
        tt(t1[:, :], ar[:, k - 1, :], ai[:, k - 1, :], ALU.mult)
        P.I("dve", "tensor_scalar", out=ai[:, k, :], in0=t1[:, :], scalar1=2.0, scalar2=None, op0=ALU.mult)
    P.I("dve", "tensor_scalar", out=nai[:, :, :], in0=ai[:, :, :], scalar1=-1.0, scalar2=None, op0=ALU.mult)
    cr, ci, nci, m2 = small("cr"), small("ci"), small("nci"), small("m2")
    am1 = small("am1")
    P.I("dve", "tensor_scalar", out=am1[:, :], in0=ar[:, 0, :], scalar1=-1.0, scalar2=None, op0=ALU.add)
    tt(t1[:, :], lr[:, :], lr[:, :], ALU.mult)
    tt(t2[:, :], li[:, :], li[:, :], ALU.mult)
    tt(m2[:, :], t1[:, :], t2[:, :], ALU.add)
    P.I("dve", "reciprocal", out=m2[:, :], in_=m2[:, :])
    tt(t1[:, :], am1[:, :], lr[:, :], ALU.mult)
    tt(t2[:, :], ai[:, 0, :], li[:, :], ALU.mult)
    tt(cr[:, :], t1[:, :], t2[:, :], ALU.add)
    tt(cr[:, :], cr[:, :], m2[:, :], ALU.mult)
    tt(t1[:, :], ai[:, 0, :], lr[:, :], ALU.mult)
    tt(t2[:, :], am1[:, :], li[:, :], ALU.mult)
    tt(ci[:, :], t1[:, :], t2[:, :], ALU.subtract)
    tt(ci[:, :], ci[:, :], m2[:, :], ALU.mult)
    P.I("dve", "tensor_scalar", out=nci[:, :], in0=ci[:, :], scalar1=-1.0, scalar2=None, op0=ALU.mult)

    X = [[P.sb(f"X{a}{b}", [128, T], F32) for b in range(2)] for a in range(2)]
    uT = P.sb("uT", [32, T], BF16)
    xc = [P.sb(f"xc{i}", [128, 8, 512], BF16) for i in range(2)]
    g1 = [P.sb(f"g1_{i}", [32, 512], F32) for i in range(2)]
    g2 = [P.sb(f"g2_{i}", [32, 512], F32) for i in range(2)]
    yo = [P.sb(f"yo{i}", [32, 512], BF16) for i in range(2)]
    pb = P.banks()

    it = 0
    for pp in range(8):
        cur = X[0]
        for tb in range(NBK):
            x = xc[it % 2]
            tok = slice(tb * 512, (tb + 1) * 512)
            xsrc(tb, x)
            ups = pb[it % 2][0:32, :]
            for c in range(8):
                P.mm(ups, w_sb[:, c, pp * 32:(pp + 1) * 32], x[:, c, :], start=(c == 0), stop=(c == 7))
            P.I("act", "activation", out=uT[:, tok], in_=ups, func=AF.Copy)
            br_ps, bi_ps = pb[2 + it % 2], pb[4 + it % 2]
            P.mm(br_ps[:, :], bbr_sb[:, pp, :], uT[:, tok])
            P.mm(bi_ps[:, :], bbi_sb[:, pp, :], uT[:, tok])
            P.I("dve", "tensor_scalar", out=cur[0][:, tok], in0=br_ps[:, :], scalar1=cr[:, pp:pp + 1], scalar2=None,
                op0=ALU.mult)
            P.I("dve", "scalar_tensor_tensor", out=cur[0][:, tok], in0=bi_ps[:, :], scalar=nci[:, pp:pp + 1],
                in1=cur[0][:, tok], op0=ALU.mult, op1=ALU.add)
            P.I("dve", "tensor_scalar", out=cur[1][:, tok], in0=bi_ps[:, :], scalar1=cr[:, pp:pp + 1], scalar2=None,
                op0=ALU.mult)
            P.I("dve", "scalar_tensor_tensor", out=cur[1][:, tok], in0=br_ps[:, :], scalar=ci[:, pp:pp + 1],
                in1=cur[1][:, tok], op0=ALU.mult, op1=ALU.add)
            it += 1
        src_i = 0
        for k in range(NK):
            d = 1 << k
            s, o2 = X[src_i], X[1 - src_i]
            a_r, a_i, na_i = ar[:, k, pp:pp + 1], ai[:, k, pp:pp + 1], nai[:, k, pp:pp + 1]
            P.I("act", "activation", out=o2[0][:, 0:d], in_=s[0][:, 0:d], func=AF.Copy)
            P.I("act", "activation", out=o2[1][:, 0:d], in_=s[1][:, 0:d], func=AF.Copy)
            P.I("dve", "scalar_tensor_tensor", out=o2[0][:, d:T], in0=s[0][:, 0:T - d], scalar=a_r, in1=s[0][:, d:T],
                op0=ALU.mult, op1=ALU.add)
            P.I("dve", "scalar_tensor_tensor", out=o2[0][:, d:T], in0=s[1][:, 0:T - d], scalar=na_i, in1=o2[0][:, d:T],
                op0=ALU.mult, op1=ALU.add)
            P.I("dve", "scalar_tensor_tensor", out=o2[1][:, d:T], in0=s[1][:, 0:T - d], scalar=a_r, in1=s[1][:, d:T],
                op0=ALU.mult, op1=ALU.add)
            P.I("dve", "scalar_tensor_tensor", out=o2[1][:, d:T], in0=s[0][:, 0:T - d], scalar=a_i, in1=o2[1][:, d:T],
                op0=ALU.mult, op1=ALU.add)
            src_i = 1 - src_i
        fin = X[src_i]
        for tb in range(NBK):
            tok = slice(tb * 512, (tb + 1) * 512)
            yps = pb[6 + tb % 2][0:32, :]
            P.mm(yps, ccr_sb[:, pp, :], fin[0][:, tok], start=True, stop=False)
            P.mm(yps, cci_sb[:, pp, :], fin[1][:, tok], start=False, stop=True)
            dps = pb[tb % 2][0:32, :]
            P.mm(dps, ddg_sb[:, pp, :], uT[:, tok])
            a1, a2, yb = g1[tb % 2], g2[tb % 2], yo[tb % 2]
            P.I("act", "activation", out=a1[:, :], in_=yps, func=AF.Copy)
            P.I("dve", "tensor_tensor", out=a1[:, :], in0=a1[:, :], in1=dps, op=ALU.add)
            P.I("pool", "tensor_tensor", out=a2[:, :], in0=a1[:, :], in1=a1[:, :], op=ALU.mult)
            P.I("pool", "tensor_scalar", out=a2[:, :], in0=a2[:, :], scalar1=0.044715, scalar2=1.0,
                op0=ALU.mult, op1=ALU.add)
            P.I("pool", "tensor_tensor", out=a2[:, :], in0=a2[:, :], in1=a1[:, :], op=ALU.mult)
            P.I("act", "activation", out=a2[:, :], in_=a2[:, :], func=AF.Tanh, scale=0.7978845608028654)
            P.I("pool", "tensor_scalar", out=a2[:, :], in0=a2[:, :], scalar1=1.0, scalar2=0.5,
                op0=ALU.add, op1=ALU.mult)
            P.I("pool", "tensor_tensor", out=yb[:, :], in0=a2[:, :], in1=a1[:, :], op=ALU.mult)
            P.dma("sp", yT[tb * 256 + pp * 32:tb * 256 + (pp + 1) * 32, :], yb[:, :])


def s5_in_maps(h, inp, j):
    maps = []
    for core in range(NCORES):
        b, hb = core // 4, core % 4
        g0 = hb * 16
        m = {"xT": _c(h[b].T), "w_in": _c(inp["s5_w_in"][j][:, g0 * 16:(g0 + 16) * 16])}
        lre = inp["s5_lam_re"][j][g0:g0 + 16]
        lim = inp["s5_lam_im"][j][g0:g0 + 16]
        ldt = np.repeat(inp["s5_log_dt"][j][g0:g0 + 16][:, None], 64, 1)

        def lay(a):
            return _c(a.reshape(8, 2, 64).transpose(1, 2, 0).reshape(128, 8))
        m["lre"], m["lim"], m["ldt"] = lay(lre), lay(lim), lay(ldt)
        bre = inp["s5_b_re"][j][g0:g0 + 16]
        bim = inp["s5_b_im"][j][g0:g0 + 16]
        cre = inp["s5_c_re"][j][g0:g0 + 16]
        cim = inp["s5_c_im"][j][g0:g0 + 16]
        dsk = inp["s5_d"][j][g0 * 16:(g0 + 16) * 16]
        bbr = np.zeros((32, 8, 128), np.float32)
        bbi = np.zeros((32, 8, 128), np.float32)
        ccr = np.zeros((128, 8, 32), np.float32)
        cci = np.zeros((128, 8, 32), np.float32)
        ddg = np.zeros((32, 8, 32), np.float32)
        for pp in range(8):
            for g2 in range(2):
                g = pp * 2 + g2
                bbr[g2 * 16:(g2 + 1) * 16, pp, g2 * 64:(g2 + 1) * 64] = bre[g].T
                bbi[g2 * 16:(g2 + 1) * 16, pp, g2 * 64:(g2 + 1) * 64] = bim[g].T
                ccr[g2 * 64:(g2 + 1) * 64, pp, g2 * 16:(g2 + 1) * 16] = cre[g].T
                cci[g2 * 64:(g2 + 1) * 64, pp, g2 * 16:(g2 + 1) * 16] = cim[g].T
            idx = np.arange(32)
            ddg[idx, pp, idx] = dsk[pp * 32:(pp + 1) * 32]
        m.update(bbr=bbr, bbi=bbi, ccr=ccr, cci=cci, ddg=ddg)
        maps.append(m)
    return maps


def s5_gather(results):
    outs = []
    for b in range(2):
        yT = np.concatenate([np.asarray(results[b * 4 + hb]["yT"]) for hb in range(4)], 0)
        outs.append(yT.T)
    return np.stack(outs, 0)


KINDS = ("ml", "ret", "gla", "s5")


def build_fused(n_exp=NE, nlayers=4, stop=0):
    nc = bass.Bass("TRN2", target_bir_lowering=False)
    P = Prog(nc)
    ext = {}

    def D(name, shape, dtype):
        if name not in ext:
            ext[name] = P.dram(name, shape, dtype, kind="ExternalInput")
        return ext[name]

    xT0 = D("xT0", [1024, SEQ], F32)
    hin0 = D("hin0", [2048, 1024], F32)
    hout = P.dram("hout", [2048, 1024], F32, kind="ExternalOutput")
    h_loc = [P.dram(f"h_loc{i}", [2048, 1024], F32) for i in range(2)]
    hT_loc = P.dram("hT_loc", [8 * 1024, 256], BF16)
    hT_all = P.dram("hT_all", [8 * 4096, 256], BF16)

    def hT_src(t0, n):
        r, tl = t0 // 2048, t0 % 2048
        k, col = tl // 256, tl % 256
        return hT_all.v(hT_all.h[k * 4096 + r * 1024:k * 4096 + (r + 1) * 1024, col:col + n]
                        .rearrange("(c p) t -> p c t", p=128))

    for layer in range(nlayers):
        kind = KINDS[layer % 4]
        rows = 256 if kind == "s5" else L1CFG[kind]["HPC"] * L1CFG[kind]["dv"]
        oT_loc = P.dram(f"oT_loc{layer}", [16 * rows, 512], BF16)
        oT_all = P.dram(f"oT_all{layer}", [16 * 2048, 512], BF16)
        P.sb_reset()
        if kind == "s5":
            def xsrc(tb, x):
                for hh in range(2):
                    P.dma("sp", x[:, :, hh * 256:(hh + 1) * 256], hT_src(tb * 512 + hh * 256, 256))
            stage_s5(P, D, xsrc, oT_loc)
        else:
            if layer == 0:
                def xsrc(ci):
                    return "pool", xT0.v(xT0.h[:, ci * 128:(ci + 1) * 128].rearrange("(c p) t -> p c t", p=128))
            else:
                def xsrc(ci):
                    return "sp", hT_src(ci * 128, 128)
            stage_l1(P, D, kind, xsrc, oT_loc)
        if stop == 10 * layer + 1:
            break
        for k in range(16):
            P.coll("AllGather", oT_all.v(oT_all.h[k * 2048:k * 2048 + 4 * rows, :]),
                   oT_loc.v(oT_loc.h[k * rows:(k + 1) * rows, :]), GROUPS)
        P.barrier()
        P.new_epoch()
        if stop == 10 * layer + 2:
            break
        P.sb_reset()
        last = layer == nlayers - 1
        hin = hin0 if layer == 0 else h_loc[(layer - 1) % 2]
        ho = hout if last else h_loc[layer % 2]
        stage_l2(P, D, layer, 4 * rows // 128, oT_all, hin, ho, None if last else hT_loc, n_exp=n_exp,
                 glu=(kind == "s5"))
        if stop == 10 * layer + 3:
            break
        if not last:
            for k in range(8):
                P.coll("AllGather", hT_all.v(hT_all.h[k * 4096:(k + 1) * 4096, :]),
                       hT_loc.v(hT_loc.h[k * 1024:(k + 1) * 1024, :]), GROUPS)
        P.barrier()
        P.new_epoch()
        if stop == 10 * layer + 4:
            break
    P.emit()
    return nc, list(ext.keys())


def fused_in_maps(inp, names, n_exp=NE):
    x = np.asarray(inp["x"], np.float32)
    xf = x.reshape(-1, 1024)
    shared = {"tri": _tri(), "idn": np.eye(128, dtype=np.float32)}
    for layer in range(4):
        L = f"_{layer}"
        kind = KINDS[layer % 4]
        j = layer // 4
        shared["w_out" + L] = _c(inp[{"ml": "ml_w_out", "ret": "ret_w_out", "gla": "gla_w_out", "s5": "s5_w_out"}[kind]][j])
        shared["lng" + L] = _c(inp["ln_g"][layer])
        shared["lnb" + L] = _c(inp["ln_b"][layer])
        shared["w_r" + L] = _c(inp["moe_w_router"][layer])
        shared["b_r" + L] = _c(inp["moe_b_router"][layer][None, :])
        shared["w_gu" + L] = _c(inp["moe_w_gate_up"][layer][:max(n_exp, 1)])
        shared["b_gu" + L] = _c(inp["moe_b_gate_up"][layer].reshape(32, 16, 128).transpose(2, 0, 1))
        shared["w_d" + L] = _c(inp["moe_w_down"][layer][:max(n_exp, 1)])
        shared["b_d" + L] = _c(inp["moe_b_down"][layer])
        if kind == "s5":
            shared["w_glu" + L] = _c(inp["s5_w_glu"][j])
            shared["b_glu" + L] = _c(inp["s5_b_glu"][j].reshape(8, 128).T)
    per_kind = {k: l1_in_maps(k, x, inp, 0) for k in ("ml", "ret", "gla")}
    s5m = s5_in_maps(x, inp, 0)
    maps = []
    for c in range(NCORES):
        b = c // 4
        m = dict(shared)
        m["xT0"] = _c(x[b].T)
        m["hin0"] = _c(xf[c * 2048:(c + 1) * 2048])
        for k in ("ml", "ret", "gla"):
            for key, val in per_kind[k][c].items():
                if key in ("xT", "tri"):
                    continue
                m[key if key in ("cosT", "sinT", "cosk", "sink", "lgc") else k + "_" + key] = val
        for key, val in s5m[c].items():
            if key != "xT":
                m["s5_" + key] = val
        maps.append({k: m[k] for k in names})
    return maps


_FUSED = {}


def kernel(**inp):
    inp = {k: np.asarray(v) for k, v in inp.items()}
    if "p" not in _FUSED:
        _FUSED["p"] = build_fused()
    nc, names = _FUSED["p"]
    res = run_bass_kernel_spmd(nc, fused_in_maps(inp, names), core_ids=list(range(NCORES))).results
    out = np.concatenate([np.asarray(r["hout"]) for r in res], 0).reshape(2, SEQ, 1024)
    return out.astype(np.float32)
```

```python
import contextlib
import numpy as np
import ml_dtypes
import concourse.bass as bass
import concourse.mybir as mybir
from concourse.bass_utils import run_bass_kernel_spmd

F32 = mybir.dt.float32
BF16 = mybir.dt.bfloat16
AF = mybir.ActivationFunctionType
ALU = mybir.AluOpType
AX = mybir.AxisListType
NPBF = ml_dtypes.bfloat16

NCORES = 8
SB_BASE = 16512
SB_TOP = 229344
GROUPS = [[0, 1, 2, 3], [4, 5, 6, 7]]
ALPHA = 8.0 ** 0.25
EPS = 1e-5


class Tr:
    __slots__ = ("w", "r")

    def __init__(self):
        self.w = None
        self.r = {}


class V:
    __slots__ = ("ap", "trs")

    def __init__(self, ap, trs):
        self.ap = ap
        self.trs = trs


class Buf:
    def __init__(self, handle, trs=None):
        self.h = handle
        self.trs = trs if trs is not None else (Tr(),)

    def __getitem__(self, key):
        return V(self.h[key], self.trs)

    def v(self, ap):
        return V(ap, self.trs)


WRITE_KW = ("out", "accum_out", "ap", "out_ap")


class Prog:
    ENG = ("pe", "dve", "act", "pool", "sp")
    DMAQ = ("sp", "pool", "act")

    def __init__(self, nc, ndma=6):
        self.nc = nc
        self.es = contextlib.ExitStack()
        self.stream = {e: [] for e in self.ENG}
        self.cnt = {e: 0 for e in self.ENG}
        self.sem = {}
        self.epoch = 0
        self.ck = {}
        for e in self.ENG:
            self.ck[e] = "c_" + e
            self.sem["c_" + e] = self.es.enter_context(nc.semaphore("c_" + e))
        self.known = {e: {} for e in self.ENG}
        self.ndma = ndma
        for q in self.DMAQ:
            for i in range(ndma):
                self.sem[f"d_{q}{i}"] = self.es.enter_context(nc.semaphore(f"d_{q}{i}"))
        self.dval = {q: [0] * ndma for q in self.DMAQ}
        self.dnext = {q: 0 for q in self.DMAQ}
        self.sb_off = SB_BASE
        self.dyn = {}
        self.nname = 0
        self.pb = None

    def banks(self):
        if self.pb is None:
            self.pb = [self.ps(f"pb{i}", [128, 512], F32) for i in range(8)]
        return self.pb

    def sb(self, name, shape, dtype):
        nbytes = int(np.prod(shape[1:])) * (4 if dtype == F32 else 2)
        nbytes = (nbytes + 63) // 64 * 64
        off = self.sb_off
        assert off + nbytes <= SB_TOP, f"out of SBUF for {name}: {off}+{nbytes}"
        self.sb_off = off + nbytes
        self.nname += 1
        return Buf(self.nc.alloc_sbuf_tensor_at(f"{name}_{self.nname}", list(shape), dtype, offset=off))

    def sb_reset(self, off=None):
        self.sb_off = SB_BASE if off is None else off

    def barrier(self):
        allv = [(self.ck[e], self.cnt[e]) for e in self.ENG if self.cnt[e] > 0]
        for q in self.DMAQ:
            for i in range(self.ndma):
                if self.dval[q][i] > 0:
                    allv.append((f"d_{q}{i}", self.dval[q][i]))
        if "cc" in self.sem and self.ccval > 0:
            allv.append(("cc", self.ccval))
        for e in self.ENG:
            waits = self._waits(e, [d for d in allv if d[0] != self.ck[e]])
            if waits:
                self.stream[e].append((waits, None, None))

    def new_epoch(self):
        self.epoch += 1
        for e in self.ENG:
            k = f"c_{e}_{self.epoch}"
            self.ck[e] = k
            self.sem[k] = self.es.enter_context(self.nc.semaphore(k))
            self.cnt[e] = 0

    def ps(self, name, shape, dtype=F32):
        return Buf(self.nc.alloc_psum_tensor(name, list(shape), dtype))

    def dram(self, name, shape, dtype, kind="Internal"):
        return Buf(self.nc.dram_tensor(name, list(shape), dtype, kind=kind))

    def _deps(self, reads, writes):
        deps = []
        for t in reads:
            if t.w is not None:
                deps.append(t.w)
        for t in writes:
            if t.w is not None:
                deps.append(t.w)
            deps.extend(t.r.items())
        return deps

    def _waits(self, eng, deps, skip_self=False):
        kn = self.known[eng]
        need = {}
        own = self.ck[eng]
        for key, val in deps:
            if skip_self and key == own:
                continue
            if kn.get(key, 0) >= val:
                continue
            if need.get(key, 0) < val:
                need[key] = val
        for key, val in need.items():
            kn[key] = val
        return [(self.sem[k], v) for k, v in need.items()]

    def _mark(self, tok, reads, writes):
        for t in reads:
            if t.r.get(tok[0], 0) < tok[1]:
                t.r[tok[0]] = tok[1]
        for t in writes:
            t.w = tok
            t.r = {}

    def op(self, eng, fn, reads=(), writes=(), skip_self=False, noinc=False):
        deps = self._deps(reads, writes)
        waits = self._waits(eng, deps, skip_self)
        if noinc:
            tok = (self.ck[eng], self.cnt[eng] + 1)
            self.stream[eng].append((waits, fn, None))
        else:
            self.cnt[eng] += 1
            tok = (self.ck[eng], self.cnt[eng])
            self.stream[eng].append((waits, fn, (self.sem[tok[0]], 1)))
        self._mark(tok, reads, writes)
        return tok

    def I(self, eng, name, **kw):
        reads, writes, args = [], [], {}
        for k, v in kw.items():
            if isinstance(v, V):
                args[k] = v.ap
                (writes if k in WRITE_KW else reads).extend(v.trs)
            else:
                args[k] = v
        return self.op(eng, lambda e: getattr(e, name)(**args), reads, writes)

    def mm(self, out, lhsT, rhs, start=True, stop=True):
        o, l, r = out.ap, lhsT.ap, rhs.ap
        return self.op("pe", lambda e: e.matmul(o, l, r, start=start, stop=stop),
                       list(lhsT.trs) + list(rhs.trs), list(out.trs), skip_self=True, noinc=not stop)

    def tp(self, out, in_, ident):
        o, i, d = out.ap, in_.ap, ident.ap
        return self.op("pe", lambda e: e.transpose(o, i, d),
                       list(in_.trs) + list(ident.trs), list(out.trs), skip_self=True)

    def dma(self, q, out, in_, **kw):
        reads, writes = list(in_.trs), list(out.trs)
        deps = self._deps(reads, writes)
        i = self.dnext[q]
        self.dnext[q] = (i + 1) % self.ndma
        key = f"d_{q}{i}"
        prev = self.dval[q][i]
        if prev > 0:
            deps.append((key, prev))
        self.dval[q][i] = prev + 16
        tok = (key, prev + 16)
        waits = self._waits(q, deps)
        o, a = out.ap, in_.ap

        def fn(e):
            return e.dma_start(out=(o(e) if callable(o) else o), in_=(a(e) if callable(a) else a), **kw)

        self.stream[q].append((waits, fn, (self.sem[key], 16)))
        self._mark(tok, reads, writes)
        return tok

    def coll(self, kind, out, in_, groups):
        q = "pool"
        if "cc" not in self.sem:
            self.sem["cc"] = self.es.enter_context(self.nc.semaphore("cc"))
            self.ccval = 0
        reads, writes = list(in_.trs), list(out.trs)
        deps = self._deps(reads, writes)
        if self.ccval > 0:
            deps.append(("cc", self.ccval))
        self.ccval += 1
        tok = ("cc", self.ccval)
        waits = self._waits(q, deps)
        o, a = out.ap, in_.ap
        self.stream[q].append((waits, lambda e: e.collective_compute(kind, ALU.bypass, groups, [a], [o]),
                               (self.sem["cc"], 1)))
        self._mark(tok, reads, writes)
        return tok

    def emit(self):
        waits = []
        for q in self.DMAQ:
            for i in range(self.ndma):
                if self.dval[q][i] > 0:
                    waits.append((self.sem[f"d_{q}{i}"], self.dval[q][i]))
        for e in self.ENG:
            if e != "sp" and self.cnt[e] > 0:
                waits.append((self.sem[self.ck[e]], self.cnt[e]))
        if "cc" in self.sem and self.ccval > 0:
            waits.append((self.sem["cc"], self.ccval))
        self.stream["sp"].append((waits, None, None))
        with self.nc.Block() as block:
            decos = {"pe": block.tensor, "dve": block.vector, "act": block.scalar,
                     "pool": block.gpsimd, "sp": block.sync}
            for e in self.ENG:
                items = self.stream[e]
                if not items:
                    continue

                def body(engine, items=items):
                    for waits, fn, inc in items:
                        for sem, val in waits:
                            engine.wait_ge(sem, val)
                        if fn is not None:
                            ins = fn(engine)
                            if inc is not None:
                                ins.then_inc(inc[0], inc[1])

                decos[e](body)
        self.es.close()
        return self.nc


def layer_norm_tile(P, src, dst, gt, bt, st, mv, rs, eng2="dve"):
    for i in range(2):
        P.I("dve", "bn_stats", out=st[:, i, :], in_=src(slice(i * 512, (i + 1) * 512)))
    P.I("dve", "bn_aggr", out=mv[:, :], in_=st[:, :, :])
    P.I("act", "activation", out=rs[:, :], in_=mv[:, 1:2], func=AF.Sqrt, bias=EPS, scale=1.0)
    P.I("dve", "reciprocal", out=rs[:, :], in_=rs[:, :])
    full = slice(0, 1024)
    P.I("dve", "tensor_scalar", out=dst(full), in0=src(full), scalar1=mv[:, 0:1], scalar2=rs[:, 0:1],
        op0=ALU.subtract, op1=ALU.mult)
    P.I(eng2, "tensor_tensor", out=dst(full), in0=dst(full), in1=gt[:, :], op=ALU.mult)
    P.I(eng2, "tensor_tensor", out=dst(full), in0=dst(full), in1=bt[:, :], op=ALU.add)


NT = 16
NE = 32
NQ = 4
NB = 4
POOL_EVAC = ()
NOLOAD_DBG = False


def stage_l2(P, D, layer, KC, oT_all, hin, hout, hT_loc, n_exp=NE, glu=False):
    nc = P.nc
    dbg = 0
    Dm = KC * 128
    IN = dict(kind="ExternalInput")
    L = f"_{layer}"
    w_out = D("w_out" + L, [Dm, 1024], F32)
    lng = D("lng" + L, [2, 1024], F32)
    lnb = D("lnb" + L, [2, 1024], F32)
    w_r = D("w_r" + L, [1024, 32], F32)
    b_r = D("b_r" + L, [1, 32], F32)
    w_gu = D("w_gu" + L, [max(n_exp, 1), 1024, 2048], F32)
    b_gu = D("b_gu" + L, [128, NE, 16], F32)
    w_d = D("w_d" + L, [max(n_exp, 1), 1024, 1024], F32)
    b_d = D("b_d" + L, [NE, 1024], F32)
    idn = D("idn", [128, 128], F32)
    if glu:
        w_glu = D("w_glu" + L, [1024, 1024], F32)
        b_glu = D("b_glu" + L, [128, 8], F32)

    acc_all = P.sb("acc", [128, NT, 1024], F32)
    acc_t = [Buf(acc_all.h) for _ in range(NT)]

    class _Acc:
        def __getitem__(self, key):
            return acc_t[key[1]][key]

    acc = _Acc()
    xT = P.sb("xT", [128, 8, 2048], BF16)
    G = P.sb("G", [128, NT, 32], F32)
    PIECE = 8 * 512 + 2 * 1024
    slot_tr = [Tr(), Tr(), Tr()]
    arena_h = P.sb("arena", [128, 3 * PIECE], BF16).h
    wout_sb = Buf(arena_h, tuple(slot_tr))
    slots = [Buf(arena_h, (slot_tr[i],)) for i in range(3)]
    idt = P.sb("idt", [128, 128], F32)
    gt = P.sb("gt", [128, 1024], F32)
    bt = P.sb("bt", [128, 1024], F32)
    wr_sb = P.sb("wr_sb", [128, 8, 32], F32)
    br_sb = P.sb("br_sb", [128, 32], F32)
    bgu_sb = P.sb("bgu_sb", [128, NE, 16], F32)
    bd_sb = P.sb("bd_sb", [128, 1024], F32)
    Gpad = P.sb("Gpad", [128, 128], F32)
    mix_sb = [P.sb(f"mix_sb{i}", [128, KC, 128], BF16) for i in range(2)]
    hin_sb = [P.sb(f"hin_sb{i}", [128, 1024], F32) for i in range(1)]
    rbuf = [P.sb(f"rbuf{i}", [128, 1024], F32) for i in range(1)]
    hm = [P.sb(f"hm{i}", [128, 1024], F32) for i in range(2)]
    hT32 = P.sb("hT32", [128, 8, 128], F32)
    st = P.sb("st", [128, 2, 6], F32)
    mv = P.sb("mv", [128, 2], F32)
    rs = P.sb("rs", [128, 1], F32)
    lg = P.sb("lg", [128, 32], F32)
    m8 = P.sb("m8", [128, 8], F32)
    msk = P.sb("msk", [128, 32], F32)
    nmx = P.sb("nmx", [128, 1], F32)
    ex = P.sb("ex", [128, 32], F32)
    den = P.sb("den", [128, 1], F32)
    GT = P.sb("GT", [128, 128], F32)
    gbuf = [P.sb(f"gbuf{i}", [128, 512], F32) for i in range(2)]
    sgbuf = [P.sb(f"sgbuf{i}", [128, 512], F32) for i in range(2)]
    tbuf = [P.sb(f"tbuf{i}", [128, 512], F32) for i in range(2)]
    actT = [P.sb(f"actT{i}", [128, 2, 512], BF16) for i in range(2)]
    ev = [P.sb(f"ev{i}", [128, 512], F32) for i in range(2)]
    pb = P.banks()

    P.dma("sp", idt[:, :], idn[:, :])
    P.dma("sp", gt[:, :], lng.v(lng.h[0:1, :].partition_broadcast(128)))
    P.dma("sp", bt[:, :], lnb.v(lnb.h[0:1, :].partition_broadcast(128)))
    P.dma("sp", wr_sb[:, :, :], w_r.v(w_r.h.rearrange("(c p) n -> p c n", p=128)))
    P.dma("sp", br_sb[:, :], b_r.v(b_r.h[0:1, :].partition_broadcast(128)))
    P.dma("sp", bgu_sb[:, :, :], b_gu[:, :, :])
    P.I("pool", "memset", ap=bd_sb[:, :], constant=0.0)
    P.I("pool", "memset", ap=Gpad[:, :], constant=0.0)
    P.dma("sp", bd_sb[0:32, :], b_d[:, :])
    wout_v = wout_sb.v(arena_h[:, 0:KC * 1024].rearrange("p (c n) -> p c n", c=KC))
    for c0 in range(0, KC, 4):
        P.dma("pool", wout_sb.v(arena_h[:, c0 * 1024:(c0 + 4) * 1024].rearrange("p (c n) -> p c n", c=4)),
              w_out.v(w_out.h[c0 * 128:(c0 + 4) * 128, :].rearrange("(c p) n -> p c n", p=128)))
    if glu:
        wglu_v = wout_sb.v(arena_h[:, 8192:16384].rearrange("p (c n) -> p c n", c=8))
        for c0 in range(0, 8, 4):
            P.dma("pool", wout_sb.v(arena_h[:, 8192 + c0 * 1024:8192 + (c0 + 4) * 1024].rearrange("p (c n) -> p c n", c=4)),
                  w_glu.v(w_glu.h[c0 * 128:(c0 + 4) * 128, :].rearrange("(c p) n -> p c n", p=128)))
        bglu_sb = P.sb("bglu_sb", [128, 8], F32)
        P.dma("sp", bglu_sb[:, :], b_glu[:, :])
        sgl = P.sb("sgl", [128, 128], F32)
        gms = [P.sb(f"gms{i}", [128, 8, 128], BF16) for i in range(1)]
    P.I("dve", "tensor_scalar", out=bgu_sb[:, :, 8:16], in0=bgu_sb[:, :, 8:16], scalar1=1.0, scalar2=None,
        op0=ALU.add)

    for i in range(NT):
        ms, hs, rb, hmt = mix_sb[i % 2], hin_sb[0], rbuf[0], hm[i % 2]
        tok = slice(i * 128, (i + 1) * 128)
        def mix_src(e, i=i):
            if "hb" not in P.dyn:
                P.dyn["hb"] = e.snap(e.partition_id() % 4, min_val=0, max_val=3)
            key = ("off", i // 4)
            if key not in P.dyn:
                P.dyn[key] = e.snap((P.dyn["hb"] * 4 + i // 4) * 2048, min_val=0, max_val=15 * 2048)
            return oT_all.h[bass.ds(P.dyn[key], Dm), (i % 4) * 128:(i % 4 + 1) * 128] \
                .rearrange("(c p) t -> p c t", p=128)

        P.dma("sp", ms[:, :, :], oT_all.v(mix_src))
        P.dma("sp", hs[:, :], hin[tok, :])
        if glu:
            gm = gms[0]
            for n in range(8):
                zps = pb[6 + n % 2][:, 0:128]
                for c in range(8):
                    P.mm(zps, wout_sb.v(wglu_v.ap[:, c, n * 128:(n + 1) * 128]), ms[:, c, :],
                         start=(c == 0), stop=(c == 7))
                P.I("act", "activation", out=sgl[:, :], in_=zps, func=AF.Sigmoid, bias=bglu_sb[:, n:n + 1], scale=1.0)
                P.I("dve", "tensor_tensor", out=gm[:, n, :], in0=sgl[:, :], in1=ms[:, n, :], op=ALU.mult)
            ms = gm
        for nh in range(2):
            for c in range(KC):
                P.mm(pb[nh][:, :], ms[:, c, :], wout_sb.v(wout_v.ap[:, c, nh * 512:(nh + 1) * 512]),
                     start=(c == 0), stop=(c == KC - 1))
            P.I("dve", "scalar_tensor_tensor", out=rb[:, nh * 512:(nh + 1) * 512],
                in0=hs[:, nh * 512:(nh + 1) * 512], scalar=ALPHA, in1=pb[nh][:, :],
                op0=ALU.mult, op1=ALU.add)
        layer_norm_tile(P, lambda s: rb[:, s], lambda s: hmt[:, s], gt, bt, st, mv, rs)
        if dbg == 1:
            P.dma("sp", hout[i * 128:(i + 1) * 128, :], hmt[:, :])
            continue
        P.I("act", "activation", out=acc[:, i, :], in_=hmt[:, :], func=AF.Copy, scale=ALPHA)
        for c in range(8):
            P.tp(pb[2 + c // 4][:, (c % 4) * 128:(c % 4 + 1) * 128], hmt[:, c * 128:(c + 1) * 128], idt[:, :])
        for half in range(2):
            src = pb[2 + half].v(pb[2 + half].h[:, :].rearrange("p (c n) -> p c n", c=4))
            P.I("act", "activation", out=hT32[:, half * 4:(half + 1) * 4, :], in_=src, func=AF.Copy)
            P.I("dve", "tensor_copy", out=xT[:, half * 4:(half + 1) * 4, tok], in_=hT32[:, half * 4:(half + 1) * 4, :])
        if dbg == 2:
            continue
        for c in range(8):
            P.mm(pb[4][:, 0:32], hT32[:, c, :], wr_sb[:, c, :], start=(c == 0), stop=(c == 7))
        P.I("dve", "tensor_tensor", out=lg[:, :], in0=pb[4][:, 0:32], in1=br_sb[:, :], op=ALU.add)
        P.I("dve", "max", out=m8[:, :], in_=lg[:, :])
        P.I("dve", "tensor_scalar", out=msk[:, :], in0=lg[:, :], scalar1=m8[:, 3:4], scalar2=None, op0=ALU.is_ge)
        P.I("dve", "tensor_scalar", out=nmx[:, :], in0=m8[:, 0:1], scalar1=-1.0, scalar2=None, op0=ALU.mult)
        P.I("act", "activation", out=ex[:, :], in_=lg[:, :], func=AF.Exp, bias=nmx[:, 0:1], scale=1.0)
        P.I("dve", "tensor_tensor", out=ex[:, :], in0=ex[:, :], in1=msk[:, :], op=ALU.mult)
        P.I("dve", "reduce_sum", out=den[:, :], in_=ex[:, :], axis=AX.X)
        P.I("dve", "reciprocal", out=den[:, :], in_=den[:, :])
        P.I("dve", "tensor_scalar", out=G[:, i, :], in0=ex[:, :], scalar1=den[:, 0:1], scalar2=None, op0=ALU.mult)
        if dbg == 3:
            continue
        P.I("dve", "tensor_copy", out=Gpad[:, 0:32], in_=G[:, i, :])
        P.tp(pb[5][:, 0:128], Gpad[:, :], idt[:, :])
        P.I("dve", "tensor_copy", out=GT[:, :], in_=pb[5][:, 0:128])
        for nh in range(2):
            P.mm(pb[6 + nh][:, :], GT[:, :], bd_sb[:, nh * 512:(nh + 1) * 512])
            P.I("dve", "tensor_tensor", out=acc[:, i, nh * 512:(nh + 1) * 512],
                in0=acc[:, i, nh * 512:(nh + 1) * 512], in1=pb[6 + nh][:, :], op=ALU.add)

    P.dma("sp", gt[:, :], lng.v(lng.h[1:2, :].partition_broadcast(128)))
    P.dma("sp", bt[:, :], lnb.v(lnb.h[1:2, :].partition_broadcast(128)))

    pieces = [(e, q) for e in range(n_exp) for q in range(NQ)]

    def load_piece(k):
        e, q = pieces[k]
        sl = slots[k % 3]
        gu_v = sl.v(arena_h[:, (k % 3) * PIECE:(k % 3) * PIECE + 4096].rearrange("p (c n) -> p c n", c=8))
        d_v = sl.v(arena_h[:, (k % 3) * PIECE + 4096:(k % 3 + 1) * PIECE].rearrange("p (c n) -> p c n", c=2))
        if NOLOAD_DBG and k >= 3:
            return gu_v, d_v
        P.dma("pool", sl.v(gu_v.ap[:, :, 0:256]),
              w_gu.v(w_gu.h[e, :, q * 256:(q + 1) * 256].rearrange("(c p) n -> p c n", p=128)))
        P.dma("pool", sl.v(gu_v.ap[:, :, 256:512]),
              w_gu.v(w_gu.h[e, :, 1024 + q * 256:1024 + (q + 1) * 256].rearrange("(c p) n -> p c n", p=128)))
        P.dma("pool", d_v, w_d.v(w_d.h[e, q * 256:(q + 1) * 256, :].rearrange("(c p) n -> p c n", p=128)))
        return gu_v, d_v

    views = {}
    for k0 in range(min(2, len(pieces))):
        views[k0] = load_piece(k0)
    it = 0
    evi = [0]

    def down_proj(at, sl, d_v, e, b):
        for tt in range(4):
            ti = b * 4 + tt
            for nh in range(2):
                yps = pb[4 + evi[0] % 4]
                evb = ev[evi[0] % 2]
                evi[0] += 1
                for jj in range(2):
                    P.mm(yps[:, :], at[:, jj, tt * 128:(tt + 1) * 128], sl.v(d_v.ap[:, jj, nh * 512:(nh + 1) * 512]),
                         start=(jj == 0), stop=(jj == 1))
                if (tt * 2 + nh) in POOL_EVAC:
                    P.I("act", "activation", out=evb[:, :], in_=yps[:, :], func=AF.Copy, scale=G[:, ti, e:e + 1])
                    P.I("dve", "tensor_tensor", out=acc[:, ti, nh * 512:(nh + 1) * 512],
                        in0=acc[:, ti, nh * 512:(nh + 1) * 512], in1=evb[:, :], op=ALU.add)
                else:
                    P.I("dve", "scalar_tensor_tensor", out=acc[:, ti, nh * 512:(nh + 1) * 512], in0=yps[:, :],
                        scalar=G[:, ti, e:e + 1], in1=acc[:, ti, nh * 512:(nh + 1) * 512],
                        op0=ALU.mult, op1=ALU.add)

    pending = None
    for k, (e, q) in enumerate(pieces):
        gu_v, d_v = views.pop(k)
        sl = slots[k % 3]
        for b in range(NB):
            at = actT[it % 2]
            tokb = slice(b * 512, (b + 1) * 512)
            for jj in range(2):
                gps, ups = pb[jj], pb[2 + jj]
                gb, sgb, tb = gbuf[jj], sgbuf[jj], tbuf[jj]
                for c in range(8):
                    P.mm(gps[:, :], sl.v(gu_v.ap[:, c, jj * 128:(jj + 1) * 128]), xT[:, c, tokb],
                         start=(c == 0), stop=(c == 7))
                for c in range(8):
                    P.mm(ups[:, :], sl.v(gu_v.ap[:, c, 256 + jj * 128:256 + (jj + 1) * 128]), xT[:, c, tokb],
                         start=(c == 0), stop=(c == 7))
                ch = q * 2 + jj
                P.I("dve", "tensor_scalar", out=gb[:, :], in0=gps[:, :], scalar1=bgu_sb[:, e, ch:ch + 1],
                    scalar2=7.0, op0=ALU.add, op1=ALU.min)
                P.I("act", "activation", out=sgb[:, :], in_=gb[:, :], func=AF.Sigmoid, scale=1.702)
                P.I("dve", "tensor_tensor", out=sgb[:, :], in0=sgb[:, :], in1=gb[:, :], op=ALU.mult)
                P.I("dve", "tensor_scalar", out=tb[:, :], in0=ups[:, :], scalar1=bgu_sb[:, e, 8 + ch:9 + ch],
                    scalar2=8.0, op0=ALU.add, op1=ALU.min)
                P.I("dve", "scalar_tensor_tensor", out=at[:, jj, :], in0=tb[:, :], scalar=-6.0, in1=sgb[:, :],
                    op0=ALU.max, op1=ALU.mult)
            if pending is not None:
                down_proj(*pending)
            pending = (at, sl, d_v, e, b)
            if b == 0 and k + 2 < len(pieces):
                views[k + 2] = load_piece(k + 2)
            it += 1
    if pending is not None:
        down_proj(*pending)

    for i in range(NT):
        ob = hm[i % 2]
        layer_norm_tile(P, lambda s: acc[:, i, s], lambda s: ob[:, s], gt, bt, st, mv, rs)
        P.dma("sp", hout[i * 128:(i + 1) * 128, :], ob[:, :])
        if hT_loc is not None:
            for c in range(8):
                P.tp(pb[2 + c // 4][:, (c % 4) * 128:(c % 4 + 1) * 128], ob[:, c * 128:(c + 1) * 128], idt[:, :])
            hb16 = mix_sb[i % 2]
            for half in range(2):
                src = pb[2 + half].v(pb[2 + half].h[:, :].rearrange("p (c n) -> p c n", c=4))
                P.I("act", "activation", out=hb16[:, half * 4:(half + 1) * 4, :], in_=src, func=AF.Copy)
            P.dma("sp", hT_loc.v(hT_loc.h[(i // 2) * 1024:(i // 2 + 1) * 1024, (i % 2) * 128:(i % 2 + 1) * 128]
                                 .rearrange("(c p) t -> p c t", p=128)), hb16[:, 0:8, :])


L1CFG = {"ml": dict(HPC=2, dk=64, dv=128), "ret": dict(HPC=2, dk=128, dv=256), "gla": dict(HPC=1, dk=128, dv=256)}
SEQ = 8192
NCH = SEQ // 128


def stage_l1(P, D, kind, xsrc, oT_loc, nch=NCH):
    cfg = L1CFG[kind]
    HPC, dk, dv = cfg["HPC"], cfg["dk"], cfg["dv"]
    dvx = dv + 1 if kind == "ml" else dv
    cscale = float(dk) ** -0.5
    nc = P.nc
    K_ = kind + "_"
    nq = 2 * HPC * dk if kind == "ret" else HPC * dk
    wq = D(K_ + "wq", [1024, nq], F32)
    wk = D(K_ + "wk", [1024, nq], F32)
    wv = D(K_ + "wv", [1024, HPC * dv], F32)
    wg = D(K_ + "wg", [1024, HPC * dv], F32)
    ng = D(K_ + "ng", [1, HPC * dv], F32)
    tri = D("tri", [128, 128], F32)
    idn = D("idn", [128, 128], F32)
    if kind == "ret":
        cosT = D("cosT", [128, SEQ], F32)
        sinT = D("sinT", [128, SEQ], F32)
        cosk = D("cosk", [SEQ, 128], F32)
        sink = D("sink", [SEQ, 128], F32)
        lgc = D("lgc", [128, HPC * 128], F32)
    if kind == "gla":
        wz = D(K_ + "wz", [1024, 16], F32)
        wga = D(K_ + "wga", [128, 128], F32)
    if kind == "ml":
        wgt = D(K_ + "wgt", [1024, 2 * HPC], F32)
        bgt = D(K_ + "bgt", [1, 2 * HPC], F32)

    idt = P.sb("idt", [128, 128], F32)
    P.dma("sp", idt[:, :], idn[:, :])
    wq_sb = P.sb("wq_sb", [128, 8, nq], BF16)
    wk_sb = P.sb("wk_sb", [128, 8, nq], BF16)
    wv_sb = P.sb("wv_sb", [128, 8, HPC * dv], BF16)
    wg_sb = P.sb("wg_sb", [128, 8, HPC * dv], BF16)
    ng_sb = P.sb("ng_sb", [128, HPC * dv], F32)
    tri_sb = P.sb("tri_sb", [128, 128], F32)
    for dst, src in ((wq_sb, wq), (wk_sb, wk), (wv_sb, wv), (wg_sb, wg)):
        P.dma("pool", dst[:, :, :], src.v(src.h.rearrange("(c p) n -> p c n", p=128)))
    P.dma("sp", ng_sb[:, :], ng.v(ng.h[0:1, :].partition_broadcast(128)))
    P.dma("sp", tri_sb[:, :], tri[:, :])
    lg = P.sb("lg", [128, 128], F32)
    if kind == "ret":
        lgc_sb = P.sb("lgc_sb", [128, HPC * 128], F32)
        P.dma("sp", lgc_sb[:, :], lgc[:, :])
        tabs = [[P.sb(f"tab{i}_{j}", [128, 128], F32) for j in range(4)] for i in range(2)]
    if kind == "gla":
        wz_sb = P.sb("wz_sb", [128, 8, 16], BF16)
        P.dma("pool", wz_sb[:, :, :], wz.v(wz.h.rearrange("(c p) n -> p c n", p=128)))
        wga_sb = P.sb("wga_sb", [128, 128], F32)
        P.dma("sp", wga_sb[:, :], wga[:, :])
        zaug = P.sb("zaug", [128, 128], F32)
        P.I("pool", "memset", ap=zaug[:, :], constant=1.0)
        esb = P.sb("esb", [128, 128], F32)
    if kind == "ml":
        wgt_sb = P.sb("wgt_sb", [128, 8, 2 * HPC], BF16)
        P.dma("pool", wgt_sb[:, :, :], wgt.v(wgt.h.rearrange("(c p) n -> p c n", p=128)))
        bgt_sb = P.sb("bgt_sb", [128, 2 * HPC], F32)
        P.dma("sp", bgt_sb[:, :], bgt.v(bgt.h[0:1, :].partition_broadcast(128)))
        gsb = P.sb("gsb", [128, 2 * HPC], F32)
        lf = P.sb("lf", [128, HPC], F32)
        ei = P.sb("ei", [128, HPC], F32)
        dd = P.sb("dd", [128, 1], F32)
    xc = [P.sb(f"xc{i}", [128, 8, 128], BF16) for i in range(2)]
    S = [P.sb(f"S{j}", [128, dvx], F32) for j in range(HPC)]
    Sbf = [P.sb(f"Sbf{j}", [128, dvx], BF16) for j in range(HPC)]
    for j in range(HPC):
        P.I("pool", "memset", ap=S[j][:, :], constant=0.0)
        P.I("pool", "memset", ap=Sbf[j][:, :], constant=0.0)
    eT = P.sb("eT", [128, 128], F32)
    enT = P.sb("enT", [128, 128], F32)
    ent = P.sb("ent", [128, 128], F32)
    tmp = P.sb("tmp", [128, 128], F32)
    qr = P.sb("qr", [128, 128], F32)
    A = P.sb("A", [128, 128], BF16)
    B = P.sb("B", [128, 128], BF16)
    C = P.sb("C", [128, 128], BF16)
    D = P.sb("D", [128, dvx], BF16)
    sT = P.sb("sT", [128, 128], BF16)
    hn = P.sb("hn", [128, dv], F32)
    gate = P.sb("gate", [128, dv], F32)
    st6 = P.sb("st6", [128, 6], F32)
    mv = P.sb("mv", [128, 2], F32)
    rs = P.sb("rs", [128, 1], F32)
    ob = [P.sb(f"ob{i}", [128, HPC * dv], F32) for i in range(2)]
    NBLK = HPC * dv // 128
    oT_sb = [P.sb(f"oT_sb{i}", [128, NBLK, 128], BF16) for i in range(2)]
    pb = P.banks()

    def proj_fm(dst, w_sb, col0, ncol, x):
        for c in range(8):
            P.mm(dst, w_sb[:, c, col0:col0 + ncol], x[:, c, :], start=(c == 0), stop=(c == 7))

    def proj_tm(dst, w_sb, col0, ncol, x):
        for c in range(8):
            P.mm(dst, x[:, c, :], w_sb[:, c, col0:col0 + ncol], start=(c == 0), stop=(c == 7))

    for ci in range(nch):
        x = xc[ci % 2]
        tok = slice(ci * 128, (ci + 1) * 128)
        xq, xv = xsrc(ci)
        P.dma(xq, x[:, :, :], xv)
        if kind == "ret":
            tb = tabs[ci % 2]
            P.dma("sp", tb[0][:, :], cosT[:, tok])
            P.dma("sp", tb[1][:, :], sinT[:, tok])
            P.dma("sp", tb[2][:, :], cosk[tok, :])
            P.dma("sp", tb[3][:, :], sink[tok, :])
        if kind == "ml":
            proj_tm(pb[7][:, 0:2 * HPC], wgt_sb, 0, 2 * HPC, x)
            P.I("dve", "tensor_tensor", out=gsb[:, :], in0=pb[7][:, 0:2 * HPC], in1=bgt_sb[:, :], op=ALU.add)
            P.I("act", "activation", out=ei[:, :], in_=gsb[:, 0:HPC], func=AF.Exp)
            P.I("act", "activation", out=lf[:, :], in_=gsb[:, HPC:2 * HPC], func=AF.Exp, scale=-1.0)
            P.I("act", "activation", out=lf[:, :], in_=lf[:, :], func=AF.Ln, bias=1.0)
            P.I("dve", "tensor_scalar", out=lf[:, :], in0=lf[:, :], scalar1=-1.0, scalar2=None, op0=ALU.mult)
        obuf = ob[ci % 2]
        for j in range(HPC):
            qT_ps, kT_ps = pb[0][0:dk, 0:128], pb[0][0:dk, 128:256]
            kt_ps = pb[1][:, 0:dk]
            vt_ps, gt_ps = pb[2][:, 0:dv], pb[2][:, 256:256 + dv]
            proj_fm(qT_ps, wq_sb, j * dk, dk, x)
            proj_fm(kT_ps, wk_sb, j * dk, dk, x)
            proj_tm(kt_ps, wk_sb, j * dk, dk, x)
            proj_tm(vt_ps, wv_sb, j * dv, dv, x)
            proj_tm(gt_ps, wg_sb, j * dv, dv, x)
            if kind == "ret":
                qsT_ps, ksT_ps = pb[0][0:dk, 256:384], pb[0][0:dk, 384:512]
                kst_ps = pb[1][:, 128:256]
                proj_fm(qsT_ps, wq_sb, (HPC + j) * dk, dk, x)
                proj_fm(ksT_ps, wk_sb, (HPC + j) * dk, dk, x)
                proj_tm(kst_ps, wk_sb, (HPC + j) * dk, dk, x)
                lgv = lgc_sb[:, j * 128:(j + 1) * 128]
            elif kind == "gla":
                proj_fm(pb[7][0:16, 0:128], wz_sb, 0, 16, x)
                P.I("dve", "tensor_copy", out=zaug[0:16, :], in_=pb[7][0:16, 0:128])
                P.mm(pb[7][:, 128:256], zaug[:, :], wga_sb[:, :])
                P.I("act", "activation", out=esb[:, :], in_=pb[7][:, 128:256], func=AF.Exp, scale=-1.0)
                P.I("act", "activation", out=esb[:, :], in_=esb[:, :], func=AF.Ln, bias=1.0)
                P.I("dve", "tensor_scalar", out=lg[:, :], in0=esb[:, :], scalar1=-1.0 / 16.0, scalar2=None,
                    op0=ALU.mult)
                lgv = lg[:, :]
            else:
                P.I("dve", "tensor_copy", out=lg[:, :], in_=lf.v(lf.h[:, j:j + 1].to_broadcast([128, 128])))
                lgv = lg[:, :]
            bT_ps, bt_ps = pb[3][:, 0:128], pb[3][:, 128:256]
            P.mm(bT_ps, lgv, tri_sb[:, :])
            P.mm(bt_ps, tri_sb[:, :], lgv)
            P.I("act", "activation", out=eT[:, :], in_=bT_ps, func=AF.Exp)
            P.I("act", "activation", out=enT[:, :], in_=bT_ps, func=AF.Exp, scale=-1.0)
            P.I("act", "activation", out=ent[:, :], in_=bt_ps, func=AF.Exp, scale=-1.0)
            if kind == "ret":
                P.I("dve", "tensor_tensor", out=tmp[:, :], in0=qsT_ps, in1=tb[1][:, :], op=ALU.mult)
                P.I("dve", "tensor_tensor", out=qr[:, :], in0=qT_ps, in1=tb[0][:, :], op=ALU.mult)
                P.I("dve", "tensor_tensor", out=qr[:, :], in0=qr[:, :], in1=tmp[:, :], op=ALU.add)
                P.I("dve", "tensor_tensor", out=A[:, :], in0=qr[:, :], in1=eT[:, :], op=ALU.mult)
                P.I("dve", "tensor_tensor", out=tmp[:, :], in0=ksT_ps, in1=tb[1][:, :], op=ALU.mult)
                P.I("dve", "tensor_tensor", out=qr[:, :], in0=kT_ps, in1=tb[0][:, :], op=ALU.mult)
                P.I("dve", "tensor_tensor", out=qr[:, :], in0=qr[:, :], in1=tmp[:, :], op=ALU.add)
                P.I("dve", "scalar_tensor_tensor", out=B[:, :], in0=qr[:, :], scalar=cscale, in1=enT[:, :],
                    op0=ALU.mult, op1=ALU.mult)
                P.I("dve", "tensor_tensor", out=tmp[:, :], in0=kst_ps, in1=tb[3][:, :], op=ALU.mult)
                P.I("dve", "tensor_tensor", out=qr[:, :], in0=kt_ps, in1=tb[2][:, :], op=ALU.mult)
                P.I("dve", "tensor_tensor", out=qr[:, :], in0=qr[:, :], in1=tmp[:, :], op=ALU.add)
                P.I("dve", "scalar_tensor_tensor", out=C[:, :], in0=qr[:, :], scalar=cscale, in1=ent[:, :],
                    op0=ALU.mult, op1=ALU.mult)
            else:
                P.I("dve", "tensor_tensor", out=A[0:dk, :], in0=qT_ps, in1=eT[0:dk, :], op=ALU.mult)
                P.I("dve", "scalar_tensor_tensor", out=B[0:dk, :], in0=kT_ps, scalar=cscale, in1=enT[0:dk, :],
                    op0=ALU.mult, op1=ALU.mult)
                P.I("dve", "scalar_tensor_tensor", out=C[:, 0:dk], in0=kt_ps, scalar=cscale, in1=ent[:, 0:dk],
                    op0=ALU.mult, op1=ALU.mult)
            if kind == "ml":
                P.I("dve", "tensor_scalar", out=D[:, 0:dv], in0=vt_ps, scalar1=ei[:, j:j + 1], scalar2=None,
                    op0=ALU.mult)
                P.I("dve", "tensor_copy", out=D[:, dv:dv + 1], in_=ei[:, j:j + 1])
            else:
                P.I("act", "activation", out=D[:, :], in_=vt_ps, func=AF.Copy)
            sT_ps = pb[4][:, 0:128]
            P.mm(sT_ps, B[0:dk, :], A[0:dk, :])
            P.I("dve", "tensor_tensor", out=sT[:, :], in0=sT_ps, in1=tri_sb[:, :], op=ALU.mult)
            o_ps = pb[5][:, 0:dvx]
            P.mm(o_ps, sT[:, :], D[:, :], start=True, stop=False)
            P.mm(o_ps, A[0:dk, :], Sbf[j][0:dk, :], start=False, stop=True)
            U_ps = pb[6][0:dk, 0:dvx]
            P.mm(U_ps, C[:, 0:dk], D[:, :])
            P.I("dve", "tensor_scalar", out=S[j][0:dk, :], in0=S[j][0:dk, :], scalar1=eT[0:dk, 127:128],
                scalar2=None, op0=ALU.mult)
            P.I("dve", "scalar_tensor_tensor", out=S[j][0:dk, :], in0=U_ps, scalar=eT[0:dk, 127:128],
                in1=S[j][0:dk, :], op0=ALU.mult, op1=ALU.add)
            P.I("dve", "tensor_copy", out=Sbf[j][0:dk, :], in_=S[j][0:dk, :])
            if kind == "ml":
                P.I("act", "activation", out=dd[:, :], in_=pb[5][:, dv:dv + 1], func=AF.Abs)
                P.I("dve", "tensor_scalar", out=dd[:, :], in0=dd[:, :], scalar1=1.0, scalar2=None, op0=ALU.max)
                P.I("dve", "reciprocal", out=dd[:, :], in_=dd[:, :])
                P.I("dve", "tensor_scalar", out=hn[:, :], in0=pb[5][:, 0:dv], scalar1=dd[:, 0:1], scalar2=None,
                    op0=ALU.mult)
                P.I("act", "activation", out=gate[:, :], in_=gt_ps, func=AF.Sigmoid)
            else:
                P.I("dve", "tensor_copy", out=hn[:, :], in_=pb[5][:, 0:dv])
                P.I("act", "activation", out=gate[:, :], in_=gt_ps, func=AF.Silu)
            P.I("dve", "bn_stats", out=st6[:, :], in_=hn[:, :])
            P.I("dve", "bn_aggr", out=mv[:, :], in_=st6[:, :])
            P.I("act", "activation", out=rs[:, :], in_=mv[:, 1:2], func=AF.Sqrt, bias=EPS, scale=1.0)
            P.I("dve", "reciprocal", out=rs[:, :], in_=rs[:, :])
            P.I("dve", "tensor_scalar", out=hn[:, :], in0=hn[:, :], scalar1=mv[:, 0:1], scalar2=rs[:, 0:1],
                op0=ALU.subtract, op1=ALU.mult)
            P.I("dve", "tensor_tensor", out=hn[:, :], in0=hn[:, :], in1=ng_sb[:, j * dv:(j + 1) * dv], op=ALU.mult)
            P.I("dve", "tensor_tensor", out=obuf[:, j * dv:(j + 1) * dv], in0=hn[:, :], in1=gate[:, :], op=ALU.mult)
        otb = oT_sb[ci % 2]
        for blk in range(NBLK):
            P.tp(pb[7][:, blk * 128:(blk + 1) * 128], obuf[:, blk * 128:(blk + 1) * 128], idt[:, :])
        P.I("act", "activation", out=otb[:, :, :],
            in_=pb[7].v(pb[7].h[:, 0:NBLK * 128].rearrange("p (c n) -> p c n", c=NBLK)), func=AF.Copy)
        R_ = HPC * dv
        P.dma("sp", oT_loc.v(oT_loc.h[(ci // 4) * R_:(ci // 4 + 1) * R_, (ci % 4) * 128:(ci % 4 + 1) * 128]
                             .rearrange("(c p) t -> p c t", p=128)), otb[:, :, :])


def _c(a):
    return np.ascontiguousarray(a)


def _tri():
    return np.triu(np.ones((128, 128), np.float32))


def l1_in_maps(kind, h, inp, j):
    cfg = L1CFG[kind]
    HPC, dk, dv = cfg["HPC"], cfg["dk"], cfg["dv"]
    maps = []
    for core in range(NCORES):
        b, hb = core // 4, core % 4
        heads = [hb * HPC + i for i in range(HPC)]
        m = {"xT": _c(h[b].T), "tri": _tri()}
        if kind == "ml":
            w = inp["ml_w_in"][j]
            m["wq"] = _c(np.concatenate([w[:, hd * 64:(hd + 1) * 64] for hd in heads], 1))
            m["wk"] = _c(np.concatenate([w[:, 512 + hd * 64:512 + (hd + 1) * 64] for hd in heads], 1))
            m["wv"] = _c(np.concatenate([w[:, 1024 + hd * 128:1024 + (hd + 1) * 128] for hd in heads], 1))
            m["wg"] = _c(np.concatenate([w[:, 2048 + hd * 128:2048 + (hd + 1) * 128] for hd in heads], 1))
            gi = [3072 + hd for hd in heads] + [3080 + hd for hd in heads]
            m["wgt"] = _c(w[:, gi])
            m["bgt"] = _c(inp["ml_b_gates"][j][[hd for hd in heads] + [8 + hd for hd in heads]][None, :])
            m["ng"] = _c(np.concatenate([inp["ml_norm_g"][j][hd * 128:(hd + 1) * 128] for hd in heads])[None, :])
        elif kind == "ret":
            w = inp["ret_w_in"][j]

            def sw(c0):
                return np.concatenate([w[:, c0 + 64:c0 + 128], w[:, c0:c0 + 64]], 1)
            m["wq"] = _c(np.concatenate([w[:, hd * 128:(hd + 1) * 128] for hd in heads] + [sw(hd * 128) for hd in heads], 1))
            m["wk"] = _c(np.concatenate([w[:, 1024 + hd * 128:1024 + (hd + 1) * 128] for hd in heads]
                                        + [sw(1024 + hd * 128) for hd in heads], 1))
            m["wv"] = _c(np.concatenate([w[:, 2048 + hd * 256:2048 + (hd + 1) * 256] for hd in heads], 1))
            m["wg"] = _c(np.concatenate([w[:, 4096 + hd * 256:4096 + (hd + 1) * 256] for hd in heads], 1))
            m["ng"] = _c(np.concatenate([inp["ret_norm_g"][j][hd * 256:(hd + 1) * 256] for hd in heads])[None, :])
            inv = (10000.0 ** (-np.arange(0, 128, 2, dtype=np.float32) / 128.0)).astype(np.float32)
            ang = np.arange(SEQ, dtype=np.float32)[:, None] * inv[None, :]
            cos, sin = np.cos(ang).astype(np.float32), np.sin(ang).astype(np.float32)
            cosk = np.concatenate([cos, cos], 1)
            sink = np.concatenate([-sin, sin], 1)
            m["cosk"], m["sink"] = _c(cosk), _c(sink)
            m["cosT"], m["sinT"] = _c(cosk.T), _c(sink.T)
            lgam = np.log1p(-(2.0 ** (-5.0 - np.arange(8, dtype=np.float32)))).astype(np.float32)
            m["lgc"] = _c(np.concatenate([np.full((128, 128), lgam[hd], np.float32) for hd in heads], 1))
        else:
            w = inp["gla_w_in"][j]
            hd = heads[0]
            m["wq"] = _c(w[:, hd * 128:(hd + 1) * 128])
            m["wk"] = _c(w[:, 512 + hd * 128:512 + (hd + 1) * 128])
            m["wv"] = _c(w[:, 1024 + hd * 256:1024 + (hd + 1) * 256])
            m["wg"] = _c(w[:, 2048 + hd * 256:2048 + (hd + 1) * 256])
            m["wz"] = _c(w[:, 3072:3088])
            wga = np.zeros((128, 128), np.float32)
            wga[0:16] = inp["gla_w_gate"][j][:, hd * 128:(hd + 1) * 128]
            wga[16] = inp["gla_b_gate"][j][hd * 128:(hd + 1) * 128]
            m["wga"] = wga
            m["ng"] = _c(inp["gla_norm_g"][j][hd * 256:(hd + 1) * 256][None, :])
        maps.append(m)
    return maps


def l1_gather(kind, results):
    outs = []
    for b in range(2):
        outs.append(np.concatenate([np.asarray(results[b * 4 + hb]["o"]) for hb in range(4)], 1))
    return np.stack(outs, 0)


def stage_s5(P, D, xsrc, yT, T=SEQ):
    nc = P.nc
    NBK = T // 512
    NK = int(np.log2(T))
    w_in = D("s5_w_in", [1024, 256], F32)
    lre = D("s5_lre", [128, 8], F32)
    lim = D("s5_lim", [128, 8], F32)
    ldt = D("s5_ldt", [128, 8], F32)
    bbr = D("s5_bbr", [32, 8, 128], F32)
    bbi = D("s5_bbi", [32, 8, 128], F32)
    ccr = D("s5_ccr", [128, 8, 32], F32)
    cci = D("s5_cci", [128, 8, 32], F32)
    ddg = D("s5_ddg", [32, 8, 32], F32)

    w_sb = P.sb("w_sb", [128, 8, 256], BF16)
    P.dma("pool", w_sb[:, :, :], w_in.v(w_in.h.rearrange("(c p) n -> p c n", p=128)))
    bbr_sb = P.sb("bbr_sb", [32, 8, 128], BF16)
    bbi_sb = P.sb("bbi_sb", [32, 8, 128], BF16)
    ddg_sb = P.sb("ddg_sb", [32, 8, 32], BF16)
    P.dma("pool", bbr_sb[:, :, :], bbr[:, :, :])
    P.dma("pool", bbi_sb[:, :, :], bbi[:, :, :])
    P.dma("pool", ddg_sb[:, :, :], ddg[:, :, :])
    ccr_sb = P.sb("ccr_sb", [128, 8, 32], F32)
    cci_sb = P.sb("cci_sb", [128, 8, 32], F32)
    P.dma("sp", ccr_sb[:, :, :], ccr[:, :, :])
    P.dma("sp", cci_sb[:, :, :], cci[:, :, :])
    P.I("dve", "tensor_scalar", out=cci_sb[:, :, :], in0=cci_sb[:, :, :], scalar1=-1.0, scalar2=None, op0=ALU.mult)

    def small(name, n=8):
        return P.sb(name, [128, n], F32)

    lr, li, dt = small("lr"), small("li"), small("dt")
    P.dma("sp", lr[:, :], lre[:, :])
    P.dma("sp", li[:, :], lim[:, :])
    P.dma("sp", dt[:, :], ldt[:, :])
    P.I("act", "activation", out=dt[:, :], in_=dt[:, :], func=AF.Exp)
    rr, th, cs, sn, t1, t2 = small("rr"), small("th"), small("cs"), small("sn"), small("t1"), small("t2")

    def tt(out, a, b, op, eng="dve"):
        P.I(eng, "tensor_tensor", out=out, in0=a, in1=b, op=op)

    tt(rr[:, :], lr[:, :], dt[:, :], ALU.mult)
    P.I("act", "activation", out=rr[:, :], in_=rr[:, :], func=AF.Exp)
    tt(th[:, :], li[:, :], dt[:, :], ALU.mult)
    P.I("act", "activation", out=sn[:, :], in_=th[:, :], func=AF.Sin, scale=1.0 / 16.0)
    hp = small("hp", 1)
    P.I("pool", "memset", ap=hp[:, :], constant=float(np.pi / 2))
    P.I("act", "activation", out=cs[:, :], in_=th[:, :], func=AF.Sin, scale=1.0 / 16.0, bias=hp[:, 0:1])

    def csq(c, s):
        tt(t1[:, :], c, c, ALU.mult)
        tt(t2[:, :], s, s, ALU.mult)
        tt(s, c, s, ALU.mult)
        P.I("dve", "tensor_scalar", out=s, in0=s, scalar1=2.0, scalar2=None, op0=ALU.mult)
        tt(c, t1[:, :], t2[:, :], ALU.subtract)

    for _ in range(4):
        csq(cs[:, :], sn[:, :])
    ar = P.sb("ar", [128, NK, 8], F32)
    ai = P.sb("ai", [128, NK, 8], F32)
    nai = P.sb("nai", [128, NK, 8], F32)
    tt(ar[:, 0, :], rr[:, :], cs[:, :], ALU.mult)
    tt(ai[:, 0, :], rr[:, :], sn[:, :], ALU.mult)
    for k in range(1, NK):
        tt(t1[:, :], ar[:, k - 1, :], ar[:, k - 1, :], ALU.mult)
        tt(t2[:, :], ai[:, k - 1, :], ai[:, k - 1, :], ALU.mult)
        tt(ar[:, k, :], t1[:, :], t2[:, :], ALU.subtract)
        tt(t1[:, :], ar[:, k - 1, :], ai[:, k - 1, :], ALU.mult)
        P.I("dve", "tensor_scalar", out=ai[:, k, :], in0=t1[:, :], scalar1=2.0, scalar2=None, op0=ALU.mult)
    P.I("dve", "tensor_scalar", out=nai[:, :, :], in0=ai[:, :, :], scalar1=-1.0, scalar2=None, op0=ALU.mult)
    cr, ci, nci, m2 = small("cr"), small("ci"), small("nci"), small("m2")
    am1 = small("am1")
    P.I("dve", "tensor_scalar", out=am1[:, :], in0=ar[:, 0, :], scalar1=-1.0, scalar2=None, op0=ALU.add)
    tt(t1[:, :], lr[:, :], lr[:, :], ALU.mult)
    tt(t2[:, :], li[:, :], li[:, :], ALU.mult)
    tt(m2[:, :], t1[:, :], t2[:, :], ALU.add)
    P.I("dve", "reciprocal", out=m2[:, :], in_=m2[:, :])
    tt(t1[:, :], am1[:, :], lr[:, :], ALU.mult)
    tt(t2[:, :], ai[:, 0, :], li[:, :], ALU.mult)
    tt(cr[:, :], t1[:, :], t2[:, :], ALU.add)
    tt(cr[:, :], cr[:, :], m2[:, :], ALU.mult)
    tt(t1[:, :], ai[:, 0, :], lr[:, :], ALU.mult)
    tt(t2[:, :], am1[:, :], li[:, :], ALU.mult)
    tt(ci[:, :], t1[:, :], t2[:, :], ALU.subtract)
    tt(ci[:, :], ci[:, :], m2[:, :], ALU.mult)
    P.I("dve", "tensor_scalar", out=nci[:, :], in0=ci[:, :], scalar1=-1.0, scalar2=None, op0=ALU.mult)

    X = [[P.sb(f"X{a}{b}", [128, T], F32) for b in range(2)] for a in range(2)]
    uT = P.sb("uT", [32, T], BF16)
    xc = [P.sb(f"xc{i}", [128, 8, 512], BF16) for i in range(2)]
    g1 = [P.sb(f"g1_{i}", [32, 512], F32) for i in range(2)]
    g2 = [P.sb(f"g2_{i}", [32, 512], F32) for i in range(2)]
    yo = [P.sb(f"yo{i}", [32, 512], BF16) for i in range(2)]
    pb = P.banks()

    it = 0
    for pp in range(8):
        cur = X[0]
        for tb in range(NBK):
            x = xc[it % 2]
            tok = slice(tb * 512, (tb + 1) * 512)
            xsrc(tb, x)
            ups = pb[it % 2][0:32, :]
            for c in range(8):
                P.mm(ups, w_sb[:, c, pp * 32:(pp + 1) * 32], x[:, c, :], start=(c == 0), stop=(c == 7))
            P.I("act", "activation", out=uT[:, tok], in_=ups, func=AF.Copy)
            br_ps, bi_ps = pb[2 + it % 2], pb[4 + it % 2]
            P.mm(br_ps[:, :], bbr_sb[:, pp, :], uT[:, tok])
            P.mm(bi_ps[:, :], bbi_sb[:, pp, :], uT[:, tok])
            P.I("dve", "tensor_scalar", out=cur[0][:, tok], in0=br_ps[:, :], scalar1=cr[:, pp:pp + 1], scalar2=None,
                op0=ALU.mult)
            P.I("dve", "scalar_tensor_tensor", out=cur[0][:, tok], in0=bi_ps[:, :], scalar=nci[:, pp:pp + 1],
                in1=cur[0][:, tok], op0=ALU.mult, op1=ALU.add)
            P.I("dve", "tensor_scalar", out=cur[1][:, tok], in0=bi_ps[:, :], scalar1=cr[:, pp:pp + 1], scalar2=None,
                op0=ALU.mult)
            P.I("dve", "scalar_tensor_tensor", out=cur[1][:, tok], in0=br_ps[:, :], scalar=ci[:, pp:pp + 1],
                in1=cur[1][:, tok], op0=ALU.mult, op1=ALU.add)
            it += 1
        src_i = 0
        for k in range(NK):
            d = 1 << k
            s, o2 = X[src_i], X[1 - src_i]
            a_r, a_i, na_i = ar[:, k, pp:pp + 1], ai[:, k, pp:pp + 1], nai[:, k, pp:pp + 1]
            P.I("act", "activation", out=o2[0][:, 0:d], in_=s[0][:, 0:d], func=AF.Copy)
            P.I("act", "activation", out=o2[1][:, 0:d], in_=s[1][:, 0:d], func=AF.Copy)
            P.I("dve", "scalar_tensor_tensor", out=o2[0][:, d:T], in0=s[0][:, 0:T - d], scalar=a_r, in1=s[0][:, d:T],
                op0=ALU.mult, op1=ALU.add)
            P.I("dve", "scalar_tensor_tensor", out=o2[0][:, d:T], in0=s[1][:, 0:T - d], scalar=na_i, in1=o2[0][:, d:T],
                op0=ALU.mult, op1=ALU.add)
            P.I("dve", "scalar_tensor_tensor", out=o2[1][:, d:T], in0=s[1][:, 0:T - d], scalar=a_r, in1=s[1][:, d:T],
                op0=ALU.mult, op1=ALU.add)
            P.I("dve", "scalar_tensor_tensor", out=o2[1][:, d:T], in0=s[0][:, 0:T - d], scalar=a_i, in1=o2[1][:, d:T],
                op0=ALU.mult, op1=ALU.add)
            src_i = 1 - src_i
        fin = X[src_i]
        for tb in range(NBK):
            tok = slice(tb * 512, (tb + 1) * 512)
            yps = pb[6 + tb % 2][0:32, :]
            P.mm(yps, ccr_sb[:, pp, :], fin[0][:, tok], start=True, stop=False)
            P.mm(yps, cci_sb[:, pp, :], fin[1][:, tok], start=False, stop=True)
            dps = pb[tb % 2][0:32, :]
            P.mm(dps, ddg_sb[:, pp, :], uT[:, tok])
            a1, a2, yb = g1[tb % 2], g2[tb % 2], yo[tb % 2]
            P.I("act", "activation", out=a1[:, :], in_=yps, func=AF.Copy)
            P.I("dve", "tensor_tensor", out=a1[:, :], in0=a1[:, :], in1=dps, op=ALU.add)
            P.I("dve", "tensor_tensor", out=a2[:, :], in0=a1[:, :], in1=a1[:, :], op=ALU.mult)
            P.I("dve", "tensor_scalar", out=a2[:, :], in0=a2[:, :], scalar1=0.044715, scalar2=1.0,
                op0=ALU.mult, op1=ALU.add)
            P.I("dve", "tensor_tensor", out=a2[:, :], in0=a2[:, :], in1=a1[:, :], op=ALU.mult)
            P.I("act", "activation", out=a2[:, :], in_=a2[:, :], func=AF.Tanh, scale=0.7978845608028654)
            P.I("dve", "tensor_scalar", out=a2[:, :], in0=a2[:, :], scalar1=1.0, scalar2=0.5,
                op0=ALU.add, op1=ALU.mult)
            P.I("dve", "tensor_tensor", out=yb[:, :], in0=a2[:, :], in1=a1[:, :], op=ALU.mult)
            P.dma("sp", yT[tb * 256 + pp * 32:tb * 256 + (pp + 1) * 32, :], yb[:, :])


def s5_in_maps(h, inp, j):
    maps = []
    for core in range(NCORES):
        b, hb = core // 4, core % 4
        g0 = hb * 16
        m = {"xT": _c(h[b].T), "w_in": _c(inp["s5_w_in"][j][:, g0 * 16:(g0 + 16) * 16])}
        lre = inp["s5_lam_re"][j][g0:g0 + 16]
        lim = inp["s5_lam_im"][j][g0:g0 + 16]
        ldt = np.repeat(inp["s5_log_dt"][j][g0:g0 + 16][:, None], 64, 1)

        def lay(a):
            return _c(a.reshape(8, 2, 64).transpose(1, 2, 0).reshape(128, 8))
        m["lre"], m["lim"], m["ldt"] = lay(lre), lay(lim), lay(ldt)
        bre = inp["s5_b_re"][j][g0:g0 + 16]
        bim = inp["s5_b_im"][j][g0:g0 + 16]
        cre = inp["s5_c_re"][j][g0:g0 + 16]
        cim = inp["s5_c_im"][j][g0:g0 + 16]
        dsk = inp["s5_d"][j][g0 * 16:(g0 + 16) * 16]
        bbr = np.zeros((32, 8, 128), np.float32)
        bbi = np.zeros((32, 8, 128), np.float32)
        ccr = np.zeros((128, 8, 32), np.float32)
        cci = np.zeros((128, 8, 32), np.float32)
        ddg = np.zeros((32, 8, 32), np.float32)
        for pp in range(8):
            for g2 in range(2):
                g = pp * 2 + g2
                bbr[g2 * 16:(g2 + 1) * 16, pp, g2 * 64:(g2 + 1) * 64] = bre[g].T
                bbi[g2 * 16:(g2 + 1) * 16, pp, g2 * 64:(g2 + 1) * 64] = bim[g].T
                ccr[g2 * 64:(g2 + 1) * 64, pp, g2 * 16:(g2 + 1) * 16] = cre[g].T
                cci[g2 * 64:(g2 + 1) * 64, pp, g2 * 16:(g2 + 1) * 16] = cim[g].T
            idx = np.arange(32)
            ddg[idx, pp, idx] = dsk[pp * 32:(pp + 1) * 32]
        m.update(bbr=bbr, bbi=bbi, ccr=ccr, cci=cci, ddg=ddg)
        maps.append(m)
    return maps


def s5_gather(results):
    outs = []
    for b in range(2):
        yT = np.concatenate([np.asarray(results[b * 4 + hb]["yT"]) for hb in range(4)], 0)
        outs.append(yT.T)
    return np.stack(outs, 0)


KINDS = ("ml", "ret", "gla", "s5")


def build_fused(n_exp=NE, nlayers=4, stop=0):
    nc = bass.Bass("TRN2", target_bir_lowering=False)
    P = Prog(nc)
    ext = {}

    def D(name, shape, dtype):
        if name not in ext:
            ext[name] = P.dram(name, shape, dtype, kind="ExternalInput")
        return ext[name]

    xT0 = D("xT0", [1024, SEQ], F32)
    hin0 = D("hin0", [2048, 1024], F32)
    hout = P.dram("hout", [2048, 1024], F32, kind="ExternalOutput")
    h_loc = [P.dram(f"h_loc{i}", [2048, 1024], F32) for i in range(2)]
    hT_loc = P.dram("hT_loc", [8 * 1024, 256], BF16)
    hT_all = P.dram("hT_all", [8 * 4096, 256], BF16)

    def hT_src(t0, n):
        r, tl = t0 // 2048, t0 % 2048
        k, col = tl // 256, tl % 256
        return hT_all.v(hT_all.h[k * 4096 + r * 1024:k * 4096 + (r + 1) * 1024, col:col + n]
                        .rearrange("(c p) t -> p c t", p=128))

    for layer in range(nlayers):
        kind = KINDS[layer % 4]
        rows = 256 if kind == "s5" else L1CFG[kind]["HPC"] * L1CFG[kind]["dv"]
        oT_loc = P.dram(f"oT_loc{layer}", [16 * rows, 512], BF16)
        oT_all = P.dram(f"oT_all{layer}", [16 * 2048, 512], BF16)
        P.sb_reset()
        if kind == "s5":
            def xsrc(tb, x):
                for hh in range(2):
                    P.dma("sp", x[:, :, hh * 256:(hh + 1) * 256], hT_src(tb * 512 + hh * 256, 256))
            stage_s5(P, D, xsrc, oT_loc)
        else:
            if layer == 0:
                def xsrc(ci):
                    return "pool", xT0.v(xT0.h[:, ci * 128:(ci + 1) * 128].rearrange("(c p) t -> p c t", p=128))
            else:
                def xsrc(ci):
                    return "sp", hT_src(ci * 128, 128)
            stage_l1(P, D, kind, xsrc, oT_loc)
        if stop == 10 * layer + 1:
            break
        for k in range(16):
            P.coll("AllGather", oT_all.v(oT_all.h[k * 2048:k * 2048 + 4 * rows, :]),
                   oT_loc.v(oT_loc.h[k * rows:(k + 1) * rows, :]), GROUPS)
        P.barrier()
        P.new_epoch()
        if stop == 10 * layer + 2:
            break
        P.sb_reset()
        last = layer == nlayers - 1
        hin = hin0 if layer == 0 else h_loc[(layer - 1) % 2]
        ho = hout if last else h_loc[layer % 2]
        stage_l2(P, D, layer, 4 * rows // 128, oT_all, hin, ho, None if last else hT_loc, n_exp=n_exp,
                 glu=(kind == "s5"))
        if stop == 10 * layer + 3:
            break
        if not last:
            for k in range(8):
                P.coll("AllGather", hT_all.v(hT_all.h[k * 4096:(k + 1) * 4096, :]),
                       hT_loc.v(hT_loc.h[k * 1024:(k + 1) * 1024, :]), GROUPS)
        P.barrier()
        P.new_epoch()
        if stop == 10 * layer + 4:
            break
    P.emit()
    return nc, list(ext.keys())


def fused_in_maps(inp, names, n_exp=NE):
    x = np.asarray(inp["x"], np.float32)
    xf = x.reshape(-1, 1024)
    shared = {"tri": _tri(), "idn": np.eye(128, dtype=np.float32)}
    for layer in range(4):
        L = f"_{layer}"
        kind = KINDS[layer % 4]
        j = layer // 4
        shared["w_out" + L] = _c(inp[{"ml": "ml_w_out", "ret": "ret_w_out", "gla": "gla_w_out", "s5": "s5_w_out"}[kind]][j])
        shared["lng" + L] = _c(inp["ln_g"][layer])
        shared["lnb" + L] = _c(inp["ln_b"][layer])
        shared["w_r" + L] = _c(inp["moe_w_router"][layer])
        shared["b_r" + L] = _c(inp["moe_b_router"][layer][None, :])
        shared["w_gu" + L] = _c(inp["moe_w_gate_up"][layer][:max(n_exp, 1)])
        shared["b_gu" + L] = _c(inp["moe_b_gate_up"][layer].reshape(32, 16, 128).transpose(2, 0, 1))
        shared["w_d" + L] = _c(inp["moe_w_down"][layer][:max(n_exp, 1)])
        shared["b_d" + L] = _c(inp["moe_b_down"][layer])
        if kind == "s5":
            shared["w_glu" + L] = _c(inp["s5_w_glu"][j])
            shared["b_glu" + L] = _c(inp["s5_b_glu"][j].reshape(8, 128).T)
    per_kind = {k: l1_in_maps(k, x, inp, 0) for k in ("ml", "ret", "gla")}
    s5m = s5_in_maps(x, inp, 0)
    maps = []
    for c in range(NCORES):
        b = c // 4
        m = dict(shared)
        m["xT0"] = _c(x[b].T)
        m["hin0"] = _c(xf[c * 2048:(c + 1) * 2048])
        for k in ("ml", "ret", "gla"):
            for key, val in per_kind[k][c].items():
                if key in ("xT", "tri"):
                    continue
                m[key if key in ("cosT", "sinT", "cosk", "sink", "lgc") else k + "_" + key] = val
        for key, val in s5m[c].items():
            if key != "xT":
                m["s5_" + key] = val
        maps.append({k: m[k] for k in names})
    return maps


_FUSED = {}


def kernel(**inp):
    inp = {k: np.asarray(v) for k, v in inp.items()}
    if "p" not in _FUSED:
        _FUSED["p"] = build_fused()
    nc, names = _FUSED["p"]
    res = run_bass_kernel_spmd(nc, fused_in_maps(inp, names), core_ids=list(range(NCORES))).results
    out = np.concatenate([np.asarray(r["hout"]) for r in res], 0).reshape(2, SEQ, 1024)
    return out.astype(np.float32)
```
